# Optimizing a Trainium2 kernel written in Bass

```python
import math
import jax, jax.numpy as jnp
from jax import lax
import numpy as np

D_MODEL = 1024
BATCH = 2
SEQ = 16384
DEPTH = 2

N_HEADS_ATTN = 8
HEAD_DIM = 64
V_DIM = 2 * HEAD_DIM
QK_WIDTH = N_HEADS_ATTN * 2 * HEAD_DIM
ATTN_WIDTH = N_HEADS_ATTN * V_DIM
Q_BLOCK = 128
N_BUCKETS = 32
MAX_DISTANCE = 128
CONV_WIDTH = D_MODEL
CONV_K = 3
IN_SPLITS = (QK_WIDTH, 2 * QK_WIDTH, 2 * QK_WIDTH + ATTN_WIDTH,
             2 * QK_WIDTH + ATTN_WIDTH + CONV_WIDTH,
             2 * QK_WIDTH + ATTN_WIDTH + 2 * CONV_WIDTH,
             2 * QK_WIDTH + ATTN_WIDTH + 3 * CONV_WIDTH,
             2 * QK_WIDTH + ATTN_WIDTH + 3 * CONV_WIDTH + D_MODEL)
IN_COLS = 2 * QK_WIDTH + ATTN_WIDTH + 3 * CONV_WIDTH + 2 * D_MODEL
N_GROUPS = 4
EXPERTS_PER_GROUP = 8
N_EXPERTS = N_GROUPS * EXPERTS_PER_GROUP
TOP_K_IN_GROUP = 2
D_EXPERT = 512
EXPERT_BLOCK = 128
LN_EPS = 1e-5
DN_ALPHA = (2 * DEPTH) ** 0.25
DN_BETA = (8 * DEPTH) ** -0.25

kernel_name = "hybrid_diffattn_shortconv_hmoe_deepnorm"


def layer_norm(x, g, b):
    x32 = x.astype(jnp.float32)
    mu = jnp.mean(x32, axis=-1, keepdims=True)
    var = jnp.mean(jnp.square(x32 - mu), axis=-1, keepdims=True)
    return ((x32 - mu) * lax.rsqrt(var + LN_EPS)).astype(x.dtype) * g + b


def rms_norm(x, g):
    x32 = x.astype(jnp.float32)
    ms = jnp.mean(jnp.square(x32), axis=-1, keepdims=True)
    return (x32 * lax.rsqrt(ms + LN_EPS)).astype(x.dtype) * g


def rel_bucket(rel):
    n = jnp.maximum(rel, 0)
    max_exact = N_BUCKETS // 2
    nf = jnp.maximum(n, 1).astype(jnp.float32)
    large = max_exact + (jnp.log(nf / max_exact) / math.log(MAX_DISTANCE / max_exact)
                         * (N_BUCKETS - max_exact)).astype(jnp.int32)
    large = jnp.minimum(large, N_BUCKETS - 1)
    return jnp.where(n < max_exact, n, large)


def diff_attention(q, k, v, lam, rel_table):
    B, S = q.shape[:2]
    nb = S // Q_BLOCK
    scale = HEAD_DIM ** -0.5
    k1, k2 = k[:, :, :, 0], k[:, :, :, 1]
    kpos = jnp.arange(S, dtype=jnp.int32)
    qb = q.reshape(B, nb, Q_BLOCK, N_HEADS_ATTN, 2, HEAD_DIM).transpose(1, 0, 2, 3, 4, 5)
    starts = jnp.arange(nb, dtype=jnp.int32) * Q_BLOCK

    def block(args):
        qblk, start = args
        qpos = start + jnp.arange(Q_BLOCK, dtype=jnp.int32)
        rel = qpos[:, None] - kpos[None, :]
        bias = jnp.transpose(rel_table[rel_bucket(rel)], (2, 0, 1)).astype(jnp.float32)
        causal = rel >= 0

        def probs(qh, kh):
            s = jnp.einsum('bqhd,bkhd->bhqk', qh, kh).astype(jnp.float32) * scale + bias
            return jax.nn.softmax(jnp.where(causal, s, -jnp.inf), axis=-1)

        a = probs(qblk[:, :, :, 0], k1) - lam * probs(qblk[:, :, :, 1], k2)
        return jnp.einsum('bhqk,bkhe->bqhe', a.astype(v.dtype), v)

    out = lax.map(block, (qb, starts))
    return out.transpose(1, 0, 2, 3, 4).reshape(B, S, N_HEADS_ATTN, V_DIM)


def short_conv(u, w):
    return lax.conv_general_dilated(u, w[:, None, :], window_strides=(1,),
                                    padding=((CONV_K - 1, 0),),
                                    dimension_numbers=('NWC', 'WIO', 'NWC'),
                                    feature_group_count=u.shape[-1])


def mixer(h, layer, w_in, gate_b, lam_vecs, subln_g, w_attn_proj, conv_w, w_conv_proj, w_out, rel_table):
    B, S, _ = h.shape
    z = h @ w_in
    q, k, v, cb, cc, cx, ga, gc = jnp.split(z, IN_SPLITS, axis=-1)
    lam_init = 0.8 - 0.6 * math.exp(-0.3 * layer)
    lv = lam_vecs.astype(jnp.float32)
    lam = jnp.exp(jnp.sum(lv[0] * lv[1])) - jnp.exp(jnp.sum(lv[2] * lv[3])) + lam_init
    o = diff_attention(q.reshape(B, S, N_HEADS_ATTN, 2, HEAD_DIM),
                       k.reshape(B, S, N_HEADS_ATTN, 2, HEAD_DIM),
                       v.reshape(B, S, N_HEADS_ATTN, V_DIM), lam, rel_table)
    o = (rms_norm(o, subln_g) * (1.0 - lam_init)).reshape(B, S, ATTN_WIDTH) @ w_attn_proj
    yc = (cb * short_conv(cc * cx, conv_w)) @ w_conv_proj
    m = jax.nn.sigmoid(ga + gate_b[0]) * o + jax.nn.sigmoid(gc + gate_b[1]) * yc
    return m @ w_out


def hier_moe(x, w_rg, b_rg, w_re, b_re, w1, w3, w2):
    B, S, D = x.shape
    n_tok = B * S
    n_asg = n_tok * TOP_K_IN_GROUP
    xt = x.reshape(n_tok, D)
    g_prob = jax.nn.softmax((xt @ w_rg + b_rg).astype(jnp.float32), axis=-1)
    g_p, g_idx = lax.top_k(g_prob, 1)
    e_logits = (xt @ w_re + b_re).astype(jnp.float32).reshape(n_tok, N_GROUPS, EXPERTS_PER_GROUP)
    e_logits = jnp.take_along_axis(e_logits, g_idx[:, :, None], axis=1)[:, 0]
    e_p, e_loc = lax.top_k(jax.nn.softmax(e_logits, axis=-1), TOP_K_IN_GROUP)
    gate = g_p * e_p / jnp.sum(e_p, axis=-1, keepdims=True)
    expert = (g_idx * EXPERTS_PER_GROUP + e_loc).reshape(-1)
    order = jnp.argsort(expert)
    e_sorted = expert[order]
    tok = order // TOP_K_IN_GROUP
    sizes = jnp.bincount(expert, length=N_EXPERTS)
    padded = (sizes + EXPERT_BLOCK - 1) // EXPERT_BLOCK * EXPERT_BLOCK
    pad_end = jnp.cumsum(padded)
    pad_start = pad_end - padded
    start = jnp.cumsum(sizes) - sizes
    dest = pad_start[e_sorted] + jnp.arange(n_asg) - start[e_sorted]
    n_blk = (n_asg + N_EXPERTS * (EXPERT_BLOCK - 1) + EXPERT_BLOCK - 1) // EXPERT_BLOCK
    xbuf = jnp.zeros((n_blk * EXPERT_BLOCK, D), x.dtype).at[dest].set(xt[tok])
    blk_e = jnp.minimum(jnp.searchsorted(pad_end, jnp.arange(n_blk) * EXPERT_BLOCK, side='right'),
                        N_EXPERTS - 1)

    def expert_block(args):
        xb, e = args
        return (jax.nn.silu(xb @ w1[e]) * (xb @ w3[e])) @ w2[e]

    ybuf = lax.map(expert_block, (xbuf.reshape(n_blk, EXPERT_BLOCK, D), blk_e)).reshape(-1, D)
    y = ybuf[dest] * gate.reshape(-1)[order][:, None].astype(x.dtype)
    return jnp.zeros_like(xt).at[tok].add(y).reshape(B, S, D)


def setup_inputs(seed: int = 0) -> dict:
    key = jax.random.key(seed)
    ks = jax.random.split(key, 24)

    def nrm(k, shape, scale):
        return jax.random.normal(k, shape, jnp.float32) * scale

    L, D = DEPTH, D_MODEL
    return {
        "x": nrm(ks[0], (BATCH, SEQ, D), 1.0),
        "ln0_g": 1.0 + nrm(ks[1], (D,), 0.02),
        "ln0_b": nrm(ks[2], (D,), 0.02),
        "rel_table": nrm(ks[3], (N_BUCKETS, N_HEADS_ATTN), 0.5),
        "w_in": nrm(ks[4], (L, D, IN_COLS), D ** -0.5),
        "gate_b": nrm(ks[5], (L, 2, D), 0.1),
        "lam_vecs": nrm(ks[6], (L, 4, HEAD_DIM), 0.1),
        "subln_g": 1.0 + nrm(ks[7], (L, V_DIM), 0.02),
        "w_attn_proj": nrm(ks[8], (L, ATTN_WIDTH, D), ATTN_WIDTH ** -0.5),
        "conv_w": nrm(ks[9], (L, CONV_K, CONV_WIDTH), CONV_K ** -0.5),
        "w_conv_proj": nrm(ks[10], (L, CONV_WIDTH, D), CONV_WIDTH ** -0.5),
        "w_out": nrm(ks[11], (L, D, D), D ** -0.5 * DN_BETA),
        "ln1_g": 1.0 + nrm(ks[12], (L, D), 0.02),
        "ln1_b": nrm(ks[13], (L, D), 0.02),
        "w_rg": nrm(ks[14], (L, D, N_GROUPS), D ** -0.5),
        "b_rg": nrm(ks[15], (L, N_GROUPS), 0.01),
        "w_re": nrm(ks[16], (L, D, N_EXPERTS), D ** -0.5),
        "b_re": nrm(ks[17], (L, N_EXPERTS), 0.01),
        "w1": nrm(ks[18], (L, N_EXPERTS, D, D_EXPERT), D ** -0.5),
        "w3": nrm(ks[19], (L, N_EXPERTS, D, D_EXPERT), D ** -0.5),
        "w2": nrm(ks[20], (L, N_EXPERTS, D_EXPERT, D), D_EXPERT ** -0.5 * DN_BETA),
        "ln2_g": 1.0 + nrm(ks[21], (L, D), 0.02),
        "ln2_b": nrm(ks[22], (L, D), 0.02),
    }


def reference(x, ln0_g, ln0_b, rel_table, w_in, gate_b, lam_vecs, subln_g, w_attn_proj, conv_w,
              w_conv_proj, w_out, ln1_g, ln1_b, w_rg, b_rg, w_re, b_re, w1, w3, w2, ln2_g, ln2_b):
    h = layer_norm(x, ln0_g, ln0_b)
    for l in range(DEPTH):
        mix = mixer(h, l, w_in[l], gate_b[l], lam_vecs[l], subln_g[l], w_attn_proj[l],
                    conv_w[l], w_conv_proj[l], w_out[l], rel_table)
        h = layer_norm(DN_ALPHA * h + mix, ln1_g[l], ln1_b[l])
        ffn = hier_moe(h, w_rg[l], b_rg[l], w_re[l], b_re[l], w1[l], w3[l], w2[l])
        h = layer_norm(DN_ALPHA * h + ffn, ln2_g[l], ln2_b[l])
    return h
```

```python
import math
from contextlib import ExitStack
import numpy as np
import concourse.bass as bass
import concourse.mybir as mybir
from concourse.bass_utils import run_bass_kernel_spmd

F32 = mybir.dt.float32
BF16 = mybir.dt.bfloat16
I32 = mybir.dt.int32
AF = mybir.ActivationFunctionType
ALU = mybir.AluOpType
AX = mybir.AxisListType


class Trk:
    __slots__ = ("w", "r")

    def __init__(self):
        self.w = None
        self.r = {}


class Buf(Trk):
    __slots__ = ("t", "subs")

    def __init__(self, t):
        Trk.__init__(self)
        self.t = t
        self.subs = {}

    def __getitem__(self, idx):
        return self.t[idx]

    def trk(self, key):
        s = self.subs.get(key)
        if s is None:
            s = self.subs[key] = Trk()
        return s


class KB:
    NDMA = 32

    def __init__(self, nc, st):
        self.nc = nc
        self.st = st
        self.eng = {"pe": nc.tensor, "act": nc.scalar, "dve": nc.vector,
                    "pool": nc.gpsimd, "sp": nc.sync}
        self.sem = {}
        self.cnt = {}
        self.known = {k: {} for k in self.eng}
        for k in ("pe", "act", "dve", "pool"):
            self.sem[k] = st.enter_context(nc.semaphore("s_" + k))
            self.cnt[k] = 0
        self.dsem = []
        for i in range(self.NDMA):
            key = "d%d" % i
            self.sem[key] = st.enter_context(nc.semaphore("s_" + key))
            self.cnt[key] = 0
            self.dsem.append(key)
        self.dnext = 0
        self._clear_sems()
        self.out_trk = Trk()
        self.prog = {k: [] for k in self.eng}
        self.nbuf = 0

    def _clear_sems(self):
        for h in self.sem.values():
            self.nc.gpsimd.sem_clear(h)
        self.nc.all_engine_barrier()

    def sb(self, name, shape, dtype):
        t = self.st.enter_context(self.nc.sbuf_tensor(name, list(shape), dtype))
        return Buf(t)

    def ps(self, name, shape, dtype):
        t = self.st.enter_context(self.nc.psum_tensor(name, list(shape), dtype))
        return Buf(t)

    def _wait(self, ename, deps):
        kn = self.known[ename]
        best = {}
        for d in deps:
            if d is None:
                continue
            k, v = d
            if best.get(k, 0) < v:
                best[k] = v
        waits = []
        for k, v in best.items():
            if k == "pe" and ename == "pe":
                continue
            if kn.get(k, 0) >= v:
                continue
            waits.append((k, v))
            kn[k] = v
        return waits

    def _deps(self, reads, writes):
        deps = []
        for b in reads:
            deps.append(b.w)
        for b in writes:
            deps.append(b.w)
            for k, v in b.r.items():
                deps.append((k, v))
        return deps

    def _commit(self, tok, reads, writes):
        k, v = tok
        for b in reads:
            if b.r.get(k, 0) < v:
                b.r[k] = v
        for b in writes:
            b.w = tok
            b.r = {}

    def op(self, ename, reads, writes, fn):
        waits = self._wait(ename, self._deps(reads, writes))
        self.cnt[ename] += 1
        self.prog[ename].append((waits, fn, ename, 1))
        tok = (ename, self.cnt[ename])
        self._commit(tok, reads, writes)
        return tok

    def dma_fn(self, qname, fn, reads, writes):
        key = self.dsem[self.dnext]
        self.dnext = (self.dnext + 1) % self.NDMA
        deps = self._deps(reads, writes)
        if self.cnt[key] > 0:
            deps.append((key, self.cnt[key]))
        waits = self._wait(qname, deps)
        self.cnt[key] += 16
        self.prog[qname].append((waits, fn, key, 16))
        tok = (key, self.cnt[key])
        self._commit(tok, reads, writes)
        return tok

    def dma(self, qname, out, in_, reads, writes, **kw):
        return self.dma_fn(qname, lambda e: e.dma_start(out=out, in_=in_, **kw), reads, writes)

    def finish(self):
        deps = [self.out_trk.w]
        for key in self.dsem:
            if self.cnt[key] > 0:
                deps.append((key, self.cnt[key]))
        for k in ("pe", "act", "dve", "pool"):
            if self.cnt[k] > 0:
                deps.append((k, self.cnt[k]))
        final_waits = self._wait("sp", deps)
        sem = self.sem
        prog = self.prog

        def emit(e, items, tail=()):
            for waits, fn, ik, iv in items:
                for k, v in waits:
                    e.wait_ge(sem[k], v)
                fn(e).then_inc(sem[ik], iv)
            for k, v in tail:
                e.wait_ge(sem[k], v)

        with self.nc.Block() as block:
            @block.tensor
            def _(e):
                emit(e, prog["pe"])

            @block.scalar
            def _(e):
                emit(e, prog["act"])

            @block.vector
            def _(e):
                emit(e, prog["dve"])

            @block.gpsimd
            def _(e):
                emit(e, prog["pool"])

            @block.sync
            def _(e):
                emit(e, prog["sp"], final_waits)

        self._clear_sems()


D = 1024
NKC = 8
LN_EPS = 1e-5
DN_ALPHA = 4 ** 0.25


class PsumRing:
    def __init__(self, kb, n, name="ps"):
        self.bufs = [kb.ps("%s%d" % (name, i), [128, 512], F32) for i in range(n)]
        self.i = 0

    def next(self):
        b = self.bufs[self.i]
        self.i = (self.i + 1) % len(self.bufs)
        return b


class Ring:
    def __init__(self, bufs):
        self.bufs = bufs
        self.i = 0

    def next(self):
        b = self.bufs[self.i]
        self.i = (self.i + 1) % len(self.bufs)
        return b


def mm_group(kb, ps, out_ap, lhs, rhs, reads):
    n = len(lhs)

    def fn(e):
        ins = None
        for k in range(n):
            ins = e.matmul(out_ap, lhs[k], rhs[k], start=(k == 0), stop=(k == n - 1))
        return ins
    return kb.op("pe", reads, [ps], fn)


def layer_norm_tile(kb, x, xo, g_rep, b_rep, st6, mv, rstd):
    def stats(e):
        e.bn_stats(st6[:, 0:6], x[:, 0:512])
        return e.bn_stats(st6[:, 6:12], x[:, 512:1024])
    kb.op("dve", [x], [st6], stats)
    kb.op("dve", [st6], [mv], lambda e: e.bn_aggr(mv[:, :], st6[:, :]))
    kb.op("dve", [mv], [rstd], lambda e: e.tensor_scalar(
        rstd[:, :], mv[:, 1:2], LN_EPS, None, ALU.add))
    kb.op("act", [rstd], [rstd], lambda e: e.activation(rstd[:, :], rstd[:, :], AF.Sqrt))
    kb.op("dve", [rstd], [rstd], lambda e: e.reciprocal(rstd[:, :], rstd[:, :]))
    kb.op("dve", [x, mv, rstd], [xo], lambda e: e.tensor_scalar(
        xo[:, :], x[:, :], mv[:, 0:1], rstd[:, 0:1], ALU.subtract, ALU.mult))
    kb.op("dve", [xo, g_rep], [xo], lambda e: e.tensor_tensor(
        xo[:, :], xo[:, :], g_rep[:, :], ALU.mult))
    kb.op("dve", [xo, b_rep], [xo], lambda e: e.tensor_tensor(
        xo[:, :], xo[:, :], b_rep[:, :], ALU.add))


def load_const(kb, name, dram_ap, shape, dtype=F32, q="sp"):
    b = kb.sb(name, shape, dtype)
    kb.dma(q, b[tuple(slice(None) for _ in shape)], dram_ap, [], [b])
    return b


def transpose_to(kb, src, identb, pT, dst_ap, dst_trks, nch, eng="act", strided=False):
    def tr(e):
        ins = None
        for c in range(nch):
            sl = slice(c, None, nch) if strided else slice(c * 128, (c + 1) * 128)
            ins = e.transpose(pT[:, c, :], src[:, sl], identb[:, :])
        return ins
    kb.op("pe", [src, identb], [pT], tr)
    if eng == "act":
        kb.op("act", [pT], dst_trks, lambda e: e.copy(dst_ap, pT[:, 0:nch, :]))
    else:
        kb.op("dve", [pT], dst_trks, lambda e: e.tensor_copy(dst_ap, pT[:, 0:nch, :]))


def build_A(T, layer0):
    nc = bass.Bass("TRN2", target_bir_lowering=False)
    TH = T + 128
    NB = T // 512
    NT = T // 128
    with ExitStack() as st:
        kb = KB(nc, st)
        dt = nc.dram_tensor
        hin = dt("hin", [TH, D], F32, kind="ExternalInput").ap()
        halo_mask = dt("halo_mask", [128, 1], F32, kind="ExternalInput").ap()
        w_in = dt("w_in", [D, 8192], F32, kind="ExternalInput").ap()
        gate_b = dt("gate_b", [128, 16], F32, kind="ExternalInput").ap()
        conv_w = dt("conv_w", [128, 24], F32, kind="ExternalInput").ap()
        w_cp = dt("w_cp", [D, D], F32, kind="ExternalInput").ap()
        ident = dt("ident", [128, 128], F32, kind="ExternalInput").ap()
        if layer0:
            lng = dt("lng", [128, D], F32, kind="ExternalInput").ap()
            lnb = dt("lnb", [128, D], F32, kind="ExternalInput").ap()
            h0 = dt("h0", [T, D], F32, kind="ExternalOutput").ap()
        qT = dt("qT", [D, T], BF16, kind="ExternalOutput").ap()
        kT = dt("kT", [D, T], BF16, kind="ExternalOutput").ap()
        v = dt("v", [T, D], BF16, kind="ExternalOutput").ap()
        SaT = dt("SaT", [D, T], BF16, kind="ExternalOutput").ap()
        YT = dt("YT", [D, T], BF16, kind="ExternalOutput").ap()
        OUT = kb.out_trk

        identf = load_const(kb, "identf", ident, [128, 128])
        identb = kb.sb("identb", [128, 128], BF16)
        kb.op("dve", [identf], [identb], lambda e: e.tensor_copy(identb[:, :], identf[:, :]))
        gb = load_const(kb, "gb", gate_b, [128, 16])
        cw = load_const(kb, "cw", conv_w, [128, 24])
        hm = load_const(kb, "hm", halo_mask, [128, 1])
        if layer0:
            g_rep = load_const(kb, "g_rep", lng, [128, D])
            b_rep = load_const(kb, "b_rep", lnb, [128, D])

        hT = kb.sb("hT", [128, NKC, TH], BF16)
        pT = kb.ps("pT", [128, NKC, 128], BF16)
        psr = PsumRing(kb, 6)

        xr = Ring([kb.sb("x%d" % i, [128, D], F32) for i in range(3)])
        hbr = Ring([kb.sb("hb%d" % i, [128, D], BF16) for i in range(2)])
        st6 = kb.sb("st6", [128, 12], F32)
        mv = kb.sb("mv", [128, 2], F32)
        rstd = kb.sb("rstd", [128, 1], F32)
        for i in range(TH // 128):
            x = xr.next()
            hb = hbr.next()
            kb.dma("sp", x[:, :], hin[i * 128:(i + 1) * 128, :], [], [x])
            if layer0:
                layer_norm_tile(kb, x, x, g_rep, b_rep, st6, mv, rstd)
                if i >= 1:
                    kb.dma("pool", h0[(i - 1) * 128:i * 128, :], x[:, :], [x], [OUT])
            kb.op("act", [x], [hb], lambda e, x=x, hb=hb: e.copy(hb[:, :], x[:, :]))
            transpose_to(kb, hb, identb, pT, hT[:, :, i * 128:(i + 1) * 128], [hT.trk(i)], NKC,
                         eng="dve" if i % 2 else "act")

        def hT_blk(n):
            return [hT.trk(1 + 4 * n + a) for a in range(4)]

        wst = Ring([kb.sb("wst%d" % i, [128, NKC, 512], F32) for i in range(1)])
        wbf = Ring([kb.sb("wbf%d" % i, [128, NKC, 512], BF16) for i in range(2)])

        def load_w(srcs):
            ws = wst.next()
            wb = wbf.next()
            c0 = 0
            for ap in srcs:
                nco = ap.shape[1]
                kb.dma("sp", ws[:, :, c0:c0 + nco], ap.rearrange("(kc p) n -> p kc n", p=128),
                       [], [ws])
                c0 += nco
            kb.op("pool", [ws], [wb], lambda e, ws=ws, wb=wb, c0=c0: e.tensor_copy(
                wb[:, :, 0:c0], ws[:, :, 0:c0]))
            return wb

        evi = [0]

        def evac(ps, out_ap, out_trks, src_ap, extra_reads=()):
            evi[0] += 1
            if evi[0] % 2:
                kb.op("act", [ps] + list(extra_reads), out_trks, lambda e: e.copy(out_ap, src_ap))
            else:
                kb.op("dve", [ps] + list(extra_reads), out_trks, lambda e: e.tensor_copy(out_ap, src_ap))

        ostage = Ring([kb.sb("ost%d" % i, [128, T], BF16) for i in range(2)])

        for p in range(4):
            wb = load_w([w_in[:, p * 512:(p + 1) * 512]])
            for cc in range(4):
                ch = p * 4 + cc
                og = ostage.next()
                for n in range(NB):
                    ps = psr.next()
                    mm_group(kb, ps, ps[:, :],
                             [wb[:, kc, cc * 128:(cc + 1) * 128] for kc in range(NKC)],
                             [hT[:, kc, 128 + 512 * n:128 + 512 * (n + 1)] for kc in range(NKC)],
                             [wb] + hT_blk(n))
                    evac(ps, og[:, 512 * n:512 * (n + 1)], [og], ps[:, :])
                dst = qT if ch < 8 else kT
                r0 = (ch % 8) * 128
                kb.dma("pool", dst[r0:r0 + 128, :], og[:, :], [og], [OUT])

        vstage = Ring([kb.sb("vst%d" % i, [128, 512], BF16) for i in range(3)])
        for p in range(2):
            wb = load_w([w_in[:, 2048 + p * 512:2048 + (p + 1) * 512]])
            for i in range(NT):
                ps = psr.next()
                mm_group(kb, ps, ps[:, :],
                         [hT[:, kc, 128 + 128 * i:128 + 128 * (i + 1)] for kc in range(NKC)],
                         [wb[:, kc, :] for kc in range(NKC)],
                         [wb, hT.trk(1 + i)])
                vs = vstage.next()
                evac(ps, vs[:, :], [vs], ps[:, :])
                kb.dma("pool", v[i * 128:(i + 1) * 128, p * 512:(p + 1) * 512], vs[:, :], [vs], [OUT])

        NH = 2 if T >= 1024 else 1
        TH2 = T // NH
        NB2 = TH2 // 512
        gT = kb.sb("gT", [128, NKC, TH2], BF16)
        uT = kb.sb("uT", [128, TH2 + 2], F32)
        cbT = kb.sb("cbT", [128, TH2], F32)
        yacc = kb.sb("yacc", [128, TH2], F32)
        tmpr = Ring([kb.sb("tmp%d" % i, [128, 512], F32) for i in range(2)])
        for hf in range(NH):
            t0 = hf * TH2
            for j in range(8):
                wb = load_w([w_in[:, 3072 + 128 * j:3072 + 128 * (j + 1)],
                             w_in[:, 4096 + 128 * j:4096 + 128 * (j + 1)],
                             w_in[:, 5120 + 128 * j:5120 + 128 * (j + 1)]])
                ps = psr.next()
                hc = 128 + t0
                htr = [hT.trk(0)] if hf == 0 else [hT.trk((hc - 2) // 128)]

                def halo_mm(e, ps=ps, wb=wb, hc=hc):
                    ins = None
                    for g in range(2):
                        for kc in range(NKC):
                            ins = e.matmul(ps[:, 2 * g:2 * g + 2], wb[:, kc, 128 * (g + 1):128 * (g + 2)],
                                           hT[:, kc, hc - 2:hc], start=(kc == 0), stop=(kc == NKC - 1))
                    return ins
                kb.op("pe", [wb] + htr, [ps], halo_mm)
                tm = tmpr.next()
                kb.op("act", [ps], [tm], lambda e, ps=ps, tm=tm: e.copy(tm[:, 0:2], ps[:, 0:2]))
                kb.op("dve", [ps, tm], [tm], lambda e, ps=ps, tm=tm: e.tensor_tensor(
                    tm[:, 2:4], tm[:, 0:2], ps[:, 2:4], ALU.mult))
                if hf == 0:
                    kb.op("dve", [tm, hm], [uT], lambda e, tm=tm: e.tensor_scalar(
                        uT[:, 0:2], tm[:, 2:4], hm[:, 0:1], None, ALU.mult))
                else:
                    kb.op("dve", [tm], [uT], lambda e, tm=tm: e.tensor_copy(uT[:, 0:2], tm[:, 2:4]))
                for n2 in range(NB2):
                    n = hf * NB2 + n2
                    rhs = [hT[:, kc, 128 + 512 * n:128 + 512 * (n + 1)] for kc in range(NKC)]
                    pcb, pcc, pcx = psr.next(), psr.next(), psr.next()
                    for g, ps in enumerate((pcb, pcc, pcx)):
                        mm_group(kb, ps, ps[:, :], [wb[:, kc, 128 * g:128 * (g + 1)] for kc in range(NKC)],
                                 rhs, [wb] + hT_blk(n))
                    tm = tmpr.next()
                    kb.op("act", [pcc], [tm], lambda e, pcc=pcc, tm=tm: e.copy(tm[:, :], pcc[:, :]))
                    kb.op("dve", [tm, pcx], [uT], lambda e, tm=tm, pcx=pcx, n2=n2: e.tensor_tensor(
                        uT[:, 2 + 512 * n2:2 + 512 * (n2 + 1)], tm[:, :], pcx[:, :], ALU.mult))
                    kb.op("act", [pcb], [cbT], lambda e, pcb=pcb, n2=n2: e.copy(
                        cbT[:, 512 * n2:512 * (n2 + 1)], pcb[:, :]))
                kb.op("dve", [uT, cw], [yacc], lambda e, j=j: e.tensor_scalar(
                    yacc[:, :], uT[:, 2:TH2 + 2], cw[:, 16 + j:17 + j], None, ALU.mult))
                kb.op("dve", [uT, cw, yacc], [yacc], lambda e, j=j: e.scalar_tensor_tensor(
                    yacc[:, :], uT[:, 1:TH2 + 1], cw[:, 8 + j:9 + j], yacc[:, :], ALU.mult, ALU.add))
                kb.op("dve", [uT, cw, yacc], [yacc], lambda e, j=j: e.scalar_tensor_tensor(
                    yacc[:, :], uT[:, 0:TH2], cw[:, j:j + 1], yacc[:, :], ALU.mult, ALU.add))
                kb.op("dve", [yacc, cbT], [gT.trk(j)], lambda e, j=j: e.tensor_tensor(
                    gT[:, j, :], yacc[:, :], cbT[:, :], ALU.mult))

            gT_all = [gT.trk(j) for j in range(8)]
            for j in range(8):
                wb = load_w([w_in[:, 7168 + 128 * j:7168 + 128 * (j + 1)], w_cp[:, 128 * j:128 * (j + 1)]])
                og = ostage.next()
                for n2 in range(NB2):
                    n = hf * NB2 + n2
                    pyc, pgc = psr.next(), psr.next()
                    mm_group(kb, pyc, pyc[:, :], [wb[:, kc, 128:256] for kc in range(NKC)],
                             [gT[:, kc, 512 * n2:512 * (n2 + 1)] for kc in range(NKC)], [wb] + gT_all)
                    mm_group(kb, pgc, pgc[:, :], [wb[:, kc, 0:128] for kc in range(NKC)],
                             [hT[:, kc, 128 + 512 * n:128 + 512 * (n + 1)] for kc in range(NKC)],
                             [wb] + hT_blk(n))
                    tm = tmpr.next()
                    kb.op("act", [pgc, gb], [tm], lambda e, pgc=pgc, tm=tm, j=j: e.activation(
                        tm[:, :], pgc[:, :], AF.Sigmoid, bias=gb[:, 8 + j:9 + j]))
                    kb.op("dve", [tm, pyc], [og], lambda e, tm=tm, pyc=pyc, og=og, n2=n2: e.tensor_tensor(
                        og[:, 512 * n2:512 * (n2 + 1)], tm[:, :], pyc[:, :], ALU.mult))
                kb.dma("pool", YT[128 * j:128 * (j + 1), t0:t0 + TH2], og[:, 0:TH2], [og], [OUT])

        for p in range(2):
            wb = load_w([w_in[:, 6144 + p * 512:6144 + (p + 1) * 512]])
            for cc in range(4):
                j = p * 4 + cc
                og = ostage.next()
                for n in range(NB):
                    ps = psr.next()
                    mm_group(kb, ps, ps[:, :],
                             [wb[:, kc, cc * 128:(cc + 1) * 128] for kc in range(NKC)],
                             [hT[:, kc, 128 + 512 * n:128 + 512 * (n + 1)] for kc in range(NKC)],
                             [wb] + hT_blk(n))
                    kb.op("act", [ps, gb], [og], lambda e, ps=ps, og=og, n=n, j=j: e.activation(
                        og[:, 512 * n:512 * (n + 1)], ps[:, :], AF.Sigmoid, bias=gb[:, j:j + 1]))
                kb.dma("pool", SaT[128 * j:128 * (j + 1), :], og[:, :], [og], [OUT])
        kb.finish()
    return nc


FL = 1151
FP = FL + 1


def rel_bucket_np(rel):
    n = np.maximum(rel, 0)
    nf = np.maximum(n, 1).astype(np.float32)
    large = 16 + (np.log(nf / np.float32(16)) / np.float32(math.log(128 / 16)) * np.float32(16)).astype(np.int32)
    large = np.minimum(large, 31)
    return np.where(n < 16, n, large)


def bias_onehot():
    rel = np.arange(FL) - 511
    b = rel_bucket_np(rel)
    oh = np.zeros((33, FL), np.float32)
    for i in range(FL):
        if rel[i] < 0:
            oh[32, i] = 1.0
        else:
            oh[b[i], i] = 1.0
    return oh


def build_ATT(S):
    nc = bass.Bass("TRN2", target_bir_lowering=False)
    NQB = S // 512
    NKCH = S // 128
    with ExitStack() as st:
        kb = KB(nc, st)
        dt = nc.dram_tensor
        qT = dt("qT", [256, S], BF16, kind="ExternalInput").ap()
        kT = dt("kT", [256, S], BF16, kind="ExternalInput").ap()
        v = dt("v", [S, 256], BF16, kind="ExternalInput").ap()
        oh = dt("oh", [33, FL], F32, kind="ExternalInput").ap()
        tab = dt("tab", [33, 2], F32, kind="ExternalInput").ap()
        lamv = dt("lamv", [128, 256], F32, kind="ExternalInput").ap()
        subg = dt("subg", [128, 128], F32, kind="ExternalInput").ap()
        lamc_d = dt("lamc", [128, 2], F32, kind="ExternalInput").ap()
        ident = dt("ident", [128, 128], F32, kind="ExternalInput").ap()
        oT = dt("oT", [256, S], BF16, kind="ExternalOutput").ap()
        fscr = [nc.dram_tensor("fscr%d" % h, [128 * FP], F32) for h in range(2)]
        OUT = kb.out_trk

        identf = load_const(kb, "identf", ident, [128, 128])
        identb = kb.sb("identb", [128, 128], BF16)
        kb.op("dve", [identf], [identb], lambda e: e.tensor_copy(identb[:, :], identf[:, :]))
        ohs = load_const(kb, "ohs", oh, [33, FL])
        tabs = load_const(kb, "tabs", tab, [33, 2])
        lv = load_const(kb, "lv", lamv, [128, 256])
        g_rep = load_const(kb, "g_rep", subg, [128, 128])
        lamc = load_const(kb, "lamc_s", lamc_d, [128, 2])
        kb.op("dve", [g_rep, lamc], [g_rep], lambda e: e.tensor_scalar(
            g_rep[:, :], g_rep[:, :], lamc[:, 1:2], None, ALU.mult))
        epst = kb.sb("epst", [128, 1], F32)
        kb.op("dve", [], [epst], lambda e: e.memset(epst[:, :], LN_EPS))

        lt = kb.sb("lt", [128, 128], F32)
        ls = kb.sb("ls", [128, 2], F32)
        nlam = kb.sb("nlam", [128, 1], F32)
        kb.op("dve", [lv], [lt], lambda e: e.tensor_tensor(
            lt[:, 0:64], lv[:, 0:64], lv[:, 64:128], ALU.mult))
        kb.op("dve", [lv, lt], [lt], lambda e: e.tensor_tensor(
            lt[:, 64:128], lv[:, 128:192], lv[:, 192:256], ALU.mult))
        kb.op("dve", [lt], [ls], lambda e: e.reduce_sum(
            ls[:, 0:2], lt[:, :].rearrange("p (a b) -> p a b", a=2), axis=AX.X))
        kb.op("act", [ls], [ls], lambda e: e.activation(ls[:, :], ls[:, :], AF.Exp))
        kb.op("dve", [ls], [nlam], lambda e: e.tensor_tensor(
            nlam[:, :], ls[:, 1:2], ls[:, 0:1], ALU.subtract))
        kb.op("dve", [nlam, lamc], [nlam], lambda e: e.tensor_scalar(
            nlam[:, :], nlam[:, :], lamc[:, 0:1], None, ALU.add))

        s12 = Ring([kb.ps("s12_%d" % i, [128, 1024], F32) for i in range(2)])
        oacc = kb.ps("oacc", [128, 3, 512], F32)
        pT = kb.ps("pT", [128, 4, 128], BF16)

        def acc_ap(h2, qc):
            g = h2 * 4 + qc
            return oacc[:, g // 3, (g % 3) * 129:(g % 3) * 129 + 129]

        kTs = kb.sb("kTs", [128, S], BF16)
        vaug = kb.sb("vaug", [128, NKCH, 129], BF16)
        tabrep = kb.sb("tabrep", [33, 128], F32)
        frep = kb.sb("frep", [128, FL], F32)
        c31 = kb.sb("c31", [128, 1], F32)
        biasT = [kb.sb("biasT%d" % d, [128, 512], F32) for d in range(5)]
        qr = Ring([kb.sb("qb%d" % i, [128, 512], BF16) for i in range(3)])
        pr = Ring([kb.sb("pb%d" % i, [128, 1024], BF16) for i in range(3)])
        tmpr = Ring([kb.sb("tm%d" % i, [128, 1024], F32) for i in range(2)])
        osb = Ring([kb.sb("osb%d" % i, [128, 8, 132], F32) for i in range(2)])
        ostage = Ring([kb.sb("ostg%d" % i, [128, 512], BF16) for i in range(2)])
        ofr = Ring([kb.sb("of%d" % i, [128, 4, 128], F32) for i in range(3)])
        onr = Ring([kb.sb("on%d" % i, [128, 4, 128], BF16) for i in range(2)])
        smr = Ring([kb.sb("sm%d" % i, [128, 16], F32) for i in range(3)])
        junk = kb.sb("junk", [128, 128], F32)

        for hh in range(2):
            kb.dma("sp", kTs[:, :], kT[hh * 128:(hh + 1) * 128, :], [], [kTs])
            kb.dma("sp", vaug[:, :, 0:128],
                   v[:, hh * 128:(hh + 1) * 128].rearrange("(c p) e -> p c e", p=128), [], [vaug])
            kb.op("pool", [], [vaug], lambda e: e.memset(vaug[:, :, 128:129], 1.0))
            kb.op("dve", [tabs], [tabrep], lambda e, hh=hh: e.tensor_copy(
                tabrep[:, :], tabs[:, hh:hh + 1].to_broadcast([33, 128])))
            ps = s12.next()
            ps2 = s12.next()

            def fmm(e, ps=ps, ps2=ps2):
                e.matmul(ps[:, 0:512], tabrep[:, :], ohs[:, 0:512], start=True, stop=True)
                e.matmul(ps[:, 512:1024], tabrep[:, :], ohs[:, 512:1024], start=True, stop=True)
                return e.matmul(ps2[:, 0:FL - 1024], tabrep[:, :], ohs[:, 1024:FL], start=True, stop=True)
            kb.op("pe", [tabrep, ohs], [ps, ps2], fmm)
            kb.op("dve", [ps], [frep], lambda e, ps=ps: e.tensor_copy(frep[:, 0:1024], ps[:, :]))
            kb.op("dve", [ps2, frep], [frep], lambda e, ps2=ps2: e.tensor_copy(
                frep[:, 1024:FL], ps2[:, 0:FL - 1024]))
            kb.op("dve", [frep], [c31], lambda e: e.tensor_copy(c31[:, :], frep[:, FL - 1:FL]))
            ftrk = Trk()
            kb.dma("sp", bass.AP(fscr[hh], 0, [[FP, 128], [1, FL]]), frep[:, :], [frep], [ftrk])
            for d in range(5):
                delta = -128 + 128 * d
                kb.dma("sp", biasT[d][:, :], bass.AP(fscr[hh], 511 - delta, [[FL, 128], [1, 512]]),
                       [ftrk], [biasT[d]])

            steps = [(I, j) for I in range(NQB) for j in range(4 * I + 4)]
            qblk = {}
            deferred = []

            def get_q(I):
                if I not in qblk:
                    qb = qr.next()
                    kb.dma("sp", qb[:, :], qT[hh * 128:(hh + 1) * 128, 512 * I:512 * (I + 1)], [], [qb])
                    qblk[I] = qb
                return qblk[I]

            def qk(I, j):
                qb = get_q(I)
                ps = s12.next()

                def fn(e, ps=ps, qb=qb, j=j):
                    e.matmul(ps[:, 0:512], kTs[0:64, 128 * j:128 * (j + 1)], qb[0:64, :],
                             start=True, stop=True)
                    return e.matmul(ps[:, 512:1024], kTs[64:128, 128 * j:128 * (j + 1)], qb[64:128, :],
                                    start=True, stop=True)
                kb.op("pe", [kTs, qb], [ps], fn)
                return ps

            cur = qk(*steps[0])
            for si, (I, j) in enumerate(steps):
                nxt = qk(*steps[si + 1]) if si + 1 < len(steps) else None
                pb = pr.next()
                if j <= 4 * I - 2:
                    kb.op("act", [cur, c31], [pb], lambda e, cur=cur, pb=pb: e.activation(
                        pb[:, :], cur[:, :], AF.Exp, bias=c31[:, 0:1], scale=0.125))
                else:
                    d = j - (4 * I - 1)
                    bt = biasT[d]
                    tm = tmpr.next()
                    for h2 in range(2):
                        kb.op("dve", [cur, bt], [tm], lambda e, cur=cur, tm=tm, bt=bt, h2=h2: e.scalar_tensor_tensor(
                            tm[:, 512 * h2:512 * (h2 + 1)], cur[:, 512 * h2:512 * (h2 + 1)], 0.125, bt[:, :],
                            ALU.mult, ALU.add))
                    kb.op("act", [tm], [pb], lambda e, tm=tm, pb=pb: e.activation(
                        pb[:, :], tm[:, :], AF.Exp))
                last = (j == 4 * I + 3)

                def av(e, pb=pb, j=j, last=last):
                    ins = None
                    for h2 in range(2):
                        for qc in range(4):
                            g = h2 * 4 + qc
                            ins = e.matmul(acc_ap(h2, qc), pb[:, 512 * h2 + 128 * qc:512 * h2 + 128 * (qc + 1)],
                                           vaug[:, j, :], start=(j == 0 and g % 3 == 0), stop=last,
                                           skip_group_check=True)
                    return ins
                kb.op("pe", [pb, vaug], [oacc], av)
                if last:
                    ob = osb.next()
                    for bnk in range(3):
                        ng = 3 if bnk < 2 else 2
                        kb.op("dve", [oacc], [ob], lambda e, ob=ob, bnk=bnk, ng=ng: e.tensor_copy(
                            ob[:, 3 * bnk:3 * bnk + ng, 0:129],
                            oacc[:, bnk, 0:ng * 129].rearrange("p (g c) -> p g c", g=ng)))
                    rl = smr.next()
                    of = ofr.next()
                    kb.op("dve", [ob], [rl], lambda e, ob=ob, rl=rl: e.reciprocal(
                        rl[:, 0:8].unsqueeze(2), ob[:, 0:8, 128:129]))
                    kb.op("dve", [rl, nlam], [rl], lambda e, rl=rl: e.tensor_scalar(
                        rl[:, 4:8], rl[:, 4:8], nlam[:, 0:1], None, ALU.mult))
                    kb.op("dve", [ob, rl], [of], lambda e, ob=ob, rl=rl, of=of: e.tensor_tensor(
                        of[:, :, :], ob[:, 0:4, 0:128], rl[:, 0:4].unsqueeze(2).broadcast_to([128, 4, 128]),
                        ALU.mult))
                    kb.op("dve", [ob, rl], [ob], lambda e, ob=ob, rl=rl: e.tensor_tensor(
                        ob[:, 4:8, 0:128], ob[:, 4:8, 0:128],
                        rl[:, 4:8].unsqueeze(2).broadcast_to([128, 4, 128]), ALU.mult))
                    kb.op("dve", [ob, of], [of], lambda e, ob=ob, of=of: e.tensor_tensor(
                        of[:, :, :], of[:, :, :], ob[:, 4:8, 0:128], ALU.add))
                    kb.op("dve", [of], [ob], lambda e, ob=ob, of=of: e.tensor_tensor(
                        ob[:, 0:4, 0:128], of[:, :, :], of[:, :, :], ALU.mult))
                    kb.op("dve", [ob], [rl], lambda e, ob=ob, rl=rl: e.reduce_sum(
                        rl[:, 8:12], ob[:, 0:4, 0:128], axis=AX.X))

                    def e2(rl=rl):
                        kb.op("act", [rl, epst], [rl], lambda e: e.activation(
                            rl[:, 12:16], rl[:, 8:12], AF.Ln, bias=epst[:, 0:1], scale=1.0 / 128))
                        kb.op("act", [rl], [rl], lambda e: e.activation(
                            rl[:, 8:12], rl[:, 12:16], AF.Exp, scale=-0.5))

                    def e3(rl=rl, of=of, I=I):
                        on = onr.next()
                        og = ostage.next()
                        kb.op("dve", [of, rl], [of], lambda e: e.tensor_tensor(
                            of[:, :, :], of[:, :, :], rl[:, 8:12].unsqueeze(2).broadcast_to([128, 4, 128]),
                            ALU.mult))
                        kb.op("dve", [of, g_rep], [on], lambda e: e.tensor_tensor(
                            on[:, :, :], of[:, :, :], g_rep[:, :].unsqueeze(1).broadcast_to([128, 4, 128]),
                            ALU.mult))

                        def trq(e):
                            ins = None
                            for qc in range(4):
                                ins = e.transpose(pT[:, qc, :], on[:, qc, :], identb[:, :])
                            return ins
                        kb.op("pe", [on, identb], [pT], trq)
                        kb.op("dve", [pT], [og], lambda e: e.tensor_copy(
                            og[:, :].rearrange("p (a b) -> p a b", a=4), pT[:, :, :]))
                        kb.dma("sp", oT[hh * 128:(hh + 1) * 128, 512 * I:512 * (I + 1)], og[:, :], [og], [OUT])
                    deferred.append((si + 3, e2))
                    deferred.append((si + 6, e3))
                while deferred and deferred[0][0] <= si:
                    deferred.pop(0)[1]()
                cur = nxt
            while deferred:
                deferred.pop(0)[1]()
        kb.finish()
    return nc


def load_w_resident(kb, name, w_ap, ncols, wst):
    wb = kb.sb(name, [128, NKC, ncols], BF16)
    for p in range(0, ncols, 512):
        nco = min(512, ncols - p)
        ws = wst.next()
        kb.dma("sp", ws[:, :, 0:nco], w_ap[:, p:p + nco].rearrange("(kc p) n -> p kc n", p=128), [], [ws])
        kb.op("pool", [ws], [wb], lambda e, ws=ws, p=p, nco=nco: e.tensor_copy(
            wb[:, :, p:p + nco], ws[:, :, 0:nco]))
    return wb


def build_C1(T):
    nc = bass.Bass("TRN2", target_bir_lowering=False)
    NB = T // 512
    with ExitStack() as st:
        kb = KB(nc, st)
        dt = nc.dram_tensor
        oT = dt("oT", [D, T], BF16, kind="ExternalInput").ap()
        SaT = dt("SaT", [D, T], BF16, kind="ExternalInput").ap()
        YT = dt("YT", [D, T], BF16, kind="ExternalInput").ap()
        h = dt("h", [T, D], F32, kind="ExternalInput").ap()
        w_ap = dt("w_ap", [D, D], F32, kind="ExternalInput").ap()
        w_out = dt("w_out", [D, D], F32, kind="ExternalInput").ap()
        lng = dt("lng", [128, D], F32, kind="ExternalInput").ap()
        lnb = dt("lnb", [128, D], F32, kind="ExternalInput").ap()
        h1 = dt("h1", [T, D], F32, kind="ExternalOutput").ap()
        OUT = kb.out_trk
        g_rep = load_const(kb, "g_rep", lng, [128, D])
        b_rep = load_const(kb, "b_rep", lnb, [128, D])
        wst = Ring([kb.sb("wst%d" % i, [128, NKC, 512], F32) for i in range(2)])
        wap_b = load_w_resident(kb, "wap_b", w_ap, D, wst)
        wout_b = load_w_resident(kb, "wout_b", w_out, D, wst)
        psr = PsumRing(kb, 6)
        obr = Ring([kb.sb("ob%d" % i, [128, NKC, 512], BF16) for i in range(2)])
        sar = Ring([kb.sb("sa%d" % i, [128, NKC, 512], BF16) for i in range(2)])
        yr = Ring([kb.sb("yb%d" % i, [128, NKC, 512], BF16) for i in range(2)])
        mbr = Ring([kb.sb("mb%d" % i, [128, NKC, 512], BF16) for i in range(2)])
        tmr = Ring([kb.sb("tm%d" % i, [128, 512], F32) for i in range(3)])
        xr = Ring([kb.sb("x%d" % i, [128, D], F32) for i in range(3)])
        st6 = kb.sb("st6", [128, 12], F32)
        mv = kb.sb("mv", [128, 2], F32)
        rstd = kb.sb("rstd", [128, 1], F32)
        for n in range(NB):
            ob, sa, yb, mb = obr.next(), sar.next(), yr.next(), mbr.next()
            sl = slice(512 * n, 512 * (n + 1))
            kb.dma("sp", ob[:, :, :], oT[:, sl].rearrange("(c p) t -> p c t", p=128), [], [ob])
            kb.dma("sp", sa[:, :, :], SaT[:, sl].rearrange("(c p) t -> p c t", p=128), [], [sa])
            kb.dma("sp", yb[:, :, :], YT[:, sl].rearrange("(c p) t -> p c t", p=128), [], [yb])
            for j in range(8):
                ps = psr.next()
                mm_group(kb, ps, ps[:, :], [wap_b[:, kc, 128 * j:128 * (j + 1)] for kc in range(NKC)],
                         [ob[:, kc, :] for kc in range(NKC)], [wap_b, ob])
                tm = tmr.next()
                kb.op("dve", [ps, sa], [tm], lambda e, ps=ps, sa=sa, tm=tm, j=j: e.tensor_tensor(
                    tm[:, :], ps[:, :], sa[:, j, :], ALU.mult))
                kb.op("pool", [tm, yb], [mb], lambda e, tm=tm, yb=yb, mb=mb, j=j: e.tensor_tensor(
                    mb[:, j, :], tm[:, :], yb[:, j, :], ALU.add))
            for a in range(4):
                i = 4 * n + a
                x = xr.next()
                kb.dma("sp", x[:, :], h[i * 128:(i + 1) * 128, :], [], [x])
                for half in range(2):
                    ps = psr.next()
                    mm_group(kb, ps, ps[:, :], [mb[:, kc, 128 * a:128 * (a + 1)] for kc in range(NKC)],
                             [wout_b[:, kc, 512 * half:512 * (half + 1)] for kc in range(NKC)], [wout_b, mb])
                    kb.op("dve", [ps, x], [x], lambda e, ps=ps, x=x, half=half: e.scalar_tensor_tensor(
                        x[:, 512 * half:512 * (half + 1)], x[:, 512 * half:512 * (half + 1)], DN_ALPHA,
                        ps[:, :], ALU.mult, ALU.add))
                layer_norm_tile(kb, x, x, g_rep, b_rep, st6, mv, rstd)
                kb.dma("act", h1[i * 128:(i + 1) * 128, :], x[:, :], [x], [OUT])
        kb.finish()
    return nc


def build_C2(T):
    nc = bass.Bass("TRN2", target_bir_lowering=False)
    NT = T // 128
    NBLK = (2 * T + 32 * 127 + 127) // 128
    with ExitStack() as st:
        kb = KB(nc, st)
        dt = nc.dram_tensor
        h1 = dt("h1", [T, D], F32, kind="ExternalInput").ap()
        w_r = dt("w_r", [D, 36], F32, kind="ExternalInput").ap()
        b_r = dt("b_r", [128, 36], F32, kind="ExternalInput").ap()
        w1 = dt("w1", [32, D, 512], F32, kind="ExternalInput").ap()
        w3 = dt("w3", [32, D, 512], F32, kind="ExternalInput").ap()
        w2 = dt("w2", [32, 512, D], F32, kind="ExternalInput").ap()
        lng = dt("lng", [128, D], F32, kind="ExternalInput").ap()
        lnb = dt("lnb", [128, D], F32, kind="ExternalInput").ap()
        ident = dt("ident", [128, 128], F32, kind="ExternalInput").ap()
        utri = dt("utri", [128, 128], F32, kind="ExternalInput").ap()
        blk128 = dt("blk128", [128, NBLK], F32, kind="ExternalInput").ap()
        pidx_d = dt("pidx", [128, 1], F32, kind="ExternalInput").ap()
        h2 = dt("h2", [T, D], F32, kind="ExternalOutput").ap()
        xbuf = nc.dram_tensor("xbuf", [NBLK * 128, D], BF16).ap()
        ybuf = nc.dram_tensor("ybuf", [NBLK * 128, D], F32).ap()
        OUT = kb.out_trk

        g_rep = load_const(kb, "g_rep", lng, [128, D])
        b_rep = load_const(kb, "b_rep", lnb, [128, D])
        identf = load_const(kb, "identf", ident, [128, 128])
        identb = kb.sb("identb", [128, 128], BF16)
        kb.op("dve", [identf], [identb], lambda e: e.tensor_copy(identb[:, :], identf[:, :]))
        utf = load_const(kb, "utf", utri, [128, 128])
        utb = kb.sb("utb", [128, 128], BF16)
        kb.op("dve", [utf], [utb], lambda e: e.tensor_copy(utb[:, :], utf[:, :]))
        onesb = kb.sb("onesb", [128, 128], BF16)
        kb.op("dve", [], [onesb], lambda e: e.memset(onesb[:, :], 1.0))
        b128 = load_const(kb, "b128", blk128, [128, NBLK])
        brs = load_const(kb, "brs", b_r, [128, 36])
        wrf = kb.sb("wrf", [128, NKC, 36], F32)
        kb.dma("sp", wrf[:, :, :], w_r.rearrange("(kc p) n -> p kc n", p=128), [], [wrf])
        wrb = kb.sb("wrb", [128, NKC, 36], BF16)
        kb.op("dve", [wrf], [wrb], lambda e: e.tensor_copy(wrb[:, :, :], wrf[:, :, :]))

        psr = PsumRing(kb, 6)
        pT = kb.ps("pT", [128, NKC, 128], BF16)
        pT2 = kb.ps("pT2", [128, 4, 128], BF16)
        xr = Ring([kb.sb("x%d" % i, [128, D], F32) for i in range(3)])
        hbr = Ring([kb.sb("hb%d" % i, [128, D], BF16) for i in range(2)])
        hTr = Ring([kb.sb("hTt%d" % i, [128, NKC, 128], BF16) for i in range(2)])
        st6 = kb.sb("st6", [128, 12], F32)
        mv = kb.sb("mv", [128, 2], F32)
        rstd = kb.sb("rstd", [128, 1], F32)

        M1 = kb.sb("M1", [128, NT, 32], F32)
        M2 = kb.sb("M2", [128, NT, 32], F32)
        Mb = kb.sb("Mb", [128, NT, 32], BF16)
        rank = kb.sb("rank", [128, NT, 32], F32)
        gates = kb.sb("gates", [128, NT, 2], F32)
        destf = kb.sb("destf", [128, NT * 2], F32)
        desti = kb.sb("desti", [128, NT * 2], I32)
        lgr = Ring([kb.sb("lg%d" % i, [128, 36], F32) for i in range(2)])
        smr = Ring([kb.sb("smr%d" % i, [128, 48], F32) for i in range(2)])

        for i in range(NT):
            x = xr.next()
            hb = hbr.next()
            hTt = hTr.next()
            kb.dma("sp", x[:, :], h1[i * 128:(i + 1) * 128, :], [], [x])
            kb.op("act", [x], [hb], lambda e, x=x, hb=hb: e.copy(hb[:, :], x[:, :]))
            transpose_to(kb, hb, identb, pT, hTt[:, :, :], [hTt], NKC, eng="act")
            ps = psr.next()
            mm_group(kb, ps, ps[:, 0:36], [hTt[:, kc, :] for kc in range(NKC)],
                     [wrb[:, kc, :] for kc in range(NKC)], [hTt, wrb])
            lg = lgr.next()
            sm = smr.next()
            m1 = M1.trk(i)
            m2 = M2.trk(i)
            kb.op("dve", [ps, brs], [lg], lambda e, ps=ps, lg=lg: e.tensor_tensor(
                lg[:, :], ps[:, 0:36], brs[:, :], ALU.add))
            kb.op("dve", [lg], [sm], lambda e, lg=lg, sm=sm: e.reduce_max(sm[:, 0:1], lg[:, 0:4], axis=AX.X))
            kb.op("dve", [sm], [sm], lambda e, sm=sm: e.tensor_scalar(
                sm[:, 1:2], sm[:, 0:1], -1.0, None, ALU.mult))
            kb.op("act", [lg, sm], [sm], lambda e, lg=lg, sm=sm: e.activation(
                sm[:, 44:48], lg[:, 0:4], AF.Exp, bias=sm[:, 1:2], accum_out=sm[:, 2:3]))
            kb.op("dve", [sm], [sm], lambda e, sm=sm: e.reciprocal(sm[:, 3:4], sm[:, 2:3]))
            kb.op("dve", [lg, sm], [sm], lambda e, lg=lg, sm=sm: e.tensor_scalar(
                sm[:, 4:8], lg[:, 0:4], sm[:, 0:1], None, ALU.is_equal))
            kb.op("dve", [lg, sm], [sm], lambda e, lg=lg, sm=sm: e.tensor_scalar(
                sm[:, 8:16], lg[:, 4:12], sm[:, 4:5], None, ALU.mult))
            for g in range(1, 4):
                kb.op("dve", [lg, sm], [sm], lambda e, lg=lg, sm=sm, g=g: e.scalar_tensor_tensor(
                    sm[:, 8:16], lg[:, 4 + 8 * g:12 + 8 * g], sm[:, 4 + g:5 + g], sm[:, 8:16], ALU.mult, ALU.add))
            kb.op("dve", [sm], [sm], lambda e, sm=sm: e.max(sm[:, 16:24], sm[:, 8:16]))
            kb.op("dve", [sm], [sm], lambda e, sm=sm: e.tensor_scalar(
                sm[:, 24:32], sm[:, 8:16], sm[:, 16:17], None, ALU.is_equal))
            kb.op("dve", [sm], [sm], lambda e, sm=sm: e.tensor_scalar(
                sm[:, 32:40], sm[:, 8:16], sm[:, 17:18], None, ALU.is_equal))
            kb.op("dve", [sm], [sm], lambda e, sm=sm: e.tensor_tensor(
                sm[:, 40:41], sm[:, 17:18], sm[:, 16:17], ALU.subtract))
            kb.op("act", [sm], [sm], lambda e, sm=sm: e.activation(sm[:, 40:41], sm[:, 40:41], AF.Exp))
            kb.op("dve", [sm], [sm], lambda e, sm=sm: e.tensor_scalar(
                sm[:, 41:42], sm[:, 40:41], 1.0, None, ALU.add))
            kb.op("dve", [sm], [sm], lambda e, sm=sm: e.reciprocal(sm[:, 41:42], sm[:, 41:42]))
            kb.op("dve", [sm], [gates.trk(i)], lambda e, sm=sm, i=i: e.tensor_tensor(
                gates[:, i, 0:1], sm[:, 3:4], sm[:, 41:42], ALU.mult))
            kb.op("dve", [sm, gates.trk(i)], [gates.trk(i)], lambda e, sm=sm, i=i: e.tensor_tensor(
                gates[:, i, 1:2], gates[:, i, 0:1], sm[:, 40:41], ALU.mult))
            for g in range(4):
                kb.op("dve", [sm], [m1], lambda e, sm=sm, i=i, g=g: e.tensor_scalar(
                    M1[:, i, 8 * g:8 * g + 8], sm[:, 24:32], sm[:, 4 + g:5 + g], None, ALU.mult))
                kb.op("dve", [sm], [m2], lambda e, sm=sm, i=i, g=g: e.tensor_scalar(
                    M2[:, i, 8 * g:8 * g + 8], sm[:, 32:40], sm[:, 4 + g:5 + g], None, ALU.mult))
            kb.op("dve", [m1, m2], [Mb.trk(i)], lambda e, i=i: e.tensor_tensor(
                Mb[:, i, :], M1[:, i, :], M2[:, i, :], ALU.add))

        maccr = Ring([kb.sb("macc%d" % i, [128, 32], BF16) for i in range(2)])
        macc = maccr.next()
        kb.op("dve", [], [macc], lambda e, macc=macc: e.memset(macc[:, :], 0.0))
        for i in range(NT):
            ps = psr.next()

            def rk(e, ps=ps, i=i, macc=macc):
                e.matmul(ps[:, 0:32], utb[:, :], Mb[:, i, :], start=True, stop=False)
                return e.matmul(ps[:, 0:32], onesb[:, :], macc[:, :], start=False, stop=True)
            kb.op("pe", [utb, onesb, Mb.trk(i), macc], [ps], rk)
            kb.op("act", [ps], [rank.trk(i)], lambda e, ps=ps, i=i: e.copy(rank[:, i, :], ps[:, 0:32]))
            nm = maccr.next()
            kb.op("dve", [macc, Mb.trk(i)], [nm], lambda e, macc=macc, nm=nm, i=i: e.tensor_tensor(
                nm[:, :], macc[:, :], Mb[:, i, :], ALU.add))
            macc = nm
        ps = psr.next()
        kb.op("pe", [onesb, macc], [ps], lambda e, ps=ps, macc=macc: e.matmul(
            ps[:, 0:32], onesb[:, :], macc[:, :], start=True, stop=True))
        sc = kb.sb("sc", [128, 6, 32], F32)
        SC = [sc]
        kb.op("dve", [ps], SC, lambda e, ps=ps: e.tensor_copy(sc[:, 0, :], ps[:, 0:32]))
        sci = kb.sb("sci", [128, 2, 32], I32)
        kb.op("dve", SC, SC, lambda e: e.tensor_scalar(sc[:, 1, :], sc[:, 0, :], 127.0, None, ALU.add))
        kb.op("dve", SC, [sci], lambda e: e.tensor_copy(sci[:, 0, :], sc[:, 1, :]))
        kb.op("dve", [sci], [sci], lambda e: e.tensor_scalar(
            sci[:, 1, :], sci[:, 0, :], 7, 7, ALU.arith_shift_right, ALU.logical_shift_left))
        kb.op("dve", [sci], SC, lambda e: e.tensor_copy(sc[:, 2, :], sci[:, 1, :]))
        kb.op("dve", SC, SC, lambda e: e.tensor_copy(sc[:, 3, :], sc[:, 2, :]))
        a, b = 3, 4
        for sft in (1, 2, 4, 8, 16):
            kb.op("dve", SC, SC, lambda e, a=a, b=b: e.tensor_copy(sc[:, b, :], sc[:, a, :]))
            kb.op("dve", SC, SC, lambda e, a=a, b=b, sft=sft: e.tensor_tensor(
                sc[:, b, sft:32], sc[:, a, sft:32], sc[:, a, 0:32 - sft], ALU.add))
            a, b = b, a
        pe_idx = a
        kb.op("dve", SC, SC, lambda e: e.tensor_tensor(sc[:, 5, :], sc[:, pe_idx, :], sc[:, 2, :], ALU.subtract))
        tmpd = Ring([kb.sb("tmpd%d" % i, [128, 32], F32) for i in range(2)])
        junk32 = kb.sb("junk32", [128, 2, 32], F32)
        for i in range(NT):
            td = tmpd.next()
            kb.op("dve", SC + [rank.trk(i)], [td], lambda e, td=td, i=i: e.tensor_tensor(
                td[:, :], rank[:, i, :], sc[:, 5, :], ALU.add))
            for jj, (MM, mt) in enumerate(((M1, M1.trk(i)), (M2, M2.trk(i)))):
                td2 = tmpd.next() if False else None
                kb.op("dve", [td, mt], [destf], lambda e, td=td, MM=MM, i=i, jj=jj: e.tensor_tensor(
                    junk32[:, jj, :], td[:, :], MM[:, i, :], ALU.mult))
                kb.op("dve", [destf], [destf], lambda e, i=i, jj=jj: e.reduce_sum(
                    destf[:, 2 * i + jj:2 * i + jj + 1], junk32[:, jj, :], axis=AX.X))
        kb.op("dve", [destf], [desti], lambda e: e.tensor_copy(desti[:, :], destf[:, :]))
        blkf = kb.sb("blkf", [128, NBLK], F32)
        kb.op("dve", [], [blkf], lambda e: e.memset(blkf[:, :], 0.0))
        for ex in range(32):
            kb.op("dve", SC + [b128, blkf], [blkf], lambda e, ex=ex: e.scalar_tensor_tensor(
                blkf[:, :], b128[:, :], sc[:, pe_idx, ex:ex + 1], blkf[:, :], ALU.is_ge, ALU.add))
        kb.op("dve", [blkf], [blkf], lambda e: e.tensor_scalar(blkf[:, :], blkf[:, :], 31.0, None, ALU.min))

        pidx = load_const(kb, "pidx_s", pidx_d, [128, 1])
        widx = kb.sb("widx", [128, NBLK], I32)
        kb.op("dve", [blkf, pidx], [blkf], lambda e: e.tensor_scalar(
            blkf[:, :], blkf[:, :], 128.0, pidx[:, 0:1], ALU.mult, ALU.add))
        kb.op("dve", [blkf], [widx], lambda e: e.tensor_copy(widx[:, :], blkf[:, :]))
        w1rows = w1.rearrange("e (p kk) n -> (e p) (kk n)", kk=8)
        w3rows = w3.rearrange("e (p kk) n -> (e p) (kk n)", kk=8)
        w2rows = w2.rearrange("e (p kk) n -> (e p) (kk n)", kk=4)

        zt = kb.sb("zt", [128, D], BF16)
        kb.op("pool", [], [zt], lambda e: e.memset(zt[:, :], 0.0))
        xz = Trk()
        for bq in range(NBLK):
            kb.dma("sp", xbuf[bq * 128:(bq + 1) * 128, :], zt[:, :], [zt], [xz])
        xs = Trk()
        for i in range(NT):
            x = xr.next()
            hb = hbr.next()
            kb.dma("sp", x[:, :], h1[i * 128:(i + 1) * 128, :], [], [x])
            kb.op("act", [x], [hb], lambda e, x=x, hb=hb: e.copy(hb[:, :], x[:, :]))
            for jj in range(2):
                kb.dma_fn("pool", lambda e, hb=hb, i=i, jj=jj: e.indirect_dma_start(
                    out=xbuf, out_offset=bass.IndirectOffsetOnAxis(ap=desti[:, 2 * i + jj:2 * i + jj + 1], axis=0),
                    in_=hb[:, :], in_offset=None), [hb, desti, xz], [xs])

        w1s = Ring([kb.sb("w1s%d" % i, [128, NKC, 512], F32) for i in range(2)])
        w3s = Ring([kb.sb("w3s%d" % i, [128, NKC, 512], F32) for i in range(1)])
        w2s = Ring([kb.sb("w2s%d" % i, [128, 4, D], F32) for i in range(1)])
        w1b = Ring([kb.sb("w1b%d" % i, [128, NKC, 512], BF16) for i in range(2)])
        w3b = Ring([kb.sb("w3b%d" % i, [128, NKC, 512], BF16) for i in range(2)])
        w2b = Ring([kb.sb("w2b%d" % i, [128, 4, D], BF16) for i in range(2)])
        xbr = Ring([kb.sb("xb%d" % i, [128, D], BF16) for i in range(2)])
        xTr = Ring([kb.sb("xT%d" % i, [128, NKC, 128], BF16) for i in range(2)])
        sar = Ring([kb.sb("sA%d" % i, [128, 512], F32) for i in range(2)])
        gr = Ring([kb.sb("g%d" % i, [128, 512], BF16) for i in range(2)])
        gTr = Ring([kb.sb("gT%d" % i, [128, 4, 128], BF16) for i in range(2)])
        yr = Ring([kb.sb("y%d" % i, [128, D], F32) for i in range(2)])
        ys = Trk()
        for bq in range(NBLK):
            xb = xbr.next()
            kb.dma("sp", xb[:, :], xbuf[bq * 128:(bq + 1) * 128, :], [xs], [xb])
            ws1, ws3, ws2 = w1s.next(), w3s.next(), w2s.next()
            for wsx, wrows in ((ws1, w1rows), (ws3, w3rows), (ws2, w2rows)):
                kb.dma_fn("pool", lambda e, wsx=wsx, wrows=wrows, bq=bq: e.indirect_dma_start(
                    out=wsx[:, :, :].rearrange("p a b -> p (a b)"), out_offset=None, in_=wrows,
                    in_offset=bass.IndirectOffsetOnAxis(ap=widx[:, bq:bq + 1], axis=0)), [widx], [wsx])
            wb1, wb3, wb2 = w1b.next(), w3b.next(), w2b.next()
            kb.op("dve", [ws1], [wb1], lambda e, ws1=ws1, wb1=wb1: e.tensor_copy(wb1[:, :, :], ws1[:, :, :]))
            kb.op("dve", [ws3], [wb3], lambda e, ws3=ws3, wb3=wb3: e.tensor_copy(wb3[:, :, :], ws3[:, :, :]))
            kb.op("act", [ws2], [wb2], lambda e, ws2=ws2, wb2=wb2: e.copy(wb2[:, :, :], ws2[:, :, :]))
            xT = xTr.next()
            transpose_to(kb, xb, identb, pT, xT[:, :, :], [xT], NKC, eng="dve", strided=True)
            pa, pb = psr.next(), psr.next()
            mm_group(kb, pa, pa[:, :], [xT[:, kc, :] for kc in range(NKC)],
                     [wb1[:, kc, :] for kc in range(NKC)], [xT, wb1])
            mm_group(kb, pb, pb[:, :], [xT[:, kc, :] for kc in range(NKC)],
                     [wb3[:, kc, :] for kc in range(NKC)], [xT, wb3])
            sA = sar.next()
            gg = gr.next()
            kb.op("act", [pa], [sA], lambda e, pa=pa, sA=sA: e.activation(sA[:, :], pa[:, :], AF.Silu))
            kb.op("dve", [sA, pb], [gg], lambda e, sA=sA, pb=pb, gg=gg: e.tensor_tensor(
                gg[:, :], sA[:, :], pb[:, :], ALU.mult))
            gT = gTr.next()

            def trg(e, gg=gg):
                ins = None
                for c in range(4):
                    ins = e.transpose(pT2[:, c, :], gg[:, slice(c, None, 4)], identb[:, :])
                return ins
            kb.op("pe", [gg, identb], [pT2], trg)
            kb.op("dve", [pT2], [gT], lambda e, gT=gT: e.tensor_copy(gT[:, :, :], pT2[:, :, :]))
            y = yr.next()
            for half in range(2):
                pc = psr.next()
                mm_group(kb, pc, pc[:, :], [gT[:, kc, :] for kc in range(4)],
                         [wb2[:, kc, 512 * half:512 * (half + 1)] for kc in range(4)], [gT, wb2])
                if half == 0:
                    kb.op("act", [pc], [y], lambda e, pc=pc, y=y: e.copy(y[:, 0:512], pc[:, :]))
                else:
                    kb.op("dve", [pc], [y], lambda e, pc=pc, y=y: e.tensor_copy(y[:, 512:1024], pc[:, :]))
            kb.dma("act", ybuf[bq * 128:(bq + 1) * 128, :], y[:, :], [y], [ys])

        y1r = yr
        y2r = Ring([kb.sb("y2_%d" % i, [128, D], F32) for i in range(2)])
        for i in range(NT):
            x = xr.next()
            y1, y2 = y1r.next(), y2r.next()
            kb.dma("sp", x[:, :], h1[i * 128:(i + 1) * 128, :], [], [x])
            for jj, yy in enumerate((y1, y2)):
                kb.dma_fn("pool", lambda e, yy=yy, i=i, jj=jj: e.indirect_dma_start(
                    out=yy[:, :], out_offset=None, in_=ybuf,
                    in_offset=bass.IndirectOffsetOnAxis(ap=desti[:, 2 * i + jj:2 * i + jj + 1], axis=0)),
                    [desti, ys], [yy])
            gt = gates.trk(i)
            kb.op("dve", [y1, gt], [y1], lambda e, y1=y1, i=i: e.tensor_scalar(
                y1[:, :], y1[:, :], gates[:, i, 0:1], None, ALU.mult))
            kb.op("dve", [y1, y2, gt], [y1], lambda e, y1=y1, y2=y2, i=i: e.scalar_tensor_tensor(
                y1[:, :], y2[:, :], gates[:, i, 1:2], y1[:, :], ALU.mult, ALU.add))
            kb.op("dve", [x, y1], [x], lambda e, x=x, y1=y1: e.scalar_tensor_tensor(
                x[:, :], x[:, :], DN_ALPHA, y1[:, :], ALU.mult, ALU.add))
            layer_norm_tile(kb, x, x, g_rep, b_rep, st6, mv, rstd)
            kb.dma("act", h2[i * 128:(i + 1) * 128, :], x[:, :], [x], [OUT])
        kb.finish()
    return nc


def build_LN0(T):
    nc = bass.Bass("TRN2", target_bir_lowering=False)
    with ExitStack() as st:
        kb = KB(nc, st)
        dt = nc.dram_tensor
        x_d = dt("x", [T, D], F32, kind="ExternalInput").ap()
        lng = dt("lng", [128, D], F32, kind="ExternalInput").ap()
        lnb = dt("lnb", [128, D], F32, kind="ExternalInput").ap()
        h0 = dt("h0", [T, D], F32, kind="ExternalOutput").ap()
        g_rep = load_const(kb, "g_rep", lng, [128, D])
        b_rep = load_const(kb, "b_rep", lnb, [128, D])
        xr = Ring([kb.sb("x%d" % i, [128, D], F32) for i in range(4)])
        st6 = kb.sb("st6", [128, 12], F32)
        mv = kb.sb("mv", [128, 2], F32)
        rstd = kb.sb("rstd", [128, 1], F32)
        for i in range(T // 128):
            x = xr.next()
            kb.dma("sp", x[:, :], x_d[i * 128:(i + 1) * 128, :], [], [x])
            layer_norm_tile(kb, x, x, g_rep, b_rep, st6, mv, rstd)
            kb.dma("act", h0[i * 128:(i + 1) * 128, :], x[:, :], [x], [kb.out_trk])
        kb.finish()
    return nc


_PROGS = {}


def _prog(name, fn, *args):
    key = (name,) + args
    if key not in _PROGS:
        _PROGS[key] = fn(*args)
    return _PROGS[key]


def _lay(a):
    J = a.shape[0]
    return np.ascontiguousarray(a.reshape(J, 8, 128).transpose(2, 0, 1).reshape(128, J * 8))


def _rep(a):
    return np.ascontiguousarray(np.broadcast_to(np.asarray(a, np.float32).reshape(1, -1), (128, a.size)))


def kernel(x, ln0_g, ln0_b, rel_table, w_in, gate_b, lam_vecs, subln_g, w_attn_proj, conv_w,
           w_conv_proj, w_out, ln1_g, ln1_b, w_rg, b_rg, w_re, b_re, w1, w3, w2, ln2_g, ln2_b):
    f32 = np.float32
    x = np.asarray(x, f32)
    B, S, _ = x.shape
    NC = 8
    T = B * S // NC
    CPS = S // T
    cores = list(range(NC))
    depth = w_in.shape[0]
    ident = np.eye(128, dtype=f32)
    xt = x.reshape(B * S, D)

    def run(nc, maps):
        return run_bass_kernel_spmd(nc, maps, core_ids=cores).results

    res = run(_prog("LN0", build_LN0, T),
              [{"x": xt[c * T:(c + 1) * T], "lng": _rep(ln0_g), "lnb": _rep(ln0_b)} for c in cores])
    h = [r["h0"] for r in res]

    oh = bias_onehot()
    NBLK = (2 * T + 32 * 127 + 127) // 128
    utri = np.triu(np.ones((128, 128), f32), 1)
    blk128 = np.ascontiguousarray(np.broadcast_to(np.arange(NBLK, dtype=f32) * 128, (128, NBLK)))
    pidx = np.arange(128, dtype=f32).reshape(128, 1)

    for l in range(depth):
        lam_init = 0.8 - 0.6 * math.exp(-0.3 * l)
        maps = []
        for c in cores:
            first = (c % CPS == 0)
            halo = np.zeros((128, D), f32) if first else h[c - 1][T - 128:]
            maps.append({"hin": np.concatenate([halo, h[c]], 0),
                         "halo_mask": np.full((128, 1), 0.0 if first else 1.0, f32),
                         "w_in": np.asarray(w_in[l], f32), "gate_b": _lay(np.asarray(gate_b[l], f32)),
                         "conv_w": _lay(np.asarray(conv_w[l], f32)),
                         "w_cp": np.asarray(w_conv_proj[l], f32), "ident": ident})
        ra = run(_prog("A", build_A, T, False), maps)
        maps = []
        for c in cores:
            b, hp = c // CPS, c % CPS
            src = [ra[b * CPS + t] for t in range(CPS)]
            rows = slice(hp * 256, (hp + 1) * 256)
            tab = np.concatenate([np.asarray(rel_table, f32)[:, 2 * hp:2 * hp + 2],
                                  np.full((1, 2), -30000.0, f32)], 0)
            maps.append({"qT": np.concatenate([r["qT"][rows] for r in src], 1),
                         "kT": np.concatenate([r["kT"][rows] for r in src], 1),
                         "v": np.concatenate([r["v"][:, rows] for r in src], 0),
                         "oh": oh, "tab": tab,
                         "lamv": _rep(np.asarray(lam_vecs[l], f32).reshape(-1)),
                         "subg": _rep(subln_g[l]),
                         "lamc": np.ascontiguousarray(np.broadcast_to(
                             np.array([[-lam_init, 1.0 - lam_init]], f32), (128, 2))),
                         "ident": ident})
        rt = run(_prog("ATT", build_ATT, S), maps)
        maps = []
        for c in cores:
            b, t = c // CPS, c % CPS
            oT = np.concatenate([rt[b * CPS + hp]["oT"][:, t * T:(t + 1) * T] for hp in range(CPS)], 0)
            maps.append({"oT": oT, "SaT": ra[c]["SaT"], "YT": ra[c]["YT"], "h": h[c],
                         "w_ap": np.asarray(w_attn_proj[l], f32), "w_out": np.asarray(w_out[l], f32),
                         "lng": _rep(ln1_g[l]), "lnb": _rep(ln1_b[l])})
        r1 = run(_prog("C1", build_C1, T), maps)
        w_r = np.concatenate([np.asarray(w_rg[l], f32), np.asarray(w_re[l], f32)], 1)
        b_r = _rep(np.concatenate([np.asarray(b_rg[l], f32), np.asarray(b_re[l], f32)]))
        w1l, w3l, w2l = np.asarray(w1[l], f32), np.asarray(w3[l], f32), np.asarray(w2[l], f32)
        maps = [{"h1": r1[c]["h1"], "w_r": w_r, "b_r": b_r, "w1": w1l, "w3": w3l, "w2": w2l,
                 "lng": _rep(ln2_g[l]), "lnb": _rep(ln2_b[l]), "ident": ident, "utri": utri,
                 "blk128": blk128, "pidx": pidx} for c in cores]
        r2 = run(_prog("C2", build_C2, T), maps)
        h = [r["h2"] for r in r2]
    return np.concatenate(h, 0).reshape(B, S, D).astype(f32)
```

```python
import math
from contextlib import ExitStack
import numpy as np
import concourse.bass as bass
import concourse.mybir as mybir
from concourse.bass_utils import run_bass_kernel_spmd

F32 = mybir.dt.float32
BF16 = mybir.dt.bfloat16
I32 = mybir.dt.int32
AF = mybir.ActivationFunctionType
ALU = mybir.AluOpType
AX = mybir.AxisListType


class Trk:
    __slots__ = ("w", "r")

    def __init__(self):
        self.w = None
        self.r = {}


class Buf(Trk):
    __slots__ = ("t", "subs")

    def __init__(self, t):
        Trk.__init__(self)
        self.t = t
        self.subs = {}

    def __getitem__(self, idx):
        return self.t[idx]

    def trk(self, key):
        s = self.subs.get(key)
        if s is None:
            s = self.subs[key] = Trk()
        return s


class KB:
    NDMA = 32

    def __init__(self, nc, st):
        self.nc = nc
        self.st = st
        self.eng = {"pe": nc.tensor, "act": nc.scalar, "dve": nc.vector,
                    "pool": nc.gpsimd, "sp": nc.sync}
        self.sem = {}
        self.cnt = {}
        self.known = {k: {} for k in self.eng}
        for k in ("pe", "act", "dve", "pool"):
            self.sem[k] = st.enter_context(nc.semaphore("s_" + k))
            self.cnt[k] = 0
        self.dsem = []
        for i in range(self.NDMA):
            key = "d%d" % i
            self.sem[key] = st.enter_context(nc.semaphore("s_" + key))
            self.cnt[key] = 0
            self.dsem.append(key)
        self.dnext = 0
        self._clear_sems()
        self.out_trk = Trk()
        self.prog = {k: [] for k in self.eng}
        self.nbuf = 0

    def _clear_sems(self):
        for h in self.sem.values():
            self.nc.gpsimd.sem_clear(h)
        self.nc.all_engine_barrier()

    def sb(self, name, shape, dtype):
        t = self.st.enter_context(self.nc.sbuf_tensor(name, list(shape), dtype))
        return Buf(t)

    def ps(self, name, shape, dtype):
        t = self.st.enter_context(self.nc.psum_tensor(name, list(shape), dtype))
        return Buf(t)

    def _wait(self, ename, deps):
        kn = self.known[ename]
        best = {}
        for d in deps:
            if d is None:
                continue
            k, v = d
            if best.get(k, 0) < v:
                best[k] = v
        waits = []
        for k, v in best.items():
            if k == "pe" and ename == "pe":
                continue
            if kn.get(k, 0) >= v:
                continue
            waits.append((k, v))
            kn[k] = v
        return waits

    def _deps(self, reads, writes):
        deps = []
        for b in reads:
            deps.append(b.w)
        for b in writes:
            deps.append(b.w)
            for k, v in b.r.items():
                deps.append((k, v))
        return deps

    def _commit(self, tok, reads, writes):
        k, v = tok
        for b in reads:
            if b.r.get(k, 0) < v:
                b.r[k] = v
        for b in writes:
            b.w = tok
            b.r = {}

    def op(self, ename, reads, writes, fn):
        waits = self._wait(ename, self._deps(reads, writes))
        self.cnt[ename] += 1
        self.prog[ename].append((waits, fn, ename, 1))
        tok = (ename, self.cnt[ename])
        self._commit(tok, reads, writes)
        return tok

    def dma_fn(self, qname, fn, reads, writes):
        key = self.dsem[self.dnext]
        self.dnext = (self.dnext + 1) % self.NDMA
        deps = self._deps(reads, writes)
        if self.cnt[key] > 0:
            deps.append((key, self.cnt[key]))
        waits = self._wait(qname, deps)
        self.cnt[key] += 16
        self.prog[qname].append((waits, fn, key, 16))
        tok = (key, self.cnt[key])
        self._commit(tok, reads, writes)
        return tok

    def dma(self, qname, out, in_, reads, writes, **kw):
        return self.dma_fn(qname, lambda e: e.dma_start(out=out, in_=in_, **kw), reads, writes)

    def finish(self):
        deps = [self.out_trk.w]
        for key in self.dsem:
            if self.cnt[key] > 0:
                deps.append((key, self.cnt[key]))
        for k in ("pe", "act", "dve", "pool"):
            if self.cnt[k] > 0:
                deps.append((k, self.cnt[k]))
        final_waits = self._wait("sp", deps)
        sem = self.sem
        prog = self.prog

        def emit(e, items, tail=()):
            for waits, fn, ik, iv in items:
                for k, v in waits:
                    e.wait_ge(sem[k], v)
                fn(e).then_inc(sem[ik], iv)
            for k, v in tail:
                e.wait_ge(sem[k], v)

        with self.nc.Block() as block:
            @block.tensor
            def _(e):
                emit(e, prog["pe"])

            @block.scalar
            def _(e):
                emit(e, prog["act"])

            @block.vector
            def _(e):
                emit(e, prog["dve"])

            @block.gpsimd
            def _(e):
                emit(e, prog["pool"])

            @block.sync
            def _(e):
                emit(e, prog["sp"], final_waits)

        self._clear_sems()


D = 1024
NKC = 8
LN_EPS = 1e-5
DN_ALPHA = 4 ** 0.25
MOE_BS = 256
MOE_SH = 8


class PsumRing:
    def __init__(self, kb, n, name="ps"):
        self.bufs = [kb.ps("%s%d" % (name, i), [128, 512], F32) for i in range(n)]
        self.i = 0

    def next(self):
        b = self.bufs[self.i]
        self.i = (self.i + 1) % len(self.bufs)
        return b


class Ring:
    def __init__(self, bufs):
        self.bufs = bufs
        self.i = 0

    def next(self):
        b = self.bufs[self.i]
        self.i = (self.i + 1) % len(self.bufs)
        return b


def mm_group(kb, ps, out_ap, lhs, rhs, reads):
    n = len(lhs)

    def fn(e):
        ins = None
        for k in range(n):
            ins = e.matmul(out_ap, lhs[k], rhs[k], start=(k == 0), stop=(k == n - 1))
        return ins
    return kb.op("pe", reads, [ps], fn)


def layer_norm_tile(kb, x, xo, g_rep, b_rep, st6, mv, rstd):
    def stats(e):
        e.bn_stats(st6[:, 0:6], x[:, 0:512])
        return e.bn_stats(st6[:, 6:12], x[:, 512:1024])
    kb.op("dve", [x], [st6], stats)
    kb.op("dve", [st6], [mv], lambda e: e.bn_aggr(mv[:, :], st6[:, :]))
    kb.op("dve", [mv], [rstd], lambda e: e.tensor_scalar(
        rstd[:, :], mv[:, 1:2], LN_EPS, None, ALU.add))
    kb.op("act", [rstd], [rstd], lambda e: e.activation(rstd[:, :], rstd[:, :], AF.Sqrt))
    kb.op("dve", [rstd], [rstd], lambda e: e.reciprocal(rstd[:, :], rstd[:, :]))
    kb.op("dve", [x, mv, rstd], [xo], lambda e: e.tensor_scalar(
        xo[:, :], x[:, :], mv[:, 0:1], rstd[:, 0:1], ALU.subtract, ALU.mult))
    kb.op("dve", [xo, g_rep], [xo], lambda e: e.tensor_tensor(
        xo[:, :], xo[:, :], g_rep[:, :], ALU.mult))
    kb.op("dve", [xo, b_rep], [xo], lambda e: e.tensor_tensor(
        xo[:, :], xo[:, :], b_rep[:, :], ALU.add))


def load_const(kb, name, dram_ap, shape, dtype=F32, q="sp"):
    b = kb.sb(name, shape, dtype)
    kb.dma(q, b[tuple(slice(None) for _ in shape)], dram_ap, [], [b])
    return b


def transpose_to(kb, src, identb, pT, dst_ap, dst_trks, nch, eng="act", strided=False):
    def tr(e):
        ins = None
        for c in range(nch):
            sl = slice(c, None, nch) if strided else slice(c * 128, (c + 1) * 128)
            ins = e.transpose(pT[:, c, :], src[:, sl], identb[:, :])
        return ins
    kb.op("pe", [src, identb], [pT], tr)
    if eng == "act":
        kb.op("act", [pT], dst_trks, lambda e: e.copy(dst_ap, pT[:, 0:nch, :]))
    else:
        kb.op("dve", [pT], dst_trks, lambda e: e.tensor_copy(dst_ap, pT[:, 0:nch, :]))


def build_A(T, layer0):
    nc = bass.Bass("TRN2", target_bir_lowering=False)
    TH = T + 128
    NB = T // 512
    NT = T // 128
    with ExitStack() as st:
        kb = KB(nc, st)
        dt = nc.dram_tensor
        hin = dt("hin", [TH, D], F32, kind="ExternalInput").ap()
        halo_mask = dt("halo_mask", [128, 1], F32, kind="ExternalInput").ap()
        w_in = dt("w_in", [D, 8192], F32, kind="ExternalInput").ap()
        gate_b = dt("gate_b", [128, 16], F32, kind="ExternalInput").ap()
        conv_w = dt("conv_w", [128, 24], F32, kind="ExternalInput").ap()
        w_cp = dt("w_cp", [D, D], F32, kind="ExternalInput").ap()
        ident = dt("ident", [128, 128], F32, kind="ExternalInput").ap()
        if layer0:
            lng = dt("lng", [128, D], F32, kind="ExternalInput").ap()
            lnb = dt("lnb", [128, D], F32, kind="ExternalInput").ap()
            h0 = dt("h0", [T, D], F32, kind="ExternalOutput").ap()
        qT = dt("qT", [D, T], BF16, kind="ExternalOutput").ap()
        kT = dt("kT", [D, T], BF16, kind="ExternalOutput").ap()
        v = dt("v", [T, D], BF16, kind="ExternalOutput").ap()
        SaT = dt("SaT", [D, T], BF16, kind="ExternalOutput").ap()
        YT = dt("YT", [D, T], BF16, kind="ExternalOutput").ap()
        OUT = kb.out_trk

        identf = load_const(kb, "identf", ident, [128, 128])
        identb = kb.sb("identb", [128, 128], BF16)
        kb.op("dve", [identf], [identb], lambda e: e.tensor_copy(identb[:, :], identf[:, :]))
        gb = load_const(kb, "gb", gate_b, [128, 16])
        cw = load_const(kb, "cw", conv_w, [128, 24])
        hm = load_const(kb, "hm", halo_mask, [128, 1])
        if layer0:
            g_rep = load_const(kb, "g_rep", lng, [128, D])
            b_rep = load_const(kb, "b_rep", lnb, [128, D])

        hT = kb.sb("hT", [128, NKC, TH], BF16)
        pT = kb.ps("pT", [128, NKC, 128], BF16)
        psr = PsumRing(kb, 6)

        xr = Ring([kb.sb("x%d" % i, [128, D], F32) for i in range(3)])
        hbr = Ring([kb.sb("hb%d" % i, [128, D], BF16) for i in range(2)])
        st6 = kb.sb("st6", [128, 12], F32)
        mv = kb.sb("mv", [128, 2], F32)
        rstd = kb.sb("rstd", [128, 1], F32)
        for i in range(TH // 128):
            x = xr.next()
            hb = hbr.next()
            kb.dma("sp", x[:, :], hin[i * 128:(i + 1) * 128, :], [], [x])
            if layer0:
                layer_norm_tile(kb, x, x, g_rep, b_rep, st6, mv, rstd)
                if i >= 1:
                    kb.dma("pool", h0[(i - 1) * 128:i * 128, :], x[:, :], [x], [OUT])
            kb.op("act", [x], [hb], lambda e, x=x, hb=hb: e.copy(hb[:, :], x[:, :]))
            transpose_to(kb, hb, identb, pT, hT[:, :, i * 128:(i + 1) * 128], [hT.trk(i)], NKC,
                         eng="dve" if i % 2 else "act")

        def hT_blk(n):
            return [hT.trk(1 + 4 * n + a) for a in range(4)]

        wst = Ring([kb.sb("wst%d" % i, [128, NKC, 512], F32) for i in range(1)])
        wbf = Ring([kb.sb("wbf%d" % i, [128, NKC, 512], BF16) for i in range(2)])

        def load_w(srcs):
            ws = wst.next()
            wb = wbf.next()
            c0 = 0
            for ap in srcs:
                nco = ap.shape[1]
                kb.dma("sp", ws[:, :, c0:c0 + nco], ap.rearrange("(kc p) n -> p kc n", p=128),
                       [], [ws])
                c0 += nco
            kb.op("pool", [ws], [wb], lambda e, ws=ws, wb=wb, c0=c0: e.tensor_copy(
                wb[:, :, 0:c0], ws[:, :, 0:c0]))
            return wb

        evi = [0]

        def evac(ps, out_ap, out_trks, src_ap, extra_reads=()):
            evi[0] += 1
            if evi[0] % 2:
                kb.op("act", [ps] + list(extra_reads), out_trks, lambda e: e.copy(out_ap, src_ap))
            else:
                kb.op("dve", [ps] + list(extra_reads), out_trks, lambda e: e.tensor_copy(out_ap, src_ap))

        ostage = Ring([kb.sb("ost%d" % i, [128, T], BF16) for i in range(2)])

        for p in range(4):
            wb = load_w([w_in[:, p * 512:(p + 1) * 512]])
            for cc in range(4):
                ch = p * 4 + cc
                og = ostage.next()
                for n in range(NB):
                    ps = psr.next()
                    mm_group(kb, ps, ps[:, :],
                             [wb[:, kc, cc * 128:(cc + 1) * 128] for kc in range(NKC)],
                             [hT[:, kc, 128 + 512 * n:128 + 512 * (n + 1)] for kc in range(NKC)],
                             [wb] + hT_blk(n))
                    evac(ps, og[:, 512 * n:512 * (n + 1)], [og], ps[:, :])
                dst = qT if ch < 8 else kT
                r0 = (ch % 8) * 128
                kb.dma("pool", dst[r0:r0 + 128, :], og[:, :], [og], [OUT])

        vstage = Ring([kb.sb("vst%d" % i, [128, 512], BF16) for i in range(3)])
        for p in range(2):
            wb = load_w([w_in[:, 2048 + p * 512:2048 + (p + 1) * 512]])
            for i in range(NT):
                ps = psr.next()
                mm_group(kb, ps, ps[:, :],
                         [hT[:, kc, 128 + 128 * i:128 + 128 * (i + 1)] for kc in range(NKC)],
                         [wb[:, kc, :] for kc in range(NKC)],
                         [wb, hT.trk(1 + i)])
                vs = vstage.next()
                evac(ps, vs[:, :], [vs], ps[:, :])
                kb.dma("pool", v[i * 128:(i + 1) * 128, p * 512:(p + 1) * 512], vs[:, :], [vs], [OUT])

        NH = 2 if T >= 1024 else 1
        TH2 = T // NH
        NB2 = TH2 // 512
        gT = kb.sb("gT", [128, NKC, TH2], BF16)
        uT = kb.sb("uT", [128, TH2 + 2], F32)
        cbT = kb.sb("cbT", [128, TH2], F32)
        yacc = kb.sb("yacc", [128, TH2], F32)
        tmpr = Ring([kb.sb("tmp%d" % i, [128, 512], F32) for i in range(2)])
        for hf in range(NH):
            t0 = hf * TH2
            for j in range(8):
                wb = load_w([w_in[:, 3072 + 128 * j:3072 + 128 * (j + 1)],
                             w_in[:, 4096 + 128 * j:4096 + 128 * (j + 1)],
                             w_in[:, 5120 + 128 * j:5120 + 128 * (j + 1)]])
                ps = psr.next()
                hc = 128 + t0
                htr = [hT.trk(0)] if hf == 0 else [hT.trk((hc - 2) // 128)]

                def halo_mm(e, ps=ps, wb=wb, hc=hc):
                    ins = None
                    for g in range(2):
                        for kc in range(NKC):
                            ins = e.matmul(ps[:, 2 * g:2 * g + 2], wb[:, kc, 128 * (g + 1):128 * (g + 2)],
                                           hT[:, kc, hc - 2:hc], start=(kc == 0), stop=(kc == NKC - 1))
                    return ins
                kb.op("pe", [wb] + htr, [ps], halo_mm)
                tm = tmpr.next()
                kb.op("act", [ps], [tm], lambda e, ps=ps, tm=tm: e.copy(tm[:, 0:2], ps[:, 0:2]))
                kb.op("dve", [ps, tm], [tm], lambda e, ps=ps, tm=tm: e.tensor_tensor(
                    tm[:, 2:4], tm[:, 0:2], ps[:, 2:4], ALU.mult))
                if hf == 0:
                    kb.op("dve", [tm, hm], [uT], lambda e, tm=tm: e.tensor_scalar(
                        uT[:, 0:2], tm[:, 2:4], hm[:, 0:1], None, ALU.mult))
                else:
                    kb.op("dve", [tm], [uT], lambda e, tm=tm: e.tensor_copy(uT[:, 0:2], tm[:, 2:4]))
                for n2 in range(NB2):
                    n = hf * NB2 + n2
                    rhs = [hT[:, kc, 128 + 512 * n:128 + 512 * (n + 1)] for kc in range(NKC)]
                    pcb, pcc, pcx = psr.next(), psr.next(), psr.next()
                    for g, ps in enumerate((pcb, pcc, pcx)):
                        mm_group(kb, ps, ps[:, :], [wb[:, kc, 128 * g:128 * (g + 1)] for kc in range(NKC)],
                                 rhs, [wb] + hT_blk(n))
                    tm = tmpr.next()
                    kb.op("act", [pcc], [tm], lambda e, pcc=pcc, tm=tm: e.copy(tm[:, :], pcc[:, :]))
                    kb.op("dve", [tm, pcx], [uT], lambda e, tm=tm, pcx=pcx, n2=n2: e.tensor_tensor(
                        uT[:, 2 + 512 * n2:2 + 512 * (n2 + 1)], tm[:, :], pcx[:, :], ALU.mult))
                    kb.op("act", [pcb], [cbT], lambda e, pcb=pcb, n2=n2: e.copy(
                        cbT[:, 512 * n2:512 * (n2 + 1)], pcb[:, :]))
                kb.op("dve", [uT, cw], [yacc], lambda e, j=j: e.tensor_scalar(
                    yacc[:, :], uT[:, 2:TH2 + 2], cw[:, 16 + j:17 + j], None, ALU.mult))
                kb.op("dve", [uT, cw, yacc], [yacc], lambda e, j=j: e.scalar_tensor_tensor(
                    yacc[:, :], uT[:, 1:TH2 + 1], cw[:, 8 + j:9 + j], yacc[:, :], ALU.mult, ALU.add))
                kb.op("dve", [uT, cw, yacc], [yacc], lambda e, j=j: e.scalar_tensor_tensor(
                    yacc[:, :], uT[:, 0:TH2], cw[:, j:j + 1], yacc[:, :], ALU.mult, ALU.add))
                kb.op("dve", [yacc, cbT], [gT.trk(j)], lambda e, j=j: e.tensor_tensor(
                    gT[:, j, :], yacc[:, :], cbT[:, :], ALU.mult))

            gT_all = [gT.trk(j) for j in range(8)]
            for j in range(8):
                wb = load_w([w_in[:, 7168 + 128 * j:7168 + 128 * (j + 1)], w_cp[:, 128 * j:128 * (j + 1)]])
                og = ostage.next()
                for n2 in range(NB2):
                    n = hf * NB2 + n2
                    pyc, pgc = psr.next(), psr.next()
                    mm_group(kb, pyc, pyc[:, :], [wb[:, kc, 128:256] for kc in range(NKC)],
                             [gT[:, kc, 512 * n2:512 * (n2 + 1)] for kc in range(NKC)], [wb] + gT_all)
                    mm_group(kb, pgc, pgc[:, :], [wb[:, kc, 0:128] for kc in range(NKC)],
                             [hT[:, kc, 128 + 512 * n:128 + 512 * (n + 1)] for kc in range(NKC)],
                             [wb] + hT_blk(n))
                    tm = tmpr.next()
                    kb.op("act", [pgc, gb], [tm], lambda e, pgc=pgc, tm=tm, j=j: e.activation(
                        tm[:, :], pgc[:, :], AF.Sigmoid, bias=gb[:, 8 + j:9 + j]))
                    kb.op("dve", [tm, pyc], [og], lambda e, tm=tm, pyc=pyc, og=og, n2=n2: e.tensor_tensor(
                        og[:, 512 * n2:512 * (n2 + 1)], tm[:, :], pyc[:, :], ALU.mult))
                kb.dma("pool", YT[128 * j:128 * (j + 1), t0:t0 + TH2], og[:, 0:TH2], [og], [OUT])

        for p in range(2):
            wb = load_w([w_in[:, 6144 + p * 512:6144 + (p + 1) * 512]])
            for cc in range(4):
                j = p * 4 + cc
                og = ostage.next()
                for n in range(NB):
                    ps = psr.next()
                    mm_group(kb, ps, ps[:, :],
                             [wb[:, kc, cc * 128:(cc + 1) * 128] for kc in range(NKC)],
                             [hT[:, kc, 128 + 512 * n:128 + 512 * (n + 1)] for kc in range(NKC)],
                             [wb] + hT_blk(n))
                    kb.op("act", [ps, gb], [og], lambda e, ps=ps, og=og, n=n, j=j: e.activation(
                        og[:, 512 * n:512 * (n + 1)], ps[:, :], AF.Sigmoid, bias=gb[:, j:j + 1]))
                kb.dma("pool", SaT[128 * j:128 * (j + 1), :], og[:, :], [og], [OUT])
        kb.finish()
    return nc


FL = 1151
FP = FL + 1


def rel_bucket_np(rel):
    n = np.maximum(rel, 0)
    nf = np.maximum(n, 1).astype(np.float32)
    large = 16 + (np.log(nf / np.float32(16)) / np.float32(math.log(128 / 16)) * np.float32(16)).astype(np.int32)
    large = np.minimum(large, 31)
    return np.where(n < 16, n, large)


def bias_onehot():
    rel = np.arange(FL) - 511
    b = rel_bucket_np(rel)
    oh = np.zeros((33, FL), np.float32)
    for i in range(FL):
        if rel[i] < 0:
            oh[32, i] = 1.0
        else:
            oh[b[i], i] = 1.0
    return oh


def build_ATT(S):
    nc = bass.Bass("TRN2", target_bir_lowering=False)
    NQB = S // 512
    NKCH = S // 128
    with ExitStack() as st:
        kb = KB(nc, st)
        dt = nc.dram_tensor
        qT = dt("qT", [256, S], BF16, kind="ExternalInput").ap()
        kT = dt("kT", [256, S], BF16, kind="ExternalInput").ap()
        v = dt("v", [S, 256], BF16, kind="ExternalInput").ap()
        oh = dt("oh", [33, FL], F32, kind="ExternalInput").ap()
        tab = dt("tab", [33, 2], F32, kind="ExternalInput").ap()
        lamv = dt("lamv", [128, 256], F32, kind="ExternalInput").ap()
        subg = dt("subg", [128, 128], F32, kind="ExternalInput").ap()
        lamc_d = dt("lamc", [128, 2], F32, kind="ExternalInput").ap()
        ident = dt("ident", [128, 128], F32, kind="ExternalInput").ap()
        oT = dt("oT", [256, S], BF16, kind="ExternalOutput").ap()
        fscr = [nc.dram_tensor("fscr%d" % h, [128 * FP], F32) for h in range(2)]
        OUT = kb.out_trk

        identf = load_const(kb, "identf", ident, [128, 128])
        identb = kb.sb("identb", [128, 128], BF16)
        kb.op("dve", [identf], [identb], lambda e: e.tensor_copy(identb[:, :], identf[:, :]))
        ohs = load_const(kb, "ohs", oh, [33, FL])
        tabs = load_const(kb, "tabs", tab, [33, 2])
        lv = load_const(kb, "lv", lamv, [128, 256])
        g_rep = load_const(kb, "g_rep", subg, [128, 128])
        lamc = load_const(kb, "lamc_s", lamc_d, [128, 2])
        kb.op("dve", [g_rep, lamc], [g_rep], lambda e: e.tensor_scalar(
            g_rep[:, :], g_rep[:, :], lamc[:, 1:2], None, ALU.mult))
        epst = kb.sb("epst", [128, 1], F32)
        kb.op("dve", [], [epst], lambda e: e.memset(epst[:, :], LN_EPS))

        lt = kb.sb("lt", [128, 128], F32)
        ls = kb.sb("ls", [128, 2], F32)
        nlam = kb.sb("nlam", [128, 1], F32)
        kb.op("dve", [lv], [lt], lambda e: e.tensor_tensor(
            lt[:, 0:64], lv[:, 0:64], lv[:, 64:128], ALU.mult))
        kb.op("dve", [lv, lt], [lt], lambda e: e.tensor_tensor(
            lt[:, 64:128], lv[:, 128:192], lv[:, 192:256], ALU.mult))
        kb.op("dve", [lt], [ls], lambda e: e.reduce_sum(
            ls[:, 0:2], lt[:, :].rearrange("p (a b) -> p a b", a=2), axis=AX.X))
        kb.op("act", [ls], [ls], lambda e: e.activation(ls[:, :], ls[:, :], AF.Exp))
        kb.op("dve", [ls], [nlam], lambda e: e.tensor_tensor(
            nlam[:, :], ls[:, 1:2], ls[:, 0:1], ALU.subtract))
        kb.op("dve", [nlam, lamc], [nlam], lambda e: e.tensor_scalar(
            nlam[:, :], nlam[:, :], lamc[:, 0:1], None, ALU.add))

        s12 = Ring([kb.ps("s12_%d" % i, [128, 1024], F32) for i in range(2)])
        oacc = kb.ps("oacc", [128, 3, 512], F32)
        pT = kb.ps("pT", [128, 4, 128], BF16)

        def acc_ap(h2, qc):
            g = h2 * 4 + qc
            return oacc[:, g // 3, (g % 3) * 129:(g % 3) * 129 + 129]

        kTs = kb.sb("kTs", [128, S], BF16)
        vaug = kb.sb("vaug", [128, NKCH, 129], BF16)
        tabrep = kb.sb("tabrep", [33, 128], F32)
        frep = kb.sb("frep", [128, FL], F32)
        c31 = kb.sb("c31", [128, 1], F32)
        biasT = [kb.sb("biasT%d" % d, [128, 512], F32) for d in range(5)]
        qr = Ring([kb.sb("qb%d" % i, [128, 512], BF16) for i in range(3)])
        pr = Ring([kb.sb("pb%d" % i, [128, 1024], BF16) for i in range(3)])
        tmpr = Ring([kb.sb("tm%d" % i, [128, 1024], F32) for i in range(2)])
        osb = Ring([kb.sb("osb%d" % i, [128, 8, 132], F32) for i in range(2)])
        ostage = Ring([kb.sb("ostg%d" % i, [128, 512], BF16) for i in range(2)])
        ofr = Ring([kb.sb("of%d" % i, [128, 4, 128], F32) for i in range(3)])
        onr = Ring([kb.sb("on%d" % i, [128, 4, 128], BF16) for i in range(2)])
        smr = Ring([kb.sb("sm%d" % i, [128, 16], F32) for i in range(3)])
        junk = kb.sb("junk", [128, 128], F32)

        for hh in range(2):
            kb.dma("sp", kTs[:, :], kT[hh * 128:(hh + 1) * 128, :], [], [kTs])
            kb.dma("sp", vaug[:, :, 0:128],
                   v[:, hh * 128:(hh + 1) * 128].rearrange("(c p) e -> p c e", p=128), [], [vaug])
            kb.op("pool", [], [vaug], lambda e: e.memset(vaug[:, :, 128:129], 1.0))
            kb.op("dve", [tabs], [tabrep], lambda e, hh=hh: e.tensor_copy(
                tabrep[:, :], tabs[:, hh:hh + 1].to_broadcast([33, 128])))
            ps = s12.next()
            ps2 = s12.next()

            def fmm(e, ps=ps, ps2=ps2):
                e.matmul(ps[:, 0:512], tabrep[:, :], ohs[:, 0:512], start=True, stop=True)
                e.matmul(ps[:, 512:1024], tabrep[:, :], ohs[:, 512:1024], start=True, stop=True)
                return e.matmul(ps2[:, 0:FL - 1024], tabrep[:, :], ohs[:, 1024:FL], start=True, stop=True)
            kb.op("pe", [tabrep, ohs], [ps, ps2], fmm)
            kb.op("dve", [ps], [frep], lambda e, ps=ps: e.tensor_copy(frep[:, 0:1024], ps[:, :]))
            kb.op("dve", [ps2, frep], [frep], lambda e, ps2=ps2: e.tensor_copy(
                frep[:, 1024:FL], ps2[:, 0:FL - 1024]))
            kb.op("dve", [frep], [c31], lambda e: e.tensor_copy(c31[:, :], frep[:, FL - 1:FL]))
            ftrk = Trk()
            kb.dma("sp", bass.AP(fscr[hh], 0, [[FP, 128], [1, FL]]), frep[:, :], [frep], [ftrk])
            for d in range(5):
                delta = -128 + 128 * d
                kb.dma("sp", biasT[d][:, :], bass.AP(fscr[hh], 511 - delta, [[FL, 128], [1, 512]]),
                       [ftrk], [biasT[d]])

            steps = [(I, j) for I in range(NQB) for j in range(4 * I + 4)]
            qblk = {}
            deferred = []

            def get_q(I):
                if I not in qblk:
                    qb = qr.next()
                    kb.dma("sp", qb[:, :], qT[hh * 128:(hh + 1) * 128, 512 * I:512 * (I + 1)], [], [qb])
                    qblk[I] = qb
                return qblk[I]

            def qk(I, j):
                qb = get_q(I)
                ps = s12.next()

                def fn(e, ps=ps, qb=qb, j=j):
                    e.matmul(ps[:, 0:512], kTs[0:64, 128 * j:128 * (j + 1)], qb[0:64, :],
                             start=True, stop=True)
                    return e.matmul(ps[:, 512:1024], kTs[64:128, 128 * j:128 * (j + 1)], qb[64:128, :],
                                    start=True, stop=True)
                kb.op("pe", [kTs, qb], [ps], fn)
                return ps

            cur = qk(*steps[0])
            for si, (I, j) in enumerate(steps):
                nxt = qk(*steps[si + 1]) if si + 1 < len(steps) else None
                pb = pr.next()
                if j <= 4 * I - 2:
                    kb.op("act", [cur, c31], [pb], lambda e, cur=cur, pb=pb: e.activation(
                        pb[:, :], cur[:, :], AF.Exp, bias=c31[:, 0:1], scale=0.125))
                else:
                    d = j - (4 * I - 1)
                    bt = biasT[d]
                    tm = tmpr.next()
                    for h2 in range(2):
                        kb.op("dve", [cur, bt], [tm], lambda e, cur=cur, tm=tm, bt=bt, h2=h2: e.scalar_tensor_tensor(
                            tm[:, 512 * h2:512 * (h2 + 1)], cur[:, 512 * h2:512 * (h2 + 1)], 0.125, bt[:, :],
                            ALU.mult, ALU.add))
                    kb.op("act", [tm], [pb], lambda e, tm=tm, pb=pb: e.activation(
                        pb[:, :], tm[:, :], AF.Exp))
                last = (j == 4 * I + 3)

                def av(e, pb=pb, j=j, last=last):
                    ins = None
                    for h2 in range(2):
                        for qc in range(4):
                            g = h2 * 4 + qc
                            ins = e.matmul(acc_ap(h2, qc), pb[:, 512 * h2 + 128 * qc:512 * h2 + 128 * (qc + 1)],
                                           vaug[:, j, :], start=(j == 0 and g % 3 == 0), stop=last,
                                           skip_group_check=True)
                    return ins
                kb.op("pe", [pb, vaug], [oacc], av)
                if last:
                    ob = osb.next()
                    for bnk in range(3):
                        ng = 3 if bnk < 2 else 2
                        kb.op("dve", [oacc], [ob], lambda e, ob=ob, bnk=bnk, ng=ng: e.tensor_copy(
                            ob[:, 3 * bnk:3 * bnk + ng, 0:129],
                            oacc[:, bnk, 0:ng * 129].rearrange("p (g c) -> p g c", g=ng)))
                    rl = smr.next()
                    of = ofr.next()
                    kb.op("dve", [ob], [rl], lambda e, ob=ob, rl=rl: e.reciprocal(
                        rl[:, 0:8].unsqueeze(2), ob[:, 0:8, 128:129]))
                    kb.op("dve", [rl, nlam], [rl], lambda e, rl=rl: e.tensor_scalar(
                        rl[:, 4:8], rl[:, 4:8], nlam[:, 0:1], None, ALU.mult))
                    kb.op("dve", [ob, rl], [of], lambda e, ob=ob, rl=rl, of=of: e.tensor_tensor(
                        of[:, :, :], ob[:, 0:4, 0:128], rl[:, 0:4].unsqueeze(2).broadcast_to([128, 4, 128]),
                        ALU.mult))
                    kb.op("dve", [ob, rl], [ob], lambda e, ob=ob, rl=rl: e.tensor_tensor(
                        ob[:, 4:8, 0:128], ob[:, 4:8, 0:128],
                        rl[:, 4:8].unsqueeze(2).broadcast_to([128, 4, 128]), ALU.mult))
                    kb.op("dve", [ob, of], [of], lambda e, ob=ob, of=of: e.tensor_tensor(
                        of[:, :, :], of[:, :, :], ob[:, 4:8, 0:128], ALU.add))
                    kb.op("dve", [of], [ob], lambda e, ob=ob, of=of: e.tensor_tensor(
                        ob[:, 0:4, 0:128], of[:, :, :], of[:, :, :], ALU.mult))
                    kb.op("dve", [ob], [rl], lambda e, ob=ob, rl=rl: e.reduce_sum(
                        rl[:, 8:12], ob[:, 0:4, 0:128], axis=AX.X))

                    def e2(rl=rl):
                        kb.op("act", [rl, epst], [rl], lambda e: e.activation(
                            rl[:, 12:16], rl[:, 8:12], AF.Ln, bias=epst[:, 0:1], scale=1.0 / 128))
                        kb.op("act", [rl], [rl], lambda e: e.activation(
                            rl[:, 8:12], rl[:, 12:16], AF.Exp, scale=-0.5))

                    def e3(rl=rl, of=of, I=I):
                        on = onr.next()
                        og = ostage.next()
                        kb.op("dve", [of, rl], [of], lambda e: e.tensor_tensor(
                            of[:, :, :], of[:, :, :], rl[:, 8:12].unsqueeze(2).broadcast_to([128, 4, 128]),
                            ALU.mult))
                        kb.op("dve", [of, g_rep], [on], lambda e: e.tensor_tensor(
                            on[:, :, :], of[:, :, :], g_rep[:, :].unsqueeze(1).broadcast_to([128, 4, 128]),
                            ALU.mult))

                        def trq(e):
                            ins = None
                            for qc in range(4):
                                ins = e.transpose(pT[:, qc, :], on[:, qc, :], identb[:, :])
                            return ins
                        kb.op("pe", [on, identb], [pT], trq)
                        kb.op("dve", [pT], [og], lambda e: e.tensor_copy(
                            og[:, :].rearrange("p (a b) -> p a b", a=4), pT[:, :, :]))
                        kb.dma("sp", oT[hh * 128:(hh + 1) * 128, 512 * I:512 * (I + 1)], og[:, :], [og], [OUT])
                    deferred.append((si + 3, e2))
                    deferred.append((si + 6, e3))
                while deferred and deferred[0][0] <= si:
                    deferred.pop(0)[1]()
                cur = nxt
            while deferred:
                deferred.pop(0)[1]()
        kb.finish()
    return nc


def load_w_resident(kb, name, w_ap, ncols, wst):
    wb = kb.sb(name, [128, NKC, ncols], BF16)
    for p in range(0, ncols, 512):
        nco = min(512, ncols - p)
        ws = wst.next()
        kb.dma("sp", ws[:, :, 0:nco], w_ap[:, p:p + nco].rearrange("(kc p) n -> p kc n", p=128), [], [ws])
        kb.op("pool", [ws], [wb], lambda e, ws=ws, p=p, nco=nco: e.tensor_copy(
            wb[:, :, p:p + nco], ws[:, :, 0:nco]))
    return wb


def build_C1(T):
    nc = bass.Bass("TRN2", target_bir_lowering=False)
    NB = T // 512
    with ExitStack() as st:
        kb = KB(nc, st)
        dt = nc.dram_tensor
        oT = dt("oT", [D, T], BF16, kind="ExternalInput").ap()
        SaT = dt("SaT", [D, T], BF16, kind="ExternalInput").ap()
        YT = dt("YT", [D, T], BF16, kind="ExternalInput").ap()
        h = dt("h", [T, D], F32, kind="ExternalInput").ap()
        w_ap = dt("w_ap", [D, D], F32, kind="ExternalInput").ap()
        w_out = dt("w_out", [D, D], F32, kind="ExternalInput").ap()
        lng = dt("lng", [128, D], F32, kind="ExternalInput").ap()
        lnb = dt("lnb", [128, D], F32, kind="ExternalInput").ap()
        h1 = dt("h1", [T, D], F32, kind="ExternalOutput").ap()
        OUT = kb.out_trk
        g_rep = load_const(kb, "g_rep", lng, [128, D])
        b_rep = load_const(kb, "b_rep", lnb, [128, D])
        wst = Ring([kb.sb("wst%d" % i, [128, NKC, 512], F32) for i in range(2)])
        wap_b = load_w_resident(kb, "wap_b", w_ap, D, wst)
        wout_b = load_w_resident(kb, "wout_b", w_out, D, wst)
        psr = PsumRing(kb, 6)
        obr = Ring([kb.sb("ob%d" % i, [128, NKC, 512], BF16) for i in range(2)])
        sar = Ring([kb.sb("sa%d" % i, [128, NKC, 512], BF16) for i in range(2)])
        yr = Ring([kb.sb("yb%d" % i, [128, NKC, 512], BF16) for i in range(2)])
        mbr = Ring([kb.sb("mb%d" % i, [128, NKC, 512], BF16) for i in range(2)])
        tmr = Ring([kb.sb("tm%d" % i, [128, 512], F32) for i in range(3)])
        xr = Ring([kb.sb("x%d" % i, [128, D], F32) for i in range(3)])
        st6 = kb.sb("st6", [128, 12], F32)
        mv = kb.sb("mv", [128, 2], F32)
        rstd = kb.sb("rstd", [128, 1], F32)
        for n in range(NB):
            ob, sa, yb, mb = obr.next(), sar.next(), yr.next(), mbr.next()
            sl = slice(512 * n, 512 * (n + 1))
            kb.dma("sp", ob[:, :, :], oT[:, sl].rearrange("(c p) t -> p c t", p=128), [], [ob])
            kb.dma("sp", sa[:, :, :], SaT[:, sl].rearrange("(c p) t -> p c t", p=128), [], [sa])
            kb.dma("sp", yb[:, :, :], YT[:, sl].rearrange("(c p) t -> p c t", p=128), [], [yb])
            for j in range(8):
                ps = psr.next()
                mm_group(kb, ps, ps[:, :], [wap_b[:, kc, 128 * j:128 * (j + 1)] for kc in range(NKC)],
                         [ob[:, kc, :] for kc in range(NKC)], [wap_b, ob])
                tm = tmr.next()
                kb.op("dve", [ps, sa], [tm], lambda e, ps=ps, sa=sa, tm=tm, j=j: e.tensor_tensor(
                    tm[:, :], ps[:, :], sa[:, j, :], ALU.mult))
                kb.op("pool", [tm, yb], [mb], lambda e, tm=tm, yb=yb, mb=mb, j=j: e.tensor_tensor(
                    mb[:, j, :], tm[:, :], yb[:, j, :], ALU.add))
            for a in range(4):
                i = 4 * n + a
                x = xr.next()
                kb.dma("sp", x[:, :], h[i * 128:(i + 1) * 128, :], [], [x])
                for half in range(2):
                    ps = psr.next()
                    mm_group(kb, ps, ps[:, :], [mb[:, kc, 128 * a:128 * (a + 1)] for kc in range(NKC)],
                             [wout_b[:, kc, 512 * half:512 * (half + 1)] for kc in range(NKC)], [wout_b, mb])
                    kb.op("dve", [ps, x], [x], lambda e, ps=ps, x=x, half=half: e.scalar_tensor_tensor(
                        x[:, 512 * half:512 * (half + 1)], x[:, 512 * half:512 * (half + 1)], DN_ALPHA,
                        ps[:, :], ALU.mult, ALU.add))
                layer_norm_tile(kb, x, x, g_rep, b_rep, st6, mv, rstd)
                kb.dma("act", h1[i * 128:(i + 1) * 128, :], x[:, :], [x], [OUT])
        kb.finish()
    return nc


def build_C2(T):
    nc = bass.Bass("TRN2", target_bir_lowering=False)
    NT = T // 128
    NBLK = (2 * T + 32 * (MOE_BS - 1) + MOE_BS - 1) // MOE_BS
    NSUB = MOE_BS // 128
    with ExitStack() as st:
        kb = KB(nc, st)
        dt = nc.dram_tensor
        h1 = dt("h1", [T, D], F32, kind="ExternalInput").ap()
        w_r = dt("w_r", [D, 36], F32, kind="ExternalInput").ap()
        b_r = dt("b_r", [128, 36], F32, kind="ExternalInput").ap()
        w1 = dt("w1", [32, D, 512], F32, kind="ExternalInput").ap()
        w3 = dt("w3", [32, D, 512], F32, kind="ExternalInput").ap()
        w2 = dt("w2", [32, 512, D], F32, kind="ExternalInput").ap()
        lng = dt("lng", [128, D], F32, kind="ExternalInput").ap()
        lnb = dt("lnb", [128, D], F32, kind="ExternalInput").ap()
        ident = dt("ident", [128, 128], F32, kind="ExternalInput").ap()
        utri = dt("utri", [128, 128], F32, kind="ExternalInput").ap()
        blk128 = dt("blk128", [128, NBLK], F32, kind="ExternalInput").ap()
        pidx_d = dt("pidx", [128, 1], F32, kind="ExternalInput").ap()
        h2 = dt("h2", [T, D], F32, kind="ExternalOutput").ap()
        xbuf = nc.dram_tensor("xbuf", [NBLK * MOE_BS, D], BF16).ap()
        ybuf = nc.dram_tensor("ybuf", [NBLK * MOE_BS, D], F32).ap()
        OUT = kb.out_trk

        g_rep = load_const(kb, "g_rep", lng, [128, D])
        b_rep = load_const(kb, "b_rep", lnb, [128, D])
        identf = load_const(kb, "identf", ident, [128, 128])
        identb = kb.sb("identb", [128, 128], BF16)
        kb.op("dve", [identf], [identb], lambda e: e.tensor_copy(identb[:, :], identf[:, :]))
        utf = load_const(kb, "utf", utri, [128, 128])
        utb = kb.sb("utb", [128, 128], BF16)
        kb.op("dve", [utf], [utb], lambda e: e.tensor_copy(utb[:, :], utf[:, :]))
        onesb = kb.sb("onesb", [128, 128], BF16)
        kb.op("dve", [], [onesb], lambda e: e.memset(onesb[:, :], 1.0))
        b128 = load_const(kb, "b128", blk128, [128, NBLK])
        brs = load_const(kb, "brs", b_r, [128, 36])
        wrf = kb.sb("wrf", [128, NKC, 36], F32)
        kb.dma("sp", wrf[:, :, :], w_r.rearrange("(kc p) n -> p kc n", p=128), [], [wrf])
        wrb = kb.sb("wrb", [128, NKC, 36], BF16)
        kb.op("dve", [wrf], [wrb], lambda e: e.tensor_copy(wrb[:, :, :], wrf[:, :, :]))

        psr = PsumRing(kb, 6)
        pT = kb.ps("pT", [128, NKC, 128], BF16)
        pT2 = kb.ps("pT2", [128, 4, 128], BF16)
        xr = Ring([kb.sb("x%d" % i, [128, D], F32) for i in range(3)])
        hbr = Ring([kb.sb("hb%d" % i, [128, D], BF16) for i in range(2)])
        hTr = Ring([kb.sb("hTt%d" % i, [128, NKC, 128], BF16) for i in range(2)])
        st6 = kb.sb("st6", [128, 12], F32)
        mv = kb.sb("mv", [128, 2], F32)
        rstd = kb.sb("rstd", [128, 1], F32)

        M1 = kb.sb("M1", [128, NT, 32], F32)
        M2 = kb.sb("M2", [128, NT, 32], F32)
        Mb = kb.sb("Mb", [128, NT, 32], BF16)
        rank = kb.sb("rank", [128, NT, 32], F32)
        gates = kb.sb("gates", [128, NT, 2], F32)
        destf = kb.sb("destf", [128, NT * 2], F32)
        desti = kb.sb("desti", [128, NT * 2], I32)
        lgr = Ring([kb.sb("lg%d" % i, [128, 36], F32) for i in range(2)])
        smr = Ring([kb.sb("smr%d" % i, [128, 48], F32) for i in range(2)])

        for i in range(NT):
            x = xr.next()
            hb = hbr.next()
            hTt = hTr.next()
            kb.dma("sp", x[:, :], h1[i * 128:(i + 1) * 128, :], [], [x])
            kb.op("act", [x], [hb], lambda e, x=x, hb=hb: e.copy(hb[:, :], x[:, :]))
            transpose_to(kb, hb, identb, pT, hTt[:, :, :], [hTt], NKC, eng="act")
            ps = psr.next()
            mm_group(kb, ps, ps[:, 0:36], [hTt[:, kc, :] for kc in range(NKC)],
                     [wrb[:, kc, :] for kc in range(NKC)], [hTt, wrb])
            lg = lgr.next()
            sm = smr.next()
            m1 = M1.trk(i)
            m2 = M2.trk(i)
            kb.op("dve", [ps, brs], [lg], lambda e, ps=ps, lg=lg: e.tensor_tensor(
                lg[:, :], ps[:, 0:36], brs[:, :], ALU.add))
            kb.op("dve", [lg], [sm], lambda e, lg=lg, sm=sm: e.reduce_max(sm[:, 0:1], lg[:, 0:4], axis=AX.X))
            kb.op("dve", [sm], [sm], lambda e, sm=sm: e.tensor_scalar(
                sm[:, 1:2], sm[:, 0:1], -1.0, None, ALU.mult))
            kb.op("act", [lg, sm], [sm], lambda e, lg=lg, sm=sm: e.activation(
                sm[:, 44:48], lg[:, 0:4], AF.Exp, bias=sm[:, 1:2], accum_out=sm[:, 2:3]))
            kb.op("dve", [sm], [sm], lambda e, sm=sm: e.reciprocal(sm[:, 3:4], sm[:, 2:3]))
            kb.op("dve", [lg, sm], [sm], lambda e, lg=lg, sm=sm: e.tensor_scalar(
                sm[:, 4:8], lg[:, 0:4], sm[:, 0:1], None, ALU.is_equal))
            kb.op("dve", [lg, sm], [sm], lambda e, lg=lg, sm=sm: e.tensor_scalar(
                sm[:, 8:16], lg[:, 4:12], sm[:, 4:5], None, ALU.mult))
            for g in range(1, 4):
                kb.op("dve", [lg, sm], [sm], lambda e, lg=lg, sm=sm, g=g: e.scalar_tensor_tensor(
                    sm[:, 8:16], lg[:, 4 + 8 * g:12 + 8 * g], sm[:, 4 + g:5 + g], sm[:, 8:16], ALU.mult, ALU.add))
            kb.op("dve", [sm], [sm], lambda e, sm=sm: e.max(sm[:, 16:24], sm[:, 8:16]))
            kb.op("dve", [sm], [sm], lambda e, sm=sm: e.tensor_scalar(
                sm[:, 24:32], sm[:, 8:16], sm[:, 16:17], None, ALU.is_equal))
            kb.op("dve", [sm], [sm], lambda e, sm=sm: e.tensor_scalar(
                sm[:, 32:40], sm[:, 8:16], sm[:, 17:18], None, ALU.is_equal))
            kb.op("dve", [sm], [sm], lambda e, sm=sm: e.tensor_tensor(
                sm[:, 40:41], sm[:, 17:18], sm[:, 16:17], ALU.subtract))
            kb.op("act", [sm], [sm], lambda e, sm=sm: e.activation(sm[:, 40:41], sm[:, 40:41], AF.Exp))
            kb.op("dve", [sm], [sm], lambda e, sm=sm: e.tensor_scalar(
                sm[:, 41:42], sm[:, 40:41], 1.0, None, ALU.add))
            kb.op("dve", [sm], [sm], lambda e, sm=sm: e.reciprocal(sm[:, 41:42], sm[:, 41:42]))
            kb.op("dve", [sm], [gates.trk(i)], lambda e, sm=sm, i=i: e.tensor_tensor(
                gates[:, i, 0:1], sm[:, 3:4], sm[:, 41:42], ALU.mult))
            kb.op("dve", [sm, gates.trk(i)], [gates.trk(i)], lambda e, sm=sm, i=i: e.tensor_tensor(
                gates[:, i, 1:2], gates[:, i, 0:1], sm[:, 40:41], ALU.mult))
            for g in range(4):
                kb.op("dve", [sm], [m1], lambda e, sm=sm, i=i, g=g: e.tensor_scalar(
                    M1[:, i, 8 * g:8 * g + 8], sm[:, 24:32], sm[:, 4 + g:5 + g], None, ALU.mult))
                kb.op("dve", [sm], [m2], lambda e, sm=sm, i=i, g=g: e.tensor_scalar(
                    M2[:, i, 8 * g:8 * g + 8], sm[:, 32:40], sm[:, 4 + g:5 + g], None, ALU.mult))
            kb.op("dve", [m1, m2], [Mb.trk(i)], lambda e, i=i: e.tensor_tensor(
                Mb[:, i, :], M1[:, i, :], M2[:, i, :], ALU.add))

        maccr = Ring([kb.sb("macc%d" % i, [128, 32], BF16) for i in range(2)])
        macc = maccr.next()
        kb.op("dve", [], [macc], lambda e, macc=macc: e.memset(macc[:, :], 0.0))
        for i in range(NT):
            ps = psr.next()

            def rk(e, ps=ps, i=i, macc=macc):
                e.matmul(ps[:, 0:32], utb[:, :], Mb[:, i, :], start=True, stop=False)
                return e.matmul(ps[:, 0:32], onesb[:, :], macc[:, :], start=False, stop=True)
            kb.op("pe", [utb, onesb, Mb.trk(i), macc], [ps], rk)
            kb.op("act", [ps], [rank.trk(i)], lambda e, ps=ps, i=i: e.copy(rank[:, i, :], ps[:, 0:32]))
            nm = maccr.next()
            kb.op("dve", [macc, Mb.trk(i)], [nm], lambda e, macc=macc, nm=nm, i=i: e.tensor_tensor(
                nm[:, :], macc[:, :], Mb[:, i, :], ALU.add))
            macc = nm
        ps = psr.next()
        kb.op("pe", [onesb, macc], [ps], lambda e, ps=ps, macc=macc: e.matmul(
            ps[:, 0:32], onesb[:, :], macc[:, :], start=True, stop=True))
        sc = kb.sb("sc", [128, 6, 32], F32)
        SC = [sc]
        kb.op("dve", [ps], SC, lambda e, ps=ps: e.tensor_copy(sc[:, 0, :], ps[:, 0:32]))
        sci = kb.sb("sci", [128, 2, 32], I32)
        kb.op("dve", SC, SC, lambda e: e.tensor_scalar(sc[:, 1, :], sc[:, 0, :], float(MOE_BS - 1), None, ALU.add))
        kb.op("dve", SC, [sci], lambda e: e.tensor_copy(sci[:, 0, :], sc[:, 1, :]))
        kb.op("dve", [sci], [sci], lambda e: e.tensor_scalar(
            sci[:, 1, :], sci[:, 0, :], MOE_SH, MOE_SH, ALU.arith_shift_right, ALU.logical_shift_left))
        kb.op("dve", [sci], SC, lambda e: e.tensor_copy(sc[:, 2, :], sci[:, 1, :]))
        kb.op("dve", SC, SC, lambda e: e.tensor_copy(sc[:, 3, :], sc[:, 2, :]))
        a, b = 3, 4
        for sft in (1, 2, 4, 8, 16):
            kb.op("dve", SC, SC, lambda e, a=a, b=b: e.tensor_copy(sc[:, b, :], sc[:, a, :]))
            kb.op("dve", SC, SC, lambda e, a=a, b=b, sft=sft: e.tensor_tensor(
                sc[:, b, sft:32], sc[:, a, sft:32], sc[:, a, 0:32 - sft], ALU.add))
            a, b = b, a
        pe_idx = a
        kb.op("dve", SC, SC, lambda e: e.tensor_tensor(sc[:, 5, :], sc[:, pe_idx, :], sc[:, 2, :], ALU.subtract))
        tmpd = Ring([kb.sb("tmpd%d" % i, [128, 32], F32) for i in range(2)])
        junk32 = kb.sb("junk32", [128, 2, 32], F32)
        for i in range(NT):
            td = tmpd.next()
            kb.op("dve", SC + [rank.trk(i)], [td], lambda e, td=td, i=i: e.tensor_tensor(
                td[:, :], rank[:, i, :], sc[:, 5, :], ALU.add))
            for jj, (MM, mt) in enumerate(((M1, M1.trk(i)), (M2, M2.trk(i)))):
                td2 = tmpd.next() if False else None
                kb.op("dve", [td, mt], [destf], lambda e, td=td, MM=MM, i=i, jj=jj: e.tensor_tensor(
                    junk32[:, jj, :], td[:, :], MM[:, i, :], ALU.mult))
                kb.op("dve", [destf], [destf], lambda e, i=i, jj=jj: e.reduce_sum(
                    destf[:, 2 * i + jj:2 * i + jj + 1], junk32[:, jj, :], axis=AX.X))
        kb.op("dve", [destf], [desti], lambda e: e.tensor_copy(desti[:, :], destf[:, :]))
        blkf = kb.sb("blkf", [128, NBLK], F32)
        kb.op("dve", [], [blkf], lambda e: e.memset(blkf[:, :], 0.0))
        for ex in range(32):
            kb.op("dve", SC + [b128, blkf], [blkf], lambda e, ex=ex: e.scalar_tensor_tensor(
                blkf[:, :], b128[:, :], sc[:, pe_idx, ex:ex + 1], blkf[:, :], ALU.is_ge, ALU.add))
        kb.op("dve", [blkf], [blkf], lambda e: e.tensor_scalar(blkf[:, :], blkf[:, :], 31.0, None, ALU.min))

        pidx = load_const(kb, "pidx_s", pidx_d, [128, 1])
        widx = kb.sb("widx", [128, NBLK], I32)
        kb.op("dve", [blkf, pidx], [blkf], lambda e: e.tensor_scalar(
            blkf[:, :], blkf[:, :], 128.0, pidx[:, 0:1], ALU.mult, ALU.add))
        kb.op("dve", [blkf], [widx], lambda e: e.tensor_copy(widx[:, :], blkf[:, :]))
        w1rows = w1.rearrange("e (p kk) n -> (e p) (kk n)", kk=8)
        w3rows = w3.rearrange("e (p kk) n -> (e p) (kk n)", kk=8)
        w2rows = w2.rearrange("e (p kk) n -> (e p) (kk n)", kk=4)

        zt = kb.sb("zt", [128, D], BF16)
        kb.op("pool", [], [zt], lambda e: e.memset(zt[:, :], 0.0))
        xz = Trk()
        for bq in range(NBLK * NSUB):
            kb.dma("sp", xbuf[bq * 128:(bq + 1) * 128, :], zt[:, :], [zt], [xz])
        xs = Trk()
        for i in range(NT):
            x = xr.next()
            hb = hbr.next()
            kb.dma("sp", x[:, :], h1[i * 128:(i + 1) * 128, :], [], [x])
            kb.op("act", [x], [hb], lambda e, x=x, hb=hb: e.copy(hb[:, :], x[:, :]))
            for jj in range(2):
                kb.dma_fn("pool", lambda e, hb=hb, i=i, jj=jj: e.indirect_dma_start(
                    out=xbuf, out_offset=bass.IndirectOffsetOnAxis(ap=desti[:, 2 * i + jj:2 * i + jj + 1], axis=0),
                    in_=hb[:, :], in_offset=None), [hb, desti, xz], [xs])

        w1s = Ring([kb.sb("w1s%d" % i, [128, NKC, 512], F32) for i in range(2)])
        w3s = Ring([kb.sb("w3s%d" % i, [128, NKC, 512], F32) for i in range(1)])
        w2s = Ring([kb.sb("w2s%d" % i, [128, 4, D], F32) for i in range(1)])
        w1b = Ring([kb.sb("w1b%d" % i, [128, NKC, 512], BF16) for i in range(2)])
        w3b = Ring([kb.sb("w3b%d" % i, [128, NKC, 512], BF16) for i in range(2)])
        w2b = Ring([kb.sb("w2b%d" % i, [128, 4, D], BF16) for i in range(2)])
        xbr = Ring([kb.sb("xb%d" % i, [128, D], BF16) for i in range(2)])
        xTr = Ring([kb.sb("xT%d" % i, [128, NKC, 128], BF16) for i in range(2)])
        sar = Ring([kb.sb("sA%d" % i, [128, 512], F32) for i in range(2)])
        gr = Ring([kb.sb("g%d" % i, [128, 512], BF16) for i in range(2)])
        gTr = Ring([kb.sb("gT%d" % i, [128, 4, 128], BF16) for i in range(2)])
        yr = Ring([kb.sb("y%d" % i, [128, D], F32) for i in range(2)])
        ys = Trk()
        for bq in range(NBLK):
            ws1, ws3, ws2 = w1s.next(), w3s.next(), w2s.next()
            for wsx, wrows in ((ws1, w1rows), (ws3, w3rows), (ws2, w2rows)):
                kb.dma_fn("pool", lambda e, wsx=wsx, wrows=wrows, bq=bq: e.indirect_dma_start(
                    out=wsx[:, :, :].rearrange("p a b -> p (a b)"), out_offset=None, in_=wrows,
                    in_offset=bass.IndirectOffsetOnAxis(ap=widx[:, bq:bq + 1], axis=0)), [widx], [wsx])
            wb1, wb3, wb2 = w1b.next(), w3b.next(), w2b.next()
            kb.op("dve", [ws1], [wb1], lambda e, ws1=ws1, wb1=wb1: e.tensor_copy(wb1[:, :, :], ws1[:, :, :]))
            kb.op("dve", [ws3], [wb3], lambda e, ws3=ws3, wb3=wb3: e.tensor_copy(wb3[:, :, :], ws3[:, :, :]))
            kb.op("act", [ws2], [wb2], lambda e, ws2=ws2, wb2=wb2: e.copy(wb2[:, :, :], ws2[:, :, :]))
            for sub in range(NSUB):
                r0 = (bq * NSUB + sub) * 128
                xb = xbr.next()
                kb.dma("sp", xb[:, :], xbuf[r0:r0 + 128, :], [xs], [xb])
                xT = xTr.next()
                transpose_to(kb, xb, identb, pT, xT[:, :, :], [xT], NKC, eng="dve", strided=True)
                pa, pb = psr.next(), psr.next()
                mm_group(kb, pa, pa[:, :], [xT[:, kc, :] for kc in range(NKC)],
                         [wb1[:, kc, :] for kc in range(NKC)], [xT, wb1])
                mm_group(kb, pb, pb[:, :], [xT[:, kc, :] for kc in range(NKC)],
                         [wb3[:, kc, :] for kc in range(NKC)], [xT, wb3])
                sA = sar.next()
                gg = gr.next()
                kb.op("act", [pa], [sA], lambda e, pa=pa, sA=sA: e.activation(sA[:, :], pa[:, :], AF.Silu))
                kb.op("dve", [sA, pb], [gg], lambda e, sA=sA, pb=pb, gg=gg: e.tensor_tensor(
                    gg[:, :], sA[:, :], pb[:, :], ALU.mult))
                gT = gTr.next()

                def trg(e, gg=gg):
                    ins = None
                    for c in range(4):
                        ins = e.transpose(pT2[:, c, :], gg[:, slice(c, None, 4)], identb[:, :])
                    return ins
                kb.op("pe", [gg, identb], [pT2], trg)
                kb.op("dve", [pT2], [gT], lambda e, gT=gT: e.tensor_copy(gT[:, :, :], pT2[:, :, :]))
                y = yr.next()
                for half in range(2):
                    pc = psr.next()
                    mm_group(kb, pc, pc[:, :], [gT[:, kc, :] for kc in range(4)],
                             [wb2[:, kc, 512 * half:512 * (half + 1)] for kc in range(4)], [gT, wb2])
                    if half == 0:
                        kb.op("act", [pc], [y], lambda e, pc=pc, y=y: e.copy(y[:, 0:512], pc[:, :]))
                    else:
                        kb.op("dve", [pc], [y], lambda e, pc=pc, y=y: e.tensor_copy(y[:, 512:1024], pc[:, :]))
                kb.dma("act", ybuf[r0:r0 + 128, :], y[:, :], [y], [ys])

        y1r = yr
        y2r = Ring([kb.sb("y2_%d" % i, [128, D], F32) for i in range(2)])
        for i in range(NT):
            x = xr.next()
            y1, y2 = y1r.next(), y2r.next()
            kb.dma("sp", x[:, :], h1[i * 128:(i + 1) * 128, :], [], [x])
            for jj, yy in enumerate((y1, y2)):
                kb.dma_fn("pool", lambda e, yy=yy, i=i, jj=jj: e.indirect_dma_start(
                    out=yy[:, :], out_offset=None, in_=ybuf,
                    in_offset=bass.IndirectOffsetOnAxis(ap=desti[:, 2 * i + jj:2 * i + jj + 1], axis=0)),
                    [desti, ys], [yy])
            gt = gates.trk(i)
            kb.op("dve", [y1, gt], [y1], lambda e, y1=y1, i=i: e.tensor_scalar(
                y1[:, :], y1[:, :], gates[:, i, 0:1], None, ALU.mult))
            kb.op("dve", [y1, y2, gt], [y1], lambda e, y1=y1, y2=y2, i=i: e.scalar_tensor_tensor(
                y1[:, :], y2[:, :], gates[:, i, 1:2], y1[:, :], ALU.mult, ALU.add))
            kb.op("dve", [x, y1], [x], lambda e, x=x, y1=y1: e.scalar_tensor_tensor(
                x[:, :], x[:, :], DN_ALPHA, y1[:, :], ALU.mult, ALU.add))
            layer_norm_tile(kb, x, x, g_rep, b_rep, st6, mv, rstd)
            kb.dma("act", h2[i * 128:(i + 1) * 128, :], x[:, :], [x], [OUT])
        kb.finish()
    return nc


def build_LN0(T):
    nc = bass.Bass("TRN2", target_bir_lowering=False)
    with ExitStack() as st:
        kb = KB(nc, st)
        dt = nc.dram_tensor
        x_d = dt("x", [T, D], F32, kind="ExternalInput").ap()
        lng = dt("lng", [128, D], F32, kind="ExternalInput").ap()
        lnb = dt("lnb", [128, D], F32, kind="ExternalInput").ap()
        h0 = dt("h0", [T, D], F32, kind="ExternalOutput").ap()
        g_rep = load_const(kb, "g_rep", lng, [128, D])
        b_rep = load_const(kb, "b_rep", lnb, [128, D])
        xr = Ring([kb.sb("x%d" % i, [128, D], F32) for i in range(4)])
        st6 = kb.sb("st6", [128, 12], F32)
        mv = kb.sb("mv", [128, 2], F32)
        rstd = kb.sb("rstd", [128, 1], F32)
        for i in range(T // 128):
            x = xr.next()
            kb.dma("sp", x[:, :], x_d[i * 128:(i + 1) * 128, :], [], [x])
            layer_norm_tile(kb, x, x, g_rep, b_rep, st6, mv, rstd)
            kb.dma("act", h0[i * 128:(i + 1) * 128, :], x[:, :], [x], [kb.out_trk])
        kb.finish()
    return nc


_PROGS = {}


def _prog(name, fn, *args):
    key = (name,) + args
    if key not in _PROGS:
        _PROGS[key] = fn(*args)
    return _PROGS[key]


def _lay(a):
    J = a.shape[0]
    return np.ascontiguousarray(a.reshape(J, 8, 128).transpose(2, 0, 1).reshape(128, J * 8))


def _rep(a):
    return np.ascontiguousarray(np.broadcast_to(np.asarray(a, np.float32).reshape(1, -1), (128, a.size)))


def kernel(x, ln0_g, ln0_b, rel_table, w_in, gate_b, lam_vecs, subln_g, w_attn_proj, conv_w,
           w_conv_proj, w_out, ln1_g, ln1_b, w_rg, b_rg, w_re, b_re, w1, w3, w2, ln2_g, ln2_b):
    f32 = np.float32
    x = np.asarray(x, f32)
    B, S, _ = x.shape
    NC = 8
    T = B * S // NC
    CPS = S // T
    cores = list(range(NC))
    depth = w_in.shape[0]
    ident = np.eye(128, dtype=f32)
    xt = x.reshape(B * S, D)

    def run(nc, maps):
        return run_bass_kernel_spmd(nc, maps, core_ids=cores).results

    res = run(_prog("LN0", build_LN0, T),
              [{"x": xt[c * T:(c + 1) * T], "lng": _rep(ln0_g), "lnb": _rep(ln0_b)} for c in cores])
    h = [r["h0"] for r in res]

    oh = bias_onehot()
    NBLK = (2 * T + 32 * (MOE_BS - 1) + MOE_BS - 1) // MOE_BS
    utri = np.triu(np.ones((128, 128), f32), 1)
    blk128 = np.ascontiguousarray(np.broadcast_to(np.arange(NBLK, dtype=f32) * MOE_BS, (128, NBLK)))
    pidx = np.arange(128, dtype=f32).reshape(128, 1)

    for l in range(depth):
        lam_init = 0.8 - 0.6 * math.exp(-0.3 * l)
        maps = []
        for c in cores:
            first = (c % CPS == 0)
            halo = np.zeros((128, D), f32) if first else h[c - 1][T - 128:]
            maps.append({"hin": np.concatenate([halo, h[c]], 0),
                         "halo_mask": np.full((128, 1), 0.0 if first else 1.0, f32),
                         "w_in": np.asarray(w_in[l], f32), "gate_b": _lay(np.asarray(gate_b[l], f32)),
                         "conv_w": _lay(np.asarray(conv_w[l], f32)),
                         "w_cp": np.asarray(w_conv_proj[l], f32), "ident": ident})
        ra = run(_prog("A", build_A, T, False), maps)
        maps = []
        for c in cores:
            b, hp = c // CPS, c % CPS
            src = [ra[b * CPS + t] for t in range(CPS)]
            rows = slice(hp * 256, (hp + 1) * 256)
            tab = np.concatenate([np.asarray(rel_table, f32)[:, 2 * hp:2 * hp + 2],
                                  np.full((1, 2), -30000.0, f32)], 0)
            maps.append({"qT": np.concatenate([r["qT"][rows] for r in src], 1),
                         "kT": np.concatenate([r["kT"][rows] for r in src], 1),
                         "v": np.concatenate([r["v"][:, rows] for r in src], 0),
                         "oh": oh, "tab": tab,
                         "lamv": _rep(np.asarray(lam_vecs[l], f32).reshape(-1)),
                         "subg": _rep(subln_g[l]),
                         "lamc": np.ascontiguousarray(np.broadcast_to(
                             np.array([[-lam_init, 1.0 - lam_init]], f32), (128, 2))),
                         "ident": ident})
        rt = run(_prog("ATT", build_ATT, S), maps)
        maps = []
        for c in cores:
            b, t = c // CPS, c % CPS
            oT = np.concatenate([rt[b * CPS + hp]["oT"][:, t * T:(t + 1) * T] for hp in range(CPS)], 0)
            maps.append({"oT": oT, "SaT": ra[c]["SaT"], "YT": ra[c]["YT"], "h": h[c],
                         "w_ap": np.asarray(w_attn_proj[l], f32), "w_out": np.asarray(w_out[l], f32),
                         "lng": _rep(ln1_g[l]), "lnb": _rep(ln1_b[l])})
        r1 = run(_prog("C1", build_C1, T), maps)
        w_r = np.concatenate([np.asarray(w_rg[l], f32), np.asarray(w_re[l], f32)], 1)
        b_r = _rep(np.concatenate([np.asarray(b_rg[l], f32), np.asarray(b_re[l], f32)]))
        w1l, w3l, w2l = np.asarray(w1[l], f32), np.asarray(w3[l], f32), np.asarray(w2[l], f32)
        maps = [{"h1": r1[c]["h1"], "w_r": w_r, "b_r": b_r, "w1": w1l, "w3": w3l, "w2": w2l,
                 "lng": _rep(ln2_g[l]), "lnb": _rep(ln2_b[l]), "ident": ident, "utri": utri,
                 "blk128": blk128, "pidx": pidx} for c in cores]
        r2 = run(_prog("C2", build_C2, T), maps)
        h = [r["h2"] for r in r2]
    return np.concatenate(h, 0).reshape(B, S, D).astype(f32)
```

```python
import math
from contextlib import ExitStack
import numpy as np
import concourse.bass as bass
import concourse.mybir as mybir
from concourse.bass_utils import run_bass_kernel_spmd

F32 = mybir.dt.float32
BF16 = mybir.dt.bfloat16
I32 = mybir.dt.int32
AF = mybir.ActivationFunctionType
ALU = mybir.AluOpType
AX = mybir.AxisListType


class Trk:
    __slots__ = ("w", "r")

    def __init__(self):
        self.w = None
        self.r = {}


class Buf(Trk):
    __slots__ = ("t", "subs")

    def __init__(self, t):
        Trk.__init__(self)
        self.t = t
        self.subs = {}

    def __getitem__(self, idx):
        return self.t[idx]

    def trk(self, key):
        s = self.subs.get(key)
        if s is None:
            s = self.subs[key] = Trk()
        return s


class KB:
    NDMA = 32

    def __init__(self, nc, st):
        self.nc = nc
        self.st = st
        self.eng = {"pe": nc.tensor, "act": nc.scalar, "dve": nc.vector,
                    "pool": nc.gpsimd, "sp": nc.sync}
        self.sem = {}
        self.cnt = {}
        self.known = {k: {} for k in self.eng}
        for k in ("pe", "act", "dve", "pool"):
            self.sem[k] = st.enter_context(nc.semaphore("s_" + k))
            self.cnt[k] = 0
        self.dsem = []
        for i in range(self.NDMA):
            key = "d%d" % i
            self.sem[key] = st.enter_context(nc.semaphore("s_" + key))
            self.cnt[key] = 0
            self.dsem.append(key)
        self.dnext = 0
        self._clear_sems()
        self.out_trk = Trk()
        self.prog = {k: [] for k in self.eng}
        self.nbuf = 0

    def _clear_sems(self):
        for h in self.sem.values():
            self.nc.gpsimd.sem_clear(h)
        self.nc.all_engine_barrier()

    def sb(self, name, shape, dtype):
        t = self.st.enter_context(self.nc.sbuf_tensor(name, list(shape), dtype))
        return Buf(t)

    def ps(self, name, shape, dtype):
        t = self.st.enter_context(self.nc.psum_tensor(name, list(shape), dtype))
        return Buf(t)

    def _wait(self, ename, deps):
        kn = self.known[ename]
        best = {}
        for d in deps:
            if d is None:
                continue
            k, v = d
            if best.get(k, 0) < v:
                best[k] = v
        waits = []
        for k, v in best.items():
            if k == "pe" and ename == "pe":
                continue
            if kn.get(k, 0) >= v:
                continue
            waits.append((k, v))
            kn[k] = v
        return waits

    def _deps(self, reads, writes):
        deps = []
        for b in reads:
            deps.append(b.w)
        for b in writes:
            deps.append(b.w)
            for k, v in b.r.items():
                deps.append((k, v))
        return deps

    def _commit(self, tok, reads, writes):
        k, v = tok
        for b in reads:
            if b.r.get(k, 0) < v:
                b.r[k] = v
        for b in writes:
            b.w = tok
            b.r = {}

    def op(self, ename, reads, writes, fn):
        waits = self._wait(ename, self._deps(reads, writes))
        self.cnt[ename] += 1
        self.prog[ename].append((waits, fn, ename, 1))
        tok = (ename, self.cnt[ename])
        self._commit(tok, reads, writes)
        return tok

    def dma_fn(self, qname, fn, reads, writes):
        key = self.dsem[self.dnext]
        self.dnext = (self.dnext + 1) % self.NDMA
        deps = self._deps(reads, writes)
        if self.cnt[key] > 0:
            deps.append((key, self.cnt[key]))
        waits = self._wait(qname, deps)
        self.cnt[key] += 16
        self.prog[qname].append((waits, fn, key, 16))
        tok = (key, self.cnt[key])
        self._commit(tok, reads, writes)
        return tok

    def dma(self, qname, out, in_, reads, writes, **kw):
        return self.dma_fn(qname, lambda e: e.dma_start(out=out, in_=in_, **kw), reads, writes)

    def finish(self):
        deps = [self.out_trk.w]
        for key in self.dsem:
            if self.cnt[key] > 0:
                deps.append((key, self.cnt[key]))
        for k in ("pe", "act", "dve", "pool"):
            if self.cnt[k] > 0:
                deps.append((k, self.cnt[k]))
        final_waits = self._wait("sp", deps)
        sem = self.sem
        prog = self.prog

        def emit(e, items, tail=()):
            for waits, fn, ik, iv in items:
                for k, v in waits:
                    e.wait_ge(sem[k], v)
                fn(e).then_inc(sem[ik], iv)
            for k, v in tail:
                e.wait_ge(sem[k], v)

        with self.nc.Block() as block:
            @block.tensor
            def _(e):
                emit(e, prog["pe"])

            @block.scalar
            def _(e):
                emit(e, prog["act"])

            @block.vector
            def _(e):
                emit(e, prog["dve"])

            @block.gpsimd
            def _(e):
                emit(e, prog["pool"])

            @block.sync
            def _(e):
                emit(e, prog["sp"], final_waits)

        self._clear_sems()


D = 1024
NKC = 8
LN_EPS = 1e-5
DN_ALPHA = 4 ** 0.25
MOE_BS = 512
MOE_SH = 9


class PsumRing:
    def __init__(self, kb, n, name="ps"):
        self.bufs = [kb.ps("%s%d" % (name, i), [128, 512], F32) for i in range(n)]
        self.i = 0

    def next(self):
        b = self.bufs[self.i]
        self.i = (self.i + 1) % len(self.bufs)
        return b


class Ring:
    def __init__(self, bufs):
        self.bufs = bufs
        self.i = 0

    def next(self):
        b = self.bufs[self.i]
        self.i = (self.i + 1) % len(self.bufs)
        return b


def mm_group(kb, ps, out_ap, lhs, rhs, reads):
    n = len(lhs)

    def fn(e):
        ins = None
        for k in range(n):
            ins = e.matmul(out_ap, lhs[k], rhs[k], start=(k == 0), stop=(k == n - 1))
        return ins
    return kb.op("pe", reads, [ps], fn)


def layer_norm_tile(kb, x, xo, g_rep, b_rep, st6, mv, rstd):
    def stats(e):
        e.bn_stats(st6[:, 0:6], x[:, 0:512])
        return e.bn_stats(st6[:, 6:12], x[:, 512:1024])
    kb.op("dve", [x], [st6], stats)
    kb.op("dve", [st6], [mv], lambda e: e.bn_aggr(mv[:, :], st6[:, :]))
    kb.op("dve", [mv], [rstd], lambda e: e.tensor_scalar(
        rstd[:, :], mv[:, 1:2], LN_EPS, None, ALU.add))
    kb.op("act", [rstd], [rstd], lambda e: e.activation(rstd[:, :], rstd[:, :], AF.Sqrt))
    kb.op("dve", [rstd], [rstd], lambda e: e.reciprocal(rstd[:, :], rstd[:, :]))
    kb.op("dve", [x, mv, rstd], [xo], lambda e: e.tensor_scalar(
        xo[:, :], x[:, :], mv[:, 0:1], rstd[:, 0:1], ALU.subtract, ALU.mult))
    kb.op("dve", [xo, g_rep], [xo], lambda e: e.tensor_tensor(
        xo[:, :], xo[:, :], g_rep[:, :], ALU.mult))
    kb.op("dve", [xo, b_rep], [xo], lambda e: e.tensor_tensor(
        xo[:, :], xo[:, :], b_rep[:, :], ALU.add))


def load_const(kb, name, dram_ap, shape, dtype=F32, q="sp"):
    b = kb.sb(name, shape, dtype)
    kb.dma(q, b[tuple(slice(None) for _ in shape)], dram_ap, [], [b])
    return b


def transpose_to(kb, src, identb, pT, dst_ap, dst_trks, nch, eng="act", strided=False):
    def tr(e):
        ins = None
        for c in range(nch):
            sl = slice(c, None, nch) if strided else slice(c * 128, (c + 1) * 128)
            ins = e.transpose(pT[:, c, :], src[:, sl], identb[:, :])
        return ins
    kb.op("pe", [src, identb], [pT], tr)
    if eng == "act":
        kb.op("act", [pT], dst_trks, lambda e: e.copy(dst_ap, pT[:, 0:nch, :]))
    else:
        kb.op("dve", [pT], dst_trks, lambda e: e.tensor_copy(dst_ap, pT[:, 0:nch, :]))


def build_A(T, layer0):
    nc = bass.Bass("TRN2", target_bir_lowering=False)
    TH = T + 128
    NB = T // 512
    NT = T // 128
    with ExitStack() as st:
        kb = KB(nc, st)
        dt = nc.dram_tensor
        hin = dt("hin", [TH, D], F32, kind="ExternalInput").ap()
        halo_mask = dt("halo_mask", [128, 1], F32, kind="ExternalInput").ap()
        w_in = dt("w_in", [D, 8192], F32, kind="ExternalInput").ap()
        gate_b = dt("gate_b", [128, 16], F32, kind="ExternalInput").ap()
        conv_w = dt("conv_w", [128, 24], F32, kind="ExternalInput").ap()
        w_cp = dt("w_cp", [D, D], F32, kind="ExternalInput").ap()
        ident = dt("ident", [128, 128], F32, kind="ExternalInput").ap()
        if layer0:
            lng = dt("lng", [128, D], F32, kind="ExternalInput").ap()
            lnb = dt("lnb", [128, D], F32, kind="ExternalInput").ap()
            h0 = dt("h0", [T, D], F32, kind="ExternalOutput").ap()
        qT = dt("qT", [D, T], BF16, kind="ExternalOutput").ap()
        kT = dt("kT", [D, T], BF16, kind="ExternalOutput").ap()
        v = dt("v", [T, D], BF16, kind="ExternalOutput").ap()
        SaT = dt("SaT", [D, T], BF16, kind="ExternalOutput").ap()
        YT = dt("YT", [D, T], BF16, kind="ExternalOutput").ap()
        OUT = kb.out_trk

        identf = load_const(kb, "identf", ident, [128, 128])
        identb = kb.sb("identb", [128, 128], BF16)
        kb.op("dve", [identf], [identb], lambda e: e.tensor_copy(identb[:, :], identf[:, :]))
        gb = load_const(kb, "gb", gate_b, [128, 16])
        cw = load_const(kb, "cw", conv_w, [128, 24])
        hm = load_const(kb, "hm", halo_mask, [128, 1])
        if layer0:
            g_rep = load_const(kb, "g_rep", lng, [128, D])
            b_rep = load_const(kb, "b_rep", lnb, [128, D])

        hT = kb.sb("hT", [128, NKC, TH], BF16)
        pT = kb.ps("pT", [128, NKC, 128], BF16)
        psr = PsumRing(kb, 6)

        xr = Ring([kb.sb("x%d" % i, [128, D], F32) for i in range(3)])
        hbr = Ring([kb.sb("hb%d" % i, [128, D], BF16) for i in range(2)])
        st6 = kb.sb("st6", [128, 12], F32)
        mv = kb.sb("mv", [128, 2], F32)
        rstd = kb.sb("rstd", [128, 1], F32)
        for i in range(TH // 128):
            x = xr.next()
            hb = hbr.next()
            kb.dma("sp", x[:, :], hin[i * 128:(i + 1) * 128, :], [], [x])
            if layer0:
                layer_norm_tile(kb, x, x, g_rep, b_rep, st6, mv, rstd)
                if i >= 1:
                    kb.dma("pool", h0[(i - 1) * 128:i * 128, :], x[:, :], [x], [OUT])
            kb.op("act", [x], [hb], lambda e, x=x, hb=hb: e.copy(hb[:, :], x[:, :]))
            transpose_to(kb, hb, identb, pT, hT[:, :, i * 128:(i + 1) * 128], [hT.trk(i)], NKC,
                         eng="dve" if i % 2 else "act")

        def hT_blk(n):
            return [hT.trk(1 + 4 * n + a) for a in range(4)]

        wst = Ring([kb.sb("wst%d" % i, [128, NKC, 512], F32) for i in range(1)])
        wbf = Ring([kb.sb("wbf%d" % i, [128, NKC, 512], BF16) for i in range(2)])

        wci = [0]

        def load_w(srcs):
            ws = wst.next()
            wb = wbf.next()
            c0 = 0
            for ap in srcs:
                nco = ap.shape[1]
                kb.dma("sp", ws[:, :, c0:c0 + nco], ap.rearrange("(kc p) n -> p kc n", p=128),
                       [], [ws])
                c0 += nco
            wci[0] += 1
            if wci[0] % 2:
                kb.op("act", [ws], [wb], lambda e, ws=ws, wb=wb, c0=c0: e.copy(
                    wb[:, :, 0:c0], ws[:, :, 0:c0]))
            else:
                kb.op("dve", [ws], [wb], lambda e, ws=ws, wb=wb, c0=c0: e.tensor_copy(
                    wb[:, :, 0:c0], ws[:, :, 0:c0]))
            return wb

        evi = [0]

        def evac(ps, out_ap, out_trks, src_ap, extra_reads=()):
            evi[0] += 1
            if evi[0] % 2:
                kb.op("act", [ps] + list(extra_reads), out_trks, lambda e: e.copy(out_ap, src_ap))
            else:
                kb.op("dve", [ps] + list(extra_reads), out_trks, lambda e: e.tensor_copy(out_ap, src_ap))

        ostage = Ring([kb.sb("ost%d" % i, [128, T], BF16) for i in range(2)])

        for p in range(4):
            wb = load_w([w_in[:, p * 512:(p + 1) * 512]])
            for cc in range(4):
                ch = p * 4 + cc
                og = ostage.next()
                for n in range(NB):
                    ps = psr.next()
                    mm_group(kb, ps, ps[:, :],
                             [wb[:, kc, cc * 128:(cc + 1) * 128] for kc in range(NKC)],
                             [hT[:, kc, 128 + 512 * n:128 + 512 * (n + 1)] for kc in range(NKC)],
                             [wb] + hT_blk(n))
                    evac(ps, og[:, 512 * n:512 * (n + 1)], [og], ps[:, :])
                dst = qT if ch < 8 else kT
                r0 = (ch % 8) * 128
                kb.dma("pool", dst[r0:r0 + 128, :], og[:, :], [og], [OUT])

        vstage = Ring([kb.sb("vst%d" % i, [128, 512], BF16) for i in range(3)])
        for p in range(2):
            wb = load_w([w_in[:, 2048 + p * 512:2048 + (p + 1) * 512]])
            for i in range(NT):
                ps = psr.next()
                mm_group(kb, ps, ps[:, :],
                         [hT[:, kc, 128 + 128 * i:128 + 128 * (i + 1)] for kc in range(NKC)],
                         [wb[:, kc, :] for kc in range(NKC)],
                         [wb, hT.trk(1 + i)])
                vs = vstage.next()
                evac(ps, vs[:, :], [vs], ps[:, :])
                kb.dma("pool", v[i * 128:(i + 1) * 128, p * 512:(p + 1) * 512], vs[:, :], [vs], [OUT])

        NH = 2 if T >= 1024 else 1
        TH2 = T // NH
        NB2 = TH2 // 512
        gT = kb.sb("gT", [128, NKC, TH2], BF16)
        uT = kb.sb("uT", [128, TH2 + 2], F32)
        cbT = kb.sb("cbT", [128, TH2], F32)
        yacc = kb.sb("yacc", [128, TH2], F32)
        tmpr = Ring([kb.sb("tmp%d" % i, [128, 512], F32) for i in range(2)])
        for hf in range(NH):
            t0 = hf * TH2
            for j in range(8):
                wb = load_w([w_in[:, 3072 + 128 * j:3072 + 128 * (j + 1)],
                             w_in[:, 4096 + 128 * j:4096 + 128 * (j + 1)],
                             w_in[:, 5120 + 128 * j:5120 + 128 * (j + 1)]])
                ps = psr.next()
                hc = 128 + t0
                htr = [hT.trk(0)] if hf == 0 else [hT.trk((hc - 2) // 128)]

                def halo_mm(e, ps=ps, wb=wb, hc=hc):
                    ins = None
                    for g in range(2):
                        for kc in range(NKC):
                            ins = e.matmul(ps[:, 2 * g:2 * g + 2], wb[:, kc, 128 * (g + 1):128 * (g + 2)],
                                           hT[:, kc, hc - 2:hc], start=(kc == 0), stop=(kc == NKC - 1))
                    return ins
                kb.op("pe", [wb] + htr, [ps], halo_mm)
                tm = tmpr.next()
                kb.op("act", [ps], [tm], lambda e, ps=ps, tm=tm: e.copy(tm[:, 0:2], ps[:, 0:2]))
                kb.op("dve", [ps, tm], [tm], lambda e, ps=ps, tm=tm: e.tensor_tensor(
                    tm[:, 2:4], tm[:, 0:2], ps[:, 2:4], ALU.mult))
                if hf == 0:
                    kb.op("dve", [tm, hm], [uT], lambda e, tm=tm: e.tensor_scalar(
                        uT[:, 0:2], tm[:, 2:4], hm[:, 0:1], None, ALU.mult))
                else:
                    kb.op("dve", [tm], [uT], lambda e, tm=tm: e.tensor_copy(uT[:, 0:2], tm[:, 2:4]))
                for n2 in range(NB2):
                    n = hf * NB2 + n2
                    rhs = [hT[:, kc, 128 + 512 * n:128 + 512 * (n + 1)] for kc in range(NKC)]
                    pcb, pcc, pcx = psr.next(), psr.next(), psr.next()
                    for g, ps in enumerate((pcb, pcc, pcx)):
                        mm_group(kb, ps, ps[:, :], [wb[:, kc, 128 * g:128 * (g + 1)] for kc in range(NKC)],
                                 rhs, [wb] + hT_blk(n))
                    tm = tmpr.next()
                    kb.op("act", [pcc], [tm], lambda e, pcc=pcc, tm=tm: e.copy(tm[:, :], pcc[:, :]))
                    kb.op("dve", [tm, pcx], [uT], lambda e, tm=tm, pcx=pcx, n2=n2: e.tensor_tensor(
                        uT[:, 2 + 512 * n2:2 + 512 * (n2 + 1)], tm[:, :], pcx[:, :], ALU.mult))
                    kb.op("act", [pcb], [cbT], lambda e, pcb=pcb, n2=n2: e.copy(
                        cbT[:, 512 * n2:512 * (n2 + 1)], pcb[:, :]))
                kb.op("dve", [uT, cw], [yacc], lambda e, j=j: e.tensor_scalar(
                    yacc[:, :], uT[:, 2:TH2 + 2], cw[:, 16 + j:17 + j], None, ALU.mult))
                kb.op("dve", [uT, cw, yacc], [yacc], lambda e, j=j: e.scalar_tensor_tensor(
                    yacc[:, :], uT[:, 1:TH2 + 1], cw[:, 8 + j:9 + j], yacc[:, :], ALU.mult, ALU.add))
                kb.op("dve", [uT, cw, yacc], [yacc], lambda e, j=j: e.scalar_tensor_tensor(
                    yacc[:, :], uT[:, 0:TH2], cw[:, j:j + 1], yacc[:, :], ALU.mult, ALU.add))
                kb.op("dve", [yacc, cbT], [gT.trk(j)], lambda e, j=j: e.tensor_tensor(
                    gT[:, j, :], yacc[:, :], cbT[:, :], ALU.mult))

            gT_all = [gT.trk(j) for j in range(8)]
            for j in range(8):
                wb = load_w([w_in[:, 7168 + 128 * j:7168 + 128 * (j + 1)], w_cp[:, 128 * j:128 * (j + 1)]])
                og = ostage.next()
                for n2 in range(NB2):
                    n = hf * NB2 + n2
                    pyc, pgc = psr.next(), psr.next()
                    mm_group(kb, pyc, pyc[:, :], [wb[:, kc, 128:256] for kc in range(NKC)],
                             [gT[:, kc, 512 * n2:512 * (n2 + 1)] for kc in range(NKC)], [wb] + gT_all)
                    mm_group(kb, pgc, pgc[:, :], [wb[:, kc, 0:128] for kc in range(NKC)],
                             [hT[:, kc, 128 + 512 * n:128 + 512 * (n + 1)] for kc in range(NKC)],
                             [wb] + hT_blk(n))
                    tm = tmpr.next()
                    kb.op("act", [pgc, gb], [tm], lambda e, pgc=pgc, tm=tm, j=j: e.activation(
                        tm[:, :], pgc[:, :], AF.Sigmoid, bias=gb[:, 8 + j:9 + j]))
                    kb.op("dve", [tm, pyc], [og], lambda e, tm=tm, pyc=pyc, og=og, n2=n2: e.tensor_tensor(
                        og[:, 512 * n2:512 * (n2 + 1)], tm[:, :], pyc[:, :], ALU.mult))
                kb.dma("pool", YT[128 * j:128 * (j + 1), t0:t0 + TH2], og[:, 0:TH2], [og], [OUT])

        for p in range(2):
            wb = load_w([w_in[:, 6144 + p * 512:6144 + (p + 1) * 512]])
            for cc in range(4):
                j = p * 4 + cc
                og = ostage.next()
                for n in range(NB):
                    ps = psr.next()
                    mm_group(kb, ps, ps[:, :],
                             [wb[:, kc, cc * 128:(cc + 1) * 128] for kc in range(NKC)],
                             [hT[:, kc, 128 + 512 * n:128 + 512 * (n + 1)] for kc in range(NKC)],
                             [wb] + hT_blk(n))
                    kb.op("act", [ps, gb], [og], lambda e, ps=ps, og=og, n=n, j=j: e.activation(
                        og[:, 512 * n:512 * (n + 1)], ps[:, :], AF.Sigmoid, bias=gb[:, j:j + 1]))
                kb.dma("pool", SaT[128 * j:128 * (j + 1), :], og[:, :], [og], [OUT])
        kb.finish()
    return nc


FL = 1151
FP = FL + 1


def rel_bucket_np(rel):
    n = np.maximum(rel, 0)
    nf = np.maximum(n, 1).astype(np.float32)
    large = 16 + (np.log(nf / np.float32(16)) / np.float32(math.log(128 / 16)) * np.float32(16)).astype(np.int32)
    large = np.minimum(large, 31)
    return np.where(n < 16, n, large)


def bias_onehot():
    rel = np.arange(FL) - 511
    b = rel_bucket_np(rel)
    oh = np.zeros((33, FL), np.float32)
    for i in range(FL):
        if rel[i] < 0:
            oh[32, i] = 1.0
        else:
            oh[b[i], i] = 1.0
    return oh


def build_ATT(S):
    nc = bass.Bass("TRN2", target_bir_lowering=False)
    NQB = S // 512
    NKCH = S // 128
    with ExitStack() as st:
        kb = KB(nc, st)
        dt = nc.dram_tensor
        qT = dt("qT", [256, S], BF16, kind="ExternalInput").ap()
        kT = dt("kT", [256, S], BF16, kind="ExternalInput").ap()
        v = dt("v", [S, 256], BF16, kind="ExternalInput").ap()
        oh = dt("oh", [33, FL], F32, kind="ExternalInput").ap()
        tab = dt("tab", [33, 2], F32, kind="ExternalInput").ap()
        lamv = dt("lamv", [128, 256], F32, kind="ExternalInput").ap()
        subg = dt("subg", [128, 128], F32, kind="ExternalInput").ap()
        lamc_d = dt("lamc", [128, 2], F32, kind="ExternalInput").ap()
        ident = dt("ident", [128, 128], F32, kind="ExternalInput").ap()
        oT = dt("oT", [256, S], BF16, kind="ExternalOutput").ap()
        fscr = [nc.dram_tensor("fscr%d" % h, [128 * FP], F32) for h in range(2)]
        OUT = kb.out_trk

        identf = load_const(kb, "identf", ident, [128, 128])
        identb = kb.sb("identb", [128, 128], BF16)
        kb.op("dve", [identf], [identb], lambda e: e.tensor_copy(identb[:, :], identf[:, :]))
        ohs = load_const(kb, "ohs", oh, [33, FL])
        tabs = load_const(kb, "tabs", tab, [33, 2])
        lv = load_const(kb, "lv", lamv, [128, 256])
        g_rep = load_const(kb, "g_rep", subg, [128, 128])
        lamc = load_const(kb, "lamc_s", lamc_d, [128, 2])
        kb.op("dve", [g_rep, lamc], [g_rep], lambda e: e.tensor_scalar(
            g_rep[:, :], g_rep[:, :], lamc[:, 1:2], None, ALU.mult))
        epst = kb.sb("epst", [128, 1], F32)
        kb.op("dve", [], [epst], lambda e: e.memset(epst[:, :], LN_EPS))

        lt = kb.sb("lt", [128, 128], F32)
        ls = kb.sb("ls", [128, 2], F32)
        nlam = kb.sb("nlam", [128, 1], F32)
        kb.op("dve", [lv], [lt], lambda e: e.tensor_tensor(
            lt[:, 0:64], lv[:, 0:64], lv[:, 64:128], ALU.mult))
        kb.op("dve", [lv, lt], [lt], lambda e: e.tensor_tensor(
            lt[:, 64:128], lv[:, 128:192], lv[:, 192:256], ALU.mult))
        kb.op("dve", [lt], [ls], lambda e: e.reduce_sum(
            ls[:, 0:2], lt[:, :].rearrange("p (a b) -> p a b", a=2), axis=AX.X))
        kb.op("act", [ls], [ls], lambda e: e.activation(ls[:, :], ls[:, :], AF.Exp))
        kb.op("dve", [ls], [nlam], lambda e: e.tensor_tensor(
            nlam[:, :], ls[:, 1:2], ls[:, 0:1], ALU.subtract))
        kb.op("dve", [nlam, lamc], [nlam], lambda e: e.tensor_scalar(
            nlam[:, :], nlam[:, :], lamc[:, 0:1], None, ALU.add))

        s12 = Ring([kb.ps("s12_%d" % i, [128, 1024], F32) for i in range(2)])
        oacc = kb.ps("oacc", [128, 3, 512], F32)
        pT = kb.ps("pT", [128, 4, 128], BF16)

        def acc_ap(h2, qc):
            g = h2 * 4 + qc
            return oacc[:, g // 3, (g % 3) * 129:(g % 3) * 129 + 129]

        kTs = kb.sb("kTs", [128, S], BF16)
        vaug = kb.sb("vaug", [128, NKCH, 129], BF16)
        tabrep = kb.sb("tabrep", [33, 128], F32)
        frep = kb.sb("frep", [128, FL], F32)
        c31 = kb.sb("c31", [128, 1], F32)
        biasT = [kb.sb("biasT%d" % d, [128, 512], F32) for d in range(5)]
        qr = Ring([kb.sb("qb%d" % i, [128, 512], BF16) for i in range(3)])
        pr = Ring([kb.sb("pb%d" % i, [128, 1024], BF16) for i in range(3)])
        tmpr = Ring([kb.sb("tm%d" % i, [128, 1024], F32) for i in range(2)])
        osb = Ring([kb.sb("osb%d" % i, [128, 8, 132], F32) for i in range(2)])
        ostage = Ring([kb.sb("ostg%d" % i, [128, 512], BF16) for i in range(2)])
        ofr = Ring([kb.sb("of%d" % i, [128, 4, 128], F32) for i in range(3)])
        onr = Ring([kb.sb("on%d" % i, [128, 4, 128], BF16) for i in range(2)])
        smr = Ring([kb.sb("sm%d" % i, [128, 16], F32) for i in range(3)])
        junk = kb.sb("junk", [128, 128], F32)

        for hh in range(2):
            kb.dma("sp", kTs[:, :], kT[hh * 128:(hh + 1) * 128, :], [], [kTs])
            kb.dma("sp", vaug[:, :, 0:128],
                   v[:, hh * 128:(hh + 1) * 128].rearrange("(c p) e -> p c e", p=128), [], [vaug])
            kb.op("pool", [], [vaug], lambda e: e.memset(vaug[:, :, 128:129], 1.0))
            kb.op("dve", [tabs], [tabrep], lambda e, hh=hh: e.tensor_copy(
                tabrep[:, :], tabs[:, hh:hh + 1].to_broadcast([33, 128])))
            ps = s12.next()
            ps2 = s12.next()

            def fmm(e, ps=ps, ps2=ps2):
                e.matmul(ps[:, 0:512], tabrep[:, :], ohs[:, 0:512], start=True, stop=True)
                e.matmul(ps[:, 512:1024], tabrep[:, :], ohs[:, 512:1024], start=True, stop=True)
                return e.matmul(ps2[:, 0:FL - 1024], tabrep[:, :], ohs[:, 1024:FL], start=True, stop=True)
            kb.op("pe", [tabrep, ohs], [ps, ps2], fmm)
            kb.op("dve", [ps], [frep], lambda e, ps=ps: e.tensor_copy(frep[:, 0:1024], ps[:, :]))
            kb.op("dve", [ps2, frep], [frep], lambda e, ps2=ps2: e.tensor_copy(
                frep[:, 1024:FL], ps2[:, 0:FL - 1024]))
            kb.op("dve", [frep], [c31], lambda e: e.tensor_copy(c31[:, :], frep[:, FL - 1:FL]))
            ftrk = Trk()
            kb.dma("sp", bass.AP(fscr[hh], 0, [[FP, 128], [1, FL]]), frep[:, :], [frep], [ftrk])
            for d in range(5):
                delta = -128 + 128 * d
                kb.dma("sp", biasT[d][:, :], bass.AP(fscr[hh], 511 - delta, [[FL, 128], [1, 512]]),
                       [ftrk], [biasT[d]])

            steps = [(I, j) for I in range(NQB) for j in range(4 * I + 4)]
            qblk = {}
            deferred = []

            def get_q(I):
                if I not in qblk:
                    qb = qr.next()
                    kb.dma("sp", qb[:, :], qT[hh * 128:(hh + 1) * 128, 512 * I:512 * (I + 1)], [], [qb])
                    qblk[I] = qb
                return qblk[I]

            def qk(I, j):
                qb = get_q(I)
                ps = s12.next()

                def fn(e, ps=ps, qb=qb, j=j):
                    e.matmul(ps[:, 0:512], kTs[0:64, 128 * j:128 * (j + 1)], qb[0:64, :],
                             start=True, stop=True)
                    return e.matmul(ps[:, 512:1024], kTs[64:128, 128 * j:128 * (j + 1)], qb[64:128, :],
                                    start=True, stop=True)
                kb.op("pe", [kTs, qb], [ps], fn)
                return ps

            pend = [qk(*steps[k]) for k in range(min(2, len(steps)))]
            for si, (I, j) in enumerate(steps):
                cur = pend.pop(0)
                pb = pr.next()
                if j <= 4 * I - 2:
                    kb.op("act", [cur, c31], [pb], lambda e, cur=cur, pb=pb: e.activation(
                        pb[:, :], cur[:, :], AF.Exp, bias=c31[:, 0:1], scale=0.125))
                else:
                    d = j - (4 * I - 1)
                    bt = biasT[d]
                    tm = tmpr.next()
                    for h2 in range(2):
                        kb.op("dve", [cur, bt], [tm], lambda e, cur=cur, tm=tm, bt=bt, h2=h2: e.scalar_tensor_tensor(
                            tm[:, 512 * h2:512 * (h2 + 1)], cur[:, 512 * h2:512 * (h2 + 1)], 0.125, bt[:, :],
                            ALU.mult, ALU.add))
                    kb.op("act", [tm], [pb], lambda e, tm=tm, pb=pb: e.activation(
                        pb[:, :], tm[:, :], AF.Exp))
                last = (j == 4 * I + 3)
                if si + 2 < len(steps):
                    pend.append(qk(*steps[si + 2]))

                def av(e, pb=pb, j=j, last=last):
                    ins = None
                    for h2 in range(2):
                        for qc in range(4):
                            g = h2 * 4 + qc
                            ins = e.matmul(acc_ap(h2, qc), pb[:, 512 * h2 + 128 * qc:512 * h2 + 128 * (qc + 1)],
                                           vaug[:, j, :], start=(j == 0 and g % 3 == 0), stop=last,
                                           skip_group_check=True)
                    return ins
                kb.op("pe", [pb, vaug], [oacc], av)
                if last:
                    ob = osb.next()
                    for bnk in range(3):
                        ng = 3 if bnk < 2 else 2
                        kb.op("dve", [oacc], [ob], lambda e, ob=ob, bnk=bnk, ng=ng: e.tensor_copy(
                            ob[:, 3 * bnk:3 * bnk + ng, 0:129],
                            oacc[:, bnk, 0:ng * 129].rearrange("p (g c) -> p g c", g=ng)))
                    rl = smr.next()
                    of = ofr.next()
                    kb.op("dve", [ob], [rl], lambda e, ob=ob, rl=rl: e.reciprocal(
                        rl[:, 0:8].unsqueeze(2), ob[:, 0:8, 128:129]))
                    kb.op("dve", [rl, nlam], [rl], lambda e, rl=rl: e.tensor_scalar(
                        rl[:, 4:8], rl[:, 4:8], nlam[:, 0:1], None, ALU.mult))
                    kb.op("dve", [ob, rl], [of], lambda e, ob=ob, rl=rl, of=of: e.tensor_tensor(
                        of[:, :, :], ob[:, 0:4, 0:128], rl[:, 0:4].unsqueeze(2).broadcast_to([128, 4, 128]),
                        ALU.mult))
                    kb.op("dve", [ob, rl], [ob], lambda e, ob=ob, rl=rl: e.tensor_tensor(
                        ob[:, 4:8, 0:128], ob[:, 4:8, 0:128],
                        rl[:, 4:8].unsqueeze(2).broadcast_to([128, 4, 128]), ALU.mult))
                    kb.op("dve", [ob, of], [of], lambda e, ob=ob, of=of: e.tensor_tensor(
                        of[:, :, :], of[:, :, :], ob[:, 4:8, 0:128], ALU.add))
                    kb.op("dve", [of], [ob], lambda e, ob=ob, of=of: e.tensor_tensor(
                        ob[:, 0:4, 0:128], of[:, :, :], of[:, :, :], ALU.mult))
                    kb.op("dve", [ob], [rl], lambda e, ob=ob, rl=rl: e.reduce_sum(
                        rl[:, 8:12], ob[:, 0:4, 0:128], axis=AX.X))

                    def e2(rl=rl):
                        kb.op("act", [rl, epst], [rl], lambda e: e.activation(
                            rl[:, 12:16], rl[:, 8:12], AF.Ln, bias=epst[:, 0:1], scale=1.0 / 128))
                        kb.op("act", [rl], [rl], lambda e: e.activation(
                            rl[:, 8:12], rl[:, 12:16], AF.Exp, scale=-0.5))

                    def e3(rl=rl, of=of, I=I):
                        on = onr.next()
                        og = ostage.next()
                        kb.op("dve", [of, rl], [of], lambda e: e.tensor_tensor(
                            of[:, :, :], of[:, :, :], rl[:, 8:12].unsqueeze(2).broadcast_to([128, 4, 128]),
                            ALU.mult))
                        kb.op("dve", [of, g_rep], [on], lambda e: e.tensor_tensor(
                            on[:, :, :], of[:, :, :], g_rep[:, :].unsqueeze(1).broadcast_to([128, 4, 128]),
                            ALU.mult))

                        def trq(e):
                            ins = None
                            for qc in range(4):
                                ins = e.transpose(pT[:, qc, :], on[:, qc, :], identb[:, :])
                            return ins
                        kb.op("pe", [on, identb], [pT], trq)
                        kb.op("dve", [pT], [og], lambda e: e.tensor_copy(
                            og[:, :].rearrange("p (a b) -> p a b", a=4), pT[:, :, :]))
                        kb.dma("sp", oT[hh * 128:(hh + 1) * 128, 512 * I:512 * (I + 1)], og[:, :], [og], [OUT])
                    deferred.append((si + 3, e2))
                    deferred.append((si + 6, e3))
                while deferred and deferred[0][0] <= si:
                    deferred.pop(0)[1]()
            while deferred:
                deferred.pop(0)[1]()
        kb.finish()
    return nc


def load_w_resident(kb, name, w_ap, ncols, wst):
    wb = kb.sb(name, [128, NKC, ncols], BF16)
    for p in range(0, ncols, 512):
        nco = min(512, ncols - p)
        ws = wst.next()
        kb.dma("sp", ws[:, :, 0:nco], w_ap[:, p:p + nco].rearrange("(kc p) n -> p kc n", p=128), [], [ws])
        kb.op("pool", [ws], [wb], lambda e, ws=ws, p=p, nco=nco: e.tensor_copy(
            wb[:, :, p:p + nco], ws[:, :, 0:nco]))
    return wb


def build_C1(T):
    nc = bass.Bass("TRN2", target_bir_lowering=False)
    NB = T // 512
    with ExitStack() as st:
        kb = KB(nc, st)
        dt = nc.dram_tensor
        oT = dt("oT", [D, T], BF16, kind="ExternalInput").ap()
        SaT = dt("SaT", [D, T], BF16, kind="ExternalInput").ap()
        YT = dt("YT", [D, T], BF16, kind="ExternalInput").ap()
        h = dt("h", [T, D], F32, kind="ExternalInput").ap()
        w_ap = dt("w_ap", [D, D], F32, kind="ExternalInput").ap()
        w_out = dt("w_out", [D, D], F32, kind="ExternalInput").ap()
        lng = dt("lng", [128, D], F32, kind="ExternalInput").ap()
        lnb = dt("lnb", [128, D], F32, kind="ExternalInput").ap()
        h1 = dt("h1", [T, D], F32, kind="ExternalOutput").ap()
        OUT = kb.out_trk
        g_rep = load_const(kb, "g_rep", lng, [128, D])
        b_rep = load_const(kb, "b_rep", lnb, [128, D])
        wst = Ring([kb.sb("wst%d" % i, [128, NKC, 512], F32) for i in range(2)])
        wap_b = load_w_resident(kb, "wap_b", w_ap, D, wst)
        wout_b = load_w_resident(kb, "wout_b", w_out, D, wst)
        psr = PsumRing(kb, 6)
        obr = Ring([kb.sb("ob%d" % i, [128, NKC, 512], BF16) for i in range(2)])
        sar = Ring([kb.sb("sa%d" % i, [128, NKC, 512], BF16) for i in range(2)])
        yr = Ring([kb.sb("yb%d" % i, [128, NKC, 512], BF16) for i in range(2)])
        mbr = Ring([kb.sb("mb%d" % i, [128, NKC, 512], BF16) for i in range(2)])
        tmr = Ring([kb.sb("tm%d" % i, [128, 512], F32) for i in range(3)])
        xr = Ring([kb.sb("x%d" % i, [128, D], F32) for i in range(3)])
        st6 = kb.sb("st6", [128, 12], F32)
        mv = kb.sb("mv", [128, 2], F32)
        rstd = kb.sb("rstd", [128, 1], F32)
        for n in range(NB):
            ob, sa, yb, mb = obr.next(), sar.next(), yr.next(), mbr.next()
            sl = slice(512 * n, 512 * (n + 1))
            kb.dma("sp", ob[:, :, :], oT[:, sl].rearrange("(c p) t -> p c t", p=128), [], [ob])
            kb.dma("sp", sa[:, :, :], SaT[:, sl].rearrange("(c p) t -> p c t", p=128), [], [sa])
            kb.dma("sp", yb[:, :, :], YT[:, sl].rearrange("(c p) t -> p c t", p=128), [], [yb])
            for j in range(8):
                ps = psr.next()
                mm_group(kb, ps, ps[:, :], [wap_b[:, kc, 128 * j:128 * (j + 1)] for kc in range(NKC)],
                         [ob[:, kc, :] for kc in range(NKC)], [wap_b, ob])
                tm = tmr.next()
                kb.op("dve", [ps, sa], [tm], lambda e, ps=ps, sa=sa, tm=tm, j=j: e.tensor_tensor(
                    tm[:, :], ps[:, :], sa[:, j, :], ALU.mult))
                kb.op("pool", [tm, yb], [mb], lambda e, tm=tm, yb=yb, mb=mb, j=j: e.tensor_tensor(
                    mb[:, j, :], tm[:, :], yb[:, j, :], ALU.add))
            for a in range(4):
                i = 4 * n + a
                x = xr.next()
                kb.dma("sp", x[:, :], h[i * 128:(i + 1) * 128, :], [], [x])
                for half in range(2):
                    ps = psr.next()
                    mm_group(kb, ps, ps[:, :], [mb[:, kc, 128 * a:128 * (a + 1)] for kc in range(NKC)],
                             [wout_b[:, kc, 512 * half:512 * (half + 1)] for kc in range(NKC)], [wout_b, mb])
                    kb.op("dve", [ps, x], [x], lambda e, ps=ps, x=x, half=half: e.scalar_tensor_tensor(
                        x[:, 512 * half:512 * (half + 1)], x[:, 512 * half:512 * (half + 1)], DN_ALPHA,
                        ps[:, :], ALU.mult, ALU.add))
                layer_norm_tile(kb, x, x, g_rep, b_rep, st6, mv, rstd)
                kb.dma("act", h1[i * 128:(i + 1) * 128, :], x[:, :], [x], [OUT])
        kb.finish()
    return nc


def build_C2(T):
    nc = bass.Bass("TRN2", target_bir_lowering=False)
    NT = T // 128
    NBLK = (2 * T + 32 * (MOE_BS - 1) + MOE_BS - 1) // MOE_BS
    NSUB = MOE_BS // 128
    with ExitStack() as st:
        kb = KB(nc, st)
        dt = nc.dram_tensor
        h1 = dt("h1", [T, D], F32, kind="ExternalInput").ap()
        w_r = dt("w_r", [D, 36], F32, kind="ExternalInput").ap()
        b_r = dt("b_r", [128, 36], F32, kind="ExternalInput").ap()
        w1 = dt("w1", [32, D, 512], F32, kind="ExternalInput").ap()
        w3 = dt("w3", [32, D, 512], F32, kind="ExternalInput").ap()
        w2 = dt("w2", [32, 512, D], F32, kind="ExternalInput").ap()
        lng = dt("lng", [128, D], F32, kind="ExternalInput").ap()
        lnb = dt("lnb", [128, D], F32, kind="ExternalInput").ap()
        ident = dt("ident", [128, 128], F32, kind="ExternalInput").ap()
        utri = dt("utri", [128, 128], F32, kind="ExternalInput").ap()
        blk128 = dt("blk128", [128, NBLK], F32, kind="ExternalInput").ap()
        pidx_d = dt("pidx", [128, 1], F32, kind="ExternalInput").ap()
        h2 = dt("h2", [T, D], F32, kind="ExternalOutput").ap()
        xbuf = nc.dram_tensor("xbuf", [NBLK * MOE_BS, D], BF16).ap()
        ybuf = nc.dram_tensor("ybuf", [NBLK * MOE_BS, D], F32).ap()
        OUT = kb.out_trk

        g_rep = load_const(kb, "g_rep", lng, [128, D])
        b_rep = load_const(kb, "b_rep", lnb, [128, D])
        identf = load_const(kb, "identf", ident, [128, 128])
        identb = kb.sb("identb", [128, 128], BF16)
        kb.op("dve", [identf], [identb], lambda e: e.tensor_copy(identb[:, :], identf[:, :]))
        utf = load_const(kb, "utf", utri, [128, 128])
        utb = kb.sb("utb", [128, 128], BF16)
        kb.op("dve", [utf], [utb], lambda e: e.tensor_copy(utb[:, :], utf[:, :]))
        onesb = kb.sb("onesb", [128, 128], BF16)
        kb.op("dve", [], [onesb], lambda e: e.memset(onesb[:, :], 1.0))
        b128 = load_const(kb, "b128", blk128, [128, NBLK])
        brs = load_const(kb, "brs", b_r, [128, 36])
        wrf = kb.sb("wrf", [128, NKC, 36], F32)
        kb.dma("sp", wrf[:, :, :], w_r.rearrange("(kc p) n -> p kc n", p=128), [], [wrf])
        wrb = kb.sb("wrb", [128, NKC, 36], BF16)
        kb.op("dve", [wrf], [wrb], lambda e: e.tensor_copy(wrb[:, :, :], wrf[:, :, :]))

        zt = kb.sb("zt", [128, D], BF16)
        kb.op("pool", [], [zt], lambda e: e.memset(zt[:, :], 0.0))
        xz = Trk()
        for bq in range(NBLK * NSUB):
            kb.dma("act", xbuf[bq * 128:(bq + 1) * 128, :], zt[:, :], [zt], [xz])

        psr = PsumRing(kb, 6)
        pT = kb.ps("pT", [128, NKC, 128], BF16)
        pT2 = kb.ps("pT2", [128, 4, 128], BF16)
        xr = Ring([kb.sb("x%d" % i, [128, D], F32) for i in range(3)])
        hbr = Ring([kb.sb("hb%d" % i, [128, D], BF16) for i in range(2)])
        hTr = Ring([kb.sb("hTt%d" % i, [128, NKC, 128], BF16) for i in range(2)])
        st6 = kb.sb("st6", [128, 12], F32)
        mv = kb.sb("mv", [128, 2], F32)
        rstd = kb.sb("rstd", [128, 1], F32)

        M1 = kb.sb("M1", [128, NT, 32], F32)
        M2 = kb.sb("M2", [128, NT, 32], F32)
        Mb = kb.sb("Mb", [128, NT, 32], BF16)
        rank = kb.sb("rank", [128, NT, 32], F32)
        gates = kb.sb("gates", [128, NT, 2], F32)
        destf = kb.sb("destf", [128, NT * 2], F32)
        desti = kb.sb("desti", [128, NT * 2], I32)
        lgr = Ring([kb.sb("lg%d" % i, [128, 36], F32) for i in range(2)])
        smr = Ring([kb.sb("smr%d" % i, [128, 48], F32) for i in range(2)])

        for i in range(NT):
            x = xr.next()
            hb = hbr.next()
            hTt = hTr.next()
            kb.dma("sp", x[:, :], h1[i * 128:(i + 1) * 128, :], [], [x])
            kb.op("act", [x], [hb], lambda e, x=x, hb=hb: e.copy(hb[:, :], x[:, :]))
            transpose_to(kb, hb, identb, pT, hTt[:, :, :], [hTt], NKC, eng="act")
            ps = psr.next()
            mm_group(kb, ps, ps[:, 0:36], [hTt[:, kc, :] for kc in range(NKC)],
                     [wrb[:, kc, :] for kc in range(NKC)], [hTt, wrb])
            lg = lgr.next()
            sm = smr.next()
            m1 = M1.trk(i)
            m2 = M2.trk(i)
            kb.op("dve", [ps, brs], [lg], lambda e, ps=ps, lg=lg: e.tensor_tensor(
                lg[:, :], ps[:, 0:36], brs[:, :], ALU.add))
            kb.op("dve", [lg], [sm], lambda e, lg=lg, sm=sm: e.reduce_max(sm[:, 0:1], lg[:, 0:4], axis=AX.X))
            kb.op("dve", [sm], [sm], lambda e, sm=sm: e.tensor_scalar(
                sm[:, 1:2], sm[:, 0:1], -1.0, None, ALU.mult))
            kb.op("act", [lg, sm], [sm], lambda e, lg=lg, sm=sm: e.activation(
                sm[:, 44:48], lg[:, 0:4], AF.Exp, bias=sm[:, 1:2], accum_out=sm[:, 2:3]))
            kb.op("dve", [sm], [sm], lambda e, sm=sm: e.reciprocal(sm[:, 3:4], sm[:, 2:3]))
            kb.op("dve", [lg, sm], [sm], lambda e, lg=lg, sm=sm: e.tensor_scalar(
                sm[:, 4:8], lg[:, 0:4], sm[:, 0:1], None, ALU.is_equal))
            kb.op("dve", [lg, sm], [sm], lambda e, lg=lg, sm=sm: e.tensor_scalar(
                sm[:, 8:16], lg[:, 4:12], sm[:, 4:5], None, ALU.mult))
            for g in range(1, 4):
                kb.op("dve", [lg, sm], [sm], lambda e, lg=lg, sm=sm, g=g: e.scalar_tensor_tensor(
                    sm[:, 8:16], lg[:, 4 + 8 * g:12 + 8 * g], sm[:, 4 + g:5 + g], sm[:, 8:16], ALU.mult, ALU.add))
            kb.op("dve", [sm], [sm], lambda e, sm=sm: e.max(sm[:, 16:24], sm[:, 8:16]))
            kb.op("dve", [sm], [sm], lambda e, sm=sm: e.tensor_scalar(
                sm[:, 24:32], sm[:, 8:16], sm[:, 16:17], None, ALU.is_equal))
            kb.op("dve", [sm], [sm], lambda e, sm=sm: e.tensor_scalar(
                sm[:, 32:40], sm[:, 8:16], sm[:, 17:18], None, ALU.is_equal))
            kb.op("dve", [sm], [sm], lambda e, sm=sm: e.tensor_tensor(
                sm[:, 40:41], sm[:, 17:18], sm[:, 16:17], ALU.subtract))
            kb.op("act", [sm], [sm], lambda e, sm=sm: e.activation(sm[:, 40:41], sm[:, 40:41], AF.Exp))
            kb.op("dve", [sm], [sm], lambda e, sm=sm: e.tensor_scalar(
                sm[:, 41:42], sm[:, 40:41], 1.0, None, ALU.add))
            kb.op("dve", [sm], [sm], lambda e, sm=sm: e.reciprocal(sm[:, 41:42], sm[:, 41:42]))
            kb.op("dve", [sm], [gates.trk(i)], lambda e, sm=sm, i=i: e.tensor_tensor(
                gates[:, i, 0:1], sm[:, 3:4], sm[:, 41:42], ALU.mult))
            kb.op("dve", [sm, gates.trk(i)], [gates.trk(i)], lambda e, sm=sm, i=i: e.tensor_tensor(
                gates[:, i, 1:2], gates[:, i, 0:1], sm[:, 40:41], ALU.mult))
            for g in range(4):
                kb.op("dve", [sm], [m1], lambda e, sm=sm, i=i, g=g: e.tensor_scalar(
                    M1[:, i, 8 * g:8 * g + 8], sm[:, 24:32], sm[:, 4 + g:5 + g], None, ALU.mult))
                kb.op("dve", [sm], [m2], lambda e, sm=sm, i=i, g=g: e.tensor_scalar(
                    M2[:, i, 8 * g:8 * g + 8], sm[:, 32:40], sm[:, 4 + g:5 + g], None, ALU.mult))
            kb.op("dve", [m1, m2], [Mb.trk(i)], lambda e, i=i: e.tensor_tensor(
                Mb[:, i, :], M1[:, i, :], M2[:, i, :], ALU.add))

        maccr = Ring([kb.sb("macc%d" % i, [128, 32], BF16) for i in range(2)])
        macc = maccr.next()
        kb.op("dve", [], [macc], lambda e, macc=macc: e.memset(macc[:, :], 0.0))
        for i in range(NT):
            ps = psr.next()

            def rk(e, ps=ps, i=i, macc=macc):
                e.matmul(ps[:, 0:32], utb[:, :], Mb[:, i, :], start=True, stop=False)
                return e.matmul(ps[:, 0:32], onesb[:, :], macc[:, :], start=False, stop=True)
            kb.op("pe", [utb, onesb, Mb.trk(i), macc], [ps], rk)
            kb.op("act", [ps], [rank.trk(i)], lambda e, ps=ps, i=i: e.copy(rank[:, i, :], ps[:, 0:32]))
            nm = maccr.next()
            kb.op("dve", [macc, Mb.trk(i)], [nm], lambda e, macc=macc, nm=nm, i=i: e.tensor_tensor(
                nm[:, :], macc[:, :], Mb[:, i, :], ALU.add))
            macc = nm
        ps = psr.next()
        kb.op("pe", [onesb, macc], [ps], lambda e, ps=ps, macc=macc: e.matmul(
            ps[:, 0:32], onesb[:, :], macc[:, :], start=True, stop=True))
        sc = kb.sb("sc", [128, 6, 32], F32)
        SC = [sc]
        kb.op("dve", [ps], SC, lambda e, ps=ps: e.tensor_copy(sc[:, 0, :], ps[:, 0:32]))
        sci = kb.sb("sci", [128, 2, 32], I32)
        kb.op("dve", SC, SC, lambda e: e.tensor_scalar(sc[:, 1, :], sc[:, 0, :], float(MOE_BS - 1), None, ALU.add))
        kb.op("dve", SC, [sci], lambda e: e.tensor_copy(sci[:, 0, :], sc[:, 1, :]))
        kb.op("dve", [sci], [sci], lambda e: e.tensor_scalar(
            sci[:, 1, :], sci[:, 0, :], MOE_SH, MOE_SH, ALU.arith_shift_right, ALU.logical_shift_left))
        kb.op("dve", [sci], SC, lambda e: e.tensor_copy(sc[:, 2, :], sci[:, 1, :]))
        kb.op("dve", SC, SC, lambda e: e.tensor_copy(sc[:, 3, :], sc[:, 2, :]))
        a, b = 3, 4
        for sft in (1, 2, 4, 8, 16):
            kb.op("dve", SC, SC, lambda e, a=a, b=b: e.tensor_copy(sc[:, b, :], sc[:, a, :]))
            kb.op("dve", SC, SC, lambda e, a=a, b=b, sft=sft: e.tensor_tensor(
                sc[:, b, sft:32], sc[:, a, sft:32], sc[:, a, 0:32 - sft], ALU.add))
            a, b = b, a
        pe_idx = a
        kb.op("dve", SC, SC, lambda e: e.tensor_tensor(sc[:, 5, :], sc[:, pe_idx, :], sc[:, 2, :], ALU.subtract))
        tmpd = Ring([kb.sb("tmpd%d" % i, [128, 32], F32) for i in range(2)])
        junk32 = kb.sb("junk32", [128, 2, 32], F32)
        for i in range(NT):
            td = tmpd.next()
            kb.op("dve", SC + [rank.trk(i)], [td], lambda e, td=td, i=i: e.tensor_tensor(
                td[:, :], rank[:, i, :], sc[:, 5, :], ALU.add))
            for jj, (MM, mt) in enumerate(((M1, M1.trk(i)), (M2, M2.trk(i)))):
                td2 = tmpd.next() if False else None
                kb.op("dve", [td, mt], [destf], lambda e, td=td, MM=MM, i=i, jj=jj: e.tensor_tensor(
                    junk32[:, jj, :], td[:, :], MM[:, i, :], ALU.mult))
                kb.op("dve", [destf], [destf], lambda e, i=i, jj=jj: e.reduce_sum(
                    destf[:, 2 * i + jj:2 * i + jj + 1], junk32[:, jj, :], axis=AX.X))
        kb.op("dve", [destf], [desti], lambda e: e.tensor_copy(desti[:, :], destf[:, :]))
        blkf = kb.sb("blkf", [128, NBLK], F32)
        kb.op("dve", [], [blkf], lambda e: e.memset(blkf[:, :], 0.0))
        for ex in range(32):
            kb.op("dve", SC + [b128, blkf], [blkf], lambda e, ex=ex: e.scalar_tensor_tensor(
                blkf[:, :], b128[:, :], sc[:, pe_idx, ex:ex + 1], blkf[:, :], ALU.is_ge, ALU.add))
        kb.op("dve", [blkf], [blkf], lambda e: e.tensor_scalar(blkf[:, :], blkf[:, :], 31.0, None, ALU.min))

        pidx = load_const(kb, "pidx_s", pidx_d, [128, 1])
        widx = kb.sb("widx", [128, NBLK], I32)
        kb.op("dve", [blkf, pidx], [blkf], lambda e: e.tensor_scalar(
            blkf[:, :], blkf[:, :], 128.0, pidx[:, 0:1], ALU.mult, ALU.add))
        kb.op("dve", [blkf], [widx], lambda e: e.tensor_copy(widx[:, :], blkf[:, :]))
        w1rows = w1.rearrange("e (p kk) n -> (e p) (kk n)", kk=8)
        w3rows = w3.rearrange("e (p kk) n -> (e p) (kk n)", kk=8)
        w2rows = w2.rearrange("e (p kk) n -> (e p) (kk n)", kk=4)

        xs = Trk()
        for i in range(NT):
            x = xr.next()
            hb = hbr.next()
            kb.dma("sp", x[:, :], h1[i * 128:(i + 1) * 128, :], [], [x])
            kb.op("act", [x], [hb], lambda e, x=x, hb=hb: e.copy(hb[:, :], x[:, :]))
            for jj in range(2):
                kb.dma_fn("pool", lambda e, hb=hb, i=i, jj=jj: e.indirect_dma_start(
                    out=xbuf, out_offset=bass.IndirectOffsetOnAxis(ap=desti[:, 2 * i + jj:2 * i + jj + 1], axis=0),
                    in_=hb[:, :], in_offset=None), [hb, desti, xz], [xs])

        w1s = Ring([kb.sb("w1s%d" % i, [128, NKC, 512], F32) for i in range(2)])
        w3s = Ring([kb.sb("w3s%d" % i, [128, NKC, 512], F32) for i in range(1)])
        w2s = Ring([kb.sb("w2s%d" % i, [128, 4, D], F32) for i in range(1)])
        w1b = Ring([kb.sb("w1b%d" % i, [128, NKC, 512], BF16) for i in range(2)])
        w3b = Ring([kb.sb("w3b%d" % i, [128, NKC, 512], BF16) for i in range(2)])
        w2b = Ring([kb.sb("w2b%d" % i, [128, 4, D], BF16) for i in range(2)])
        xbr = Ring([kb.sb("xb%d" % i, [128, D], BF16) for i in range(2)])
        xTr = Ring([kb.sb("xT%d" % i, [128, NKC, 128], BF16) for i in range(2)])
        sar = Ring([kb.sb("sA%d" % i, [128, 512], F32) for i in range(2)])
        gr = Ring([kb.sb("g%d" % i, [128, 512], BF16) for i in range(2)])
        gTr = Ring([kb.sb("gT%d" % i, [128, 4, 128], BF16) for i in range(2)])
        yr = Ring([kb.sb("y%d" % i, [128, D], F32) for i in range(2)])
        ys = Trk()
        for bq in range(NBLK):
            ws1, ws3, ws2 = w1s.next(), w3s.next(), w2s.next()
            for wsx, wrows in ((ws1, w1rows), (ws3, w3rows), (ws2, w2rows)):
                kb.dma_fn("pool", lambda e, wsx=wsx, wrows=wrows, bq=bq: e.indirect_dma_start(
                    out=wsx[:, :, :].rearrange("p a b -> p (a b)"), out_offset=None, in_=wrows,
                    in_offset=bass.IndirectOffsetOnAxis(ap=widx[:, bq:bq + 1], axis=0)), [widx], [wsx])
            wb1, wb3, wb2 = w1b.next(), w3b.next(), w2b.next()
            kb.op("dve", [ws1], [wb1], lambda e, ws1=ws1, wb1=wb1: e.tensor_copy(wb1[:, :, :], ws1[:, :, :]))
            kb.op("dve", [ws3], [wb3], lambda e, ws3=ws3, wb3=wb3: e.tensor_copy(wb3[:, :, :], ws3[:, :, :]))
            kb.op("act", [ws2], [wb2], lambda e, ws2=ws2, wb2=wb2: e.copy(wb2[:, :, :], ws2[:, :, :]))
            for sub in range(NSUB):
                r0 = (bq * NSUB + sub) * 128
                xb = xbr.next()
                kb.dma("sp", xb[:, :], xbuf[r0:r0 + 128, :], [xs], [xb])
                xT = xTr.next()
                transpose_to(kb, xb, identb, pT, xT[:, :, :], [xT], NKC, eng="dve", strided=True)
                pa, pb = psr.next(), psr.next()
                mm_group(kb, pa, pa[:, :], [xT[:, kc, :] for kc in range(NKC)],
                         [wb1[:, kc, :] for kc in range(NKC)], [xT, wb1])
                mm_group(kb, pb, pb[:, :], [xT[:, kc, :] for kc in range(NKC)],
                         [wb3[:, kc, :] for kc in range(NKC)], [xT, wb3])
                sA = sar.next()
                gg = gr.next()
                kb.op("act", [pa], [sA], lambda e, pa=pa, sA=sA: e.activation(sA[:, :], pa[:, :], AF.Silu))
                kb.op("dve", [sA, pb], [gg], lambda e, sA=sA, pb=pb, gg=gg: e.tensor_tensor(
                    gg[:, :], sA[:, :], pb[:, :], ALU.mult))
                gT = gTr.next()

                def trg(e, gg=gg):
                    ins = None
                    for c in range(4):
                        ins = e.transpose(pT2[:, c, :], gg[:, slice(c, None, 4)], identb[:, :])
                    return ins
                kb.op("pe", [gg, identb], [pT2], trg)
                kb.op("dve", [pT2], [gT], lambda e, gT=gT: e.tensor_copy(gT[:, :, :], pT2[:, :, :]))
                y = yr.next()
                for half in range(2):
                    pc = psr.next()
                    mm_group(kb, pc, pc[:, :], [gT[:, kc, :] for kc in range(4)],
                             [wb2[:, kc, 512 * half:512 * (half + 1)] for kc in range(4)], [gT, wb2])
                    if half == 0:
                        kb.op("act", [pc], [y], lambda e, pc=pc, y=y: e.copy(y[:, 0:512], pc[:, :]))
                    else:
                        kb.op("dve", [pc], [y], lambda e, pc=pc, y=y: e.tensor_copy(y[:, 512:1024], pc[:, :]))
                kb.dma("act", ybuf[r0:r0 + 128, :], y[:, :], [y], [ys])

        y1r = yr
        y2r = Ring([kb.sb("y2_%d" % i, [128, D], F32) for i in range(2)])
        for i in range(NT):
            x = xr.next()
            y1, y2 = y1r.next(), y2r.next()
            kb.dma("sp", x[:, :], h1[i * 128:(i + 1) * 128, :], [], [x])
            for jj, yy in enumerate((y1, y2)):
                kb.dma_fn("pool", lambda e, yy=yy, i=i, jj=jj: e.indirect_dma_start(
                    out=yy[:, :], out_offset=None, in_=ybuf,
                    in_offset=bass.IndirectOffsetOnAxis(ap=desti[:, 2 * i + jj:2 * i + jj + 1], axis=0)),
                    [desti, ys], [yy])
            gt = gates.trk(i)
            kb.op("dve", [y1, gt], [y1], lambda e, y1=y1, i=i: e.tensor_scalar(
                y1[:, :], y1[:, :], gates[:, i, 0:1], None, ALU.mult))
            kb.op("dve", [y1, y2, gt], [y1], lambda e, y1=y1, y2=y2, i=i: e.scalar_tensor_tensor(
                y1[:, :], y2[:, :], gates[:, i, 1:2], y1[:, :], ALU.mult, ALU.add))
            kb.op("dve", [x, y1], [x], lambda e, x=x, y1=y1: e.scalar_tensor_tensor(
                x[:, :], x[:, :], DN_ALPHA, y1[:, :], ALU.mult, ALU.add))
            layer_norm_tile(kb, x, x, g_rep, b_rep, st6, mv, rstd)
            kb.dma("act", h2[i * 128:(i + 1) * 128, :], x[:, :], [x], [OUT])
        kb.finish()
    return nc


def build_LN0(T):
    nc = bass.Bass("TRN2", target_bir_lowering=False)
    with ExitStack() as st:
        kb = KB(nc, st)
        dt = nc.dram_tensor
        x_d = dt("x", [T, D], F32, kind="ExternalInput").ap()
        lng = dt("lng", [128, D], F32, kind="ExternalInput").ap()
        lnb = dt("lnb", [128, D], F32, kind="ExternalInput").ap()
        h0 = dt("h0", [T, D], F32, kind="ExternalOutput").ap()
        g_rep = load_const(kb, "g_rep", lng, [128, D])
        b_rep = load_const(kb, "b_rep", lnb, [128, D])
        xr = Ring([kb.sb("x%d" % i, [128, D], F32) for i in range(4)])
        st6 = kb.sb("st6", [128, 12], F32)
        mv = kb.sb("mv", [128, 2], F32)
        rstd = kb.sb("rstd", [128, 1], F32)
        for i in range(T // 128):
            x = xr.next()
            kb.dma("sp", x[:, :], x_d[i * 128:(i + 1) * 128, :], [], [x])
            layer_norm_tile(kb, x, x, g_rep, b_rep, st6, mv, rstd)
            kb.dma("act", h0[i * 128:(i + 1) * 128, :], x[:, :], [x], [kb.out_trk])
        kb.finish()
    return nc


_PROGS = {}


def _prog(name, fn, *args):
    key = (name,) + args
    if key not in _PROGS:
        _PROGS[key] = fn(*args)
    return _PROGS[key]


def _lay(a):
    J = a.shape[0]
    return np.ascontiguousarray(a.reshape(J, 8, 128).transpose(2, 0, 1).reshape(128, J * 8))


def _rep(a):
    return np.ascontiguousarray(np.broadcast_to(np.asarray(a, np.float32).reshape(1, -1), (128, a.size)))


def kernel(x, ln0_g, ln0_b, rel_table, w_in, gate_b, lam_vecs, subln_g, w_attn_proj, conv_w,
           w_conv_proj, w_out, ln1_g, ln1_b, w_rg, b_rg, w_re, b_re, w1, w3, w2, ln2_g, ln2_b):
    f32 = np.float32
    x = np.asarray(x, f32)
    B, S, _ = x.shape
    NC = 8
    T = B * S // NC
    CPS = S // T
    cores = list(range(NC))
    depth = w_in.shape[0]
    ident = np.eye(128, dtype=f32)
    xt = x.reshape(B * S, D)

    def run(nc, maps):
        return run_bass_kernel_spmd(nc, maps, core_ids=cores).results

    res = run(_prog("LN0", build_LN0, T),
              [{"x": xt[c * T:(c + 1) * T], "lng": _rep(ln0_g), "lnb": _rep(ln0_b)} for c in cores])
    h = [r["h0"] for r in res]

    oh = bias_onehot()
    NBLK = (2 * T + 32 * (MOE_BS - 1) + MOE_BS - 1) // MOE_BS
    utri = np.triu(np.ones((128, 128), f32), 1)
    blk128 = np.ascontiguousarray(np.broadcast_to(np.arange(NBLK, dtype=f32) * MOE_BS, (128, NBLK)))
    pidx = np.arange(128, dtype=f32).reshape(128, 1)

    for l in range(depth):
        lam_init = 0.8 - 0.6 * math.exp(-0.3 * l)
        maps = []
        for c in cores:
            first = (c % CPS == 0)
            halo = np.zeros((128, D), f32) if first else h[c - 1][T - 128:]
            maps.append({"hin": np.concatenate([halo, h[c]], 0),
                         "halo_mask": np.full((128, 1), 0.0 if first else 1.0, f32),
                         "w_in": np.asarray(w_in[l], f32), "gate_b": _lay(np.asarray(gate_b[l], f32)),
                         "conv_w": _lay(np.asarray(conv_w[l], f32)),
                         "w_cp": np.asarray(w_conv_proj[l], f32), "ident": ident})
        ra = run(_prog("A", build_A, T, False), maps)
        maps = []
        for c in cores:
            b, hp = c // CPS, c % CPS
            src = [ra[b * CPS + t] for t in range(CPS)]
            rows = slice(hp * 256, (hp + 1) * 256)
            tab = np.concatenate([np.asarray(rel_table, f32)[:, 2 * hp:2 * hp + 2],
                                  np.full((1, 2), -30000.0, f32)], 0)
            maps.append({"qT": np.concatenate([r["qT"][rows] for r in src], 1),
                         "kT": np.concatenate([r["kT"][rows] for r in src], 1),
                         "v": np.concatenate([r["v"][:, rows] for r in src], 0),
                         "oh": oh, "tab": tab,
                         "lamv": _rep(np.asarray(lam_vecs[l], f32).reshape(-1)),
                         "subg": _rep(subln_g[l]),
                         "lamc": np.ascontiguousarray(np.broadcast_to(
                             np.array([[-lam_init, 1.0 - lam_init]], f32), (128, 2))),
                         "ident": ident})
        rt = run(_prog("ATT", build_ATT, S), maps)
        maps = []
        for c in cores:
            b, t = c // CPS, c % CPS
            oT = np.concatenate([rt[b * CPS + hp]["oT"][:, t * T:(t + 1) * T] for hp in range(CPS)], 0)
            maps.append({"oT": oT, "SaT": ra[c]["SaT"], "YT": ra[c]["YT"], "h": h[c],
                         "w_ap": np.asarray(w_attn_proj[l], f32), "w_out": np.asarray(w_out[l], f32),
                         "lng": _rep(ln1_g[l]), "lnb": _rep(ln1_b[l])})
        r1 = run(_prog("C1", build_C1, T), maps)
        w_r = np.concatenate([np.asarray(w_rg[l], f32), np.asarray(w_re[l], f32)], 1)
        b_r = _rep(np.concatenate([np.asarray(b_rg[l], f32), np.asarray(b_re[l], f32)]))
        w1l, w3l, w2l = np.asarray(w1[l], f32), np.asarray(w3[l], f32), np.asarray(w2[l], f32)
        maps = [{"h1": r1[c]["h1"], "w_r": w_r, "b_r": b_r, "w1": w1l, "w3": w3l, "w2": w2l,
                 "lng": _rep(ln2_g[l]), "lnb": _rep(ln2_b[l]), "ident": ident, "utri": utri,
                 "blk128": blk128, "pidx": pidx} for c in cores]
        r2 = run(_prog("C2", build_C2, T), maps)
        h = [r["h2"] for r in r2]
    return np.concatenate(h, 0).reshape(B, S, D).astype(f32)
```

```python
import math
from contextlib import ExitStack
import numpy as np
import concourse.bass as bass
import concourse.mybir as mybir
from concourse.bass_utils import run_bass_kernel_spmd

F32 = mybir.dt.float32
BF16 = mybir.dt.bfloat16
I32 = mybir.dt.int32
AF = mybir.ActivationFunctionType
ALU = mybir.AluOpType
AX = mybir.AxisListType


class Trk:
    __slots__ = ("w", "r")

    def __init__(self):
        self.w = None
        self.r = {}


class Buf(Trk):
    __slots__ = ("t", "subs")

    def __init__(self, t):
        Trk.__init__(self)
        self.t = t
        self.subs = {}

    def __getitem__(self, idx):
        return self.t[idx]

    def trk(self, key):
        s = self.subs.get(key)
        if s is None:
            s = self.subs[key] = Trk()
        return s


class KB:
    NDMA = 32

    def __init__(self, nc, st):
        self.nc = nc
        self.st = st
        self.eng = {"pe": nc.tensor, "act": nc.scalar, "dve": nc.vector,
                    "pool": nc.gpsimd, "sp": nc.sync}
        self.sem = {}
        self.cnt = {}
        self.known = {k: {} for k in self.eng}
        for k in ("pe", "act", "dve", "pool"):
            self.sem[k] = st.enter_context(nc.semaphore("s_" + k))
            self.cnt[k] = 0
        self.dsem = []
        for i in range(self.NDMA):
            key = "d%d" % i
            self.sem[key] = st.enter_context(nc.semaphore("s_" + key))
            self.cnt[key] = 0
            self.dsem.append(key)
        self.dnext = 0
        self._clear_sems()
        self.out_trk = Trk()
        self.prog = {k: [] for k in self.eng}
        self.nbuf = 0

    def _clear_sems(self):
        for h in self.sem.values():
            self.nc.gpsimd.sem_clear(h)
        self.nc.all_engine_barrier()

    def sb(self, name, shape, dtype):
        t = self.st.enter_context(self.nc.sbuf_tensor(name, list(shape), dtype))
        return Buf(t)

    def ps(self, name, shape, dtype):
        t = self.st.enter_context(self.nc.psum_tensor(name, list(shape), dtype))
        return Buf(t)

    def _wait(self, ename, deps):
        kn = self.known[ename]
        best = {}
        for d in deps:
            if d is None:
                continue
            k, v = d
            if best.get(k, 0) < v:
                best[k] = v
        waits = []
        for k, v in best.items():
            if k == "pe" and ename == "pe":
                continue
            if kn.get(k, 0) >= v:
                continue
            waits.append((k, v))
            kn[k] = v
        return waits

    def _deps(self, reads, writes):
        deps = []
        for b in reads:
            deps.append(b.w)
        for b in writes:
            deps.append(b.w)
            for k, v in b.r.items():
                deps.append((k, v))
        return deps

    def _commit(self, tok, reads, writes):
        k, v = tok
        for b in reads:
            if b.r.get(k, 0) < v:
                b.r[k] = v
        for b in writes:
            b.w = tok
            b.r = {}

    def op(self, ename, reads, writes, fn):
        waits = self._wait(ename, self._deps(reads, writes))
        self.cnt[ename] += 1
        self.prog[ename].append((waits, fn, ename, 1))
        tok = (ename, self.cnt[ename])
        self._commit(tok, reads, writes)
        return tok

    def dma_fn(self, qname, fn, reads, writes):
        key = self.dsem[self.dnext]
        self.dnext = (self.dnext + 1) % self.NDMA
        deps = self._deps(reads, writes)
        if self.cnt[key] > 0:
            deps.append((key, self.cnt[key]))
        waits = self._wait(qname, deps)
        self.cnt[key] += 16
        self.prog[qname].append((waits, fn, key, 16))
        tok = (key, self.cnt[key])
        self._commit(tok, reads, writes)
        return tok

    def dma(self, qname, out, in_, reads, writes, **kw):
        return self.dma_fn(qname, lambda e: e.dma_start(out=out, in_=in_, **kw), reads, writes)

    def finish(self):
        deps = [self.out_trk.w]
        for key in self.dsem:
            if self.cnt[key] > 0:
                deps.append((key, self.cnt[key]))
        for k in ("pe", "act", "dve", "pool"):
            if self.cnt[k] > 0:
                deps.append((k, self.cnt[k]))
        final_waits = self._wait("sp", deps)
        sem = self.sem
        prog = self.prog

        def emit(e, items, tail=()):
            for waits, fn, ik, iv in items:
                for k, v in waits:
                    e.wait_ge(sem[k], v)
                fn(e).then_inc(sem[ik], iv)
            for k, v in tail:
                e.wait_ge(sem[k], v)

        with self.nc.Block() as block:
            @block.tensor
            def _(e):
                emit(e, prog["pe"])

            @block.scalar
            def _(e):
                emit(e, prog["act"])

            @block.vector
            def _(e):
                emit(e, prog["dve"])

            @block.gpsimd
            def _(e):
                emit(e, prog["pool"])

            @block.sync
            def _(e):
                emit(e, prog["sp"], final_waits)

        self._clear_sems()


D = 1024
NKC = 8
LN_EPS = 1e-5
DN_ALPHA = 4 ** 0.25
MOE_BS = 256
MOE_SH = 8


class PsumRing:
    def __init__(self, kb, n, name="ps"):
        self.bufs = [kb.ps("%s%d" % (name, i), [128, 512], F32) for i in range(n)]
        self.i = 0

    def next(self):
        b = self.bufs[self.i]
        self.i = (self.i + 1) % len(self.bufs)
        return b


class Ring:
    def __init__(self, bufs):
        self.bufs = bufs
        self.i = 0

    def next(self):
        b = self.bufs[self.i]
        self.i = (self.i + 1) % len(self.bufs)
        return b


def mm_group(kb, ps, out_ap, lhs, rhs, reads):
    n = len(lhs)

    def fn(e):
        ins = None
        for k in range(n):
            ins = e.matmul(out_ap, lhs[k], rhs[k], start=(k == 0), stop=(k == n - 1))
        return ins
    return kb.op("pe", reads, [ps], fn)


def layer_norm_tile(kb, x, xo, g_rep, b_rep, st6, mv, rstd):
    def stats(e):
        e.bn_stats(st6[:, 0:6], x[:, 0:512])
        return e.bn_stats(st6[:, 6:12], x[:, 512:1024])
    kb.op("dve", [x], [st6], stats)
    kb.op("dve", [st6], [mv], lambda e: e.bn_aggr(mv[:, :], st6[:, :]))
    kb.op("dve", [mv], [rstd], lambda e: e.tensor_scalar(
        rstd[:, :], mv[:, 1:2], LN_EPS, None, ALU.add))
    kb.op("act", [rstd], [rstd], lambda e: e.activation(rstd[:, :], rstd[:, :], AF.Sqrt))
    kb.op("dve", [rstd], [rstd], lambda e: e.reciprocal(rstd[:, :], rstd[:, :]))
    kb.op("dve", [x, mv, rstd], [xo], lambda e: e.tensor_scalar(
        xo[:, :], x[:, :], mv[:, 0:1], rstd[:, 0:1], ALU.subtract, ALU.mult))
    kb.op("dve", [xo, g_rep], [xo], lambda e: e.tensor_tensor(
        xo[:, :], xo[:, :], g_rep[:, :], ALU.mult))
    kb.op("dve", [xo, b_rep], [xo], lambda e: e.tensor_tensor(
        xo[:, :], xo[:, :], b_rep[:, :], ALU.add))


def load_const(kb, name, dram_ap, shape, dtype=F32, q="sp"):
    b = kb.sb(name, shape, dtype)
    kb.dma(q, b[tuple(slice(None) for _ in shape)], dram_ap, [], [b])
    return b


def transpose_to(kb, src, identb, pT, dst_ap, dst_trks, nch, eng="act", strided=False):
    def tr(e):
        ins = None
        for c in range(nch):
            sl = slice(c, None, nch) if strided else slice(c * 128, (c + 1) * 128)
            ins = e.transpose(pT[:, c, :], src[:, sl], identb[:, :])
        return ins
    kb.op("pe", [src, identb], [pT], tr)
    if eng == "act":
        kb.op("act", [pT], dst_trks, lambda e: e.copy(dst_ap, pT[:, 0:nch, :]))
    else:
        kb.op("dve", [pT], dst_trks, lambda e: e.tensor_copy(dst_ap, pT[:, 0:nch, :]))


def build_A(T, layer0):
    nc = bass.Bass("TRN2", target_bir_lowering=False)
    TH = T + 128
    NB = T // 512
    NT = T // 128
    with ExitStack() as st:
        kb = KB(nc, st)
        dt = nc.dram_tensor
        hin = dt("hin", [TH, D], F32, kind="ExternalInput").ap()
        halo_mask = dt("halo_mask", [128, 1], F32, kind="ExternalInput").ap()
        w_in = dt("w_in", [D, 8192], F32, kind="ExternalInput").ap()
        gate_b = dt("gate_b", [128, 16], F32, kind="ExternalInput").ap()
        conv_w = dt("conv_w", [128, 24], F32, kind="ExternalInput").ap()
        w_cp = dt("w_cp", [D, D], F32, kind="ExternalInput").ap()
        ident = dt("ident", [128, 128], F32, kind="ExternalInput").ap()
        if layer0:
            lng = dt("lng", [128, D], F32, kind="ExternalInput").ap()
            lnb = dt("lnb", [128, D], F32, kind="ExternalInput").ap()
            h0 = dt("h0", [T, D], F32, kind="ExternalOutput").ap()
        qT = dt("qT", [D, T], BF16, kind="ExternalOutput").ap()
        kT = dt("kT", [D, T], BF16, kind="ExternalOutput").ap()
        v = dt("v", [T, D], BF16, kind="ExternalOutput").ap()
        SaT = dt("SaT", [D, T], BF16, kind="ExternalOutput").ap()
        YT = dt("YT", [D, T], BF16, kind="ExternalOutput").ap()
        OUT = kb.out_trk

        identf = load_const(kb, "identf", ident, [128, 128])
        identb = kb.sb("identb", [128, 128], BF16)
        kb.op("dve", [identf], [identb], lambda e: e.tensor_copy(identb[:, :], identf[:, :]))
        gb = load_const(kb, "gb", gate_b, [128, 16])
        cw = load_const(kb, "cw", conv_w, [128, 24])
        hm = load_const(kb, "hm", halo_mask, [128, 1])
        if layer0:
            g_rep = load_const(kb, "g_rep", lng, [128, D])
            b_rep = load_const(kb, "b_rep", lnb, [128, D])

        hT = kb.sb("hT", [128, NKC, TH], BF16)
        pT = kb.ps("pT", [128, NKC, 128], BF16)
        psr = PsumRing(kb, 6)

        xr = Ring([kb.sb("x%d" % i, [128, D], F32) for i in range(3)])
        hbr = Ring([kb.sb("hb%d" % i, [128, D], BF16) for i in range(2)])
        st6 = kb.sb("st6", [128, 12], F32)
        mv = kb.sb("mv", [128, 2], F32)
        rstd = kb.sb("rstd", [128, 1], F32)
        for i in range(TH // 128):
            x = xr.next()
            hb = hbr.next()
            kb.dma("sp", x[:, :], hin[i * 128:(i + 1) * 128, :], [], [x])
            if layer0:
                layer_norm_tile(kb, x, x, g_rep, b_rep, st6, mv, rstd)
                if i >= 1:
                    kb.dma("pool", h0[(i - 1) * 128:i * 128, :], x[:, :], [x], [OUT])
            kb.op("act", [x], [hb], lambda e, x=x, hb=hb: e.copy(hb[:, :], x[:, :]))
            transpose_to(kb, hb, identb, pT, hT[:, :, i * 128:(i + 1) * 128], [hT.trk(i)], NKC,
                         eng="dve" if i % 2 else "act")

        def hT_blk(n):
            return [hT.trk(1 + 4 * n + a) for a in range(4)]

        wst = Ring([kb.sb("wst%d" % i, [128, NKC, 512], F32) for i in range(1)])
        wbf = Ring([kb.sb("wbf%d" % i, [128, NKC, 512], BF16) for i in range(2)])

        wci = [0]

        def load_w(srcs):
            ws = wst.next()
            wb = wbf.next()
            c0 = 0
            for ap in srcs:
                nco = ap.shape[1]
                kb.dma("sp", ws[:, :, c0:c0 + nco], ap.rearrange("(kc p) n -> p kc n", p=128),
                       [], [ws])
                c0 += nco
            wci[0] += 1
            if wci[0] % 2:
                kb.op("act", [ws], [wb], lambda e, ws=ws, wb=wb, c0=c0: e.copy(
                    wb[:, :, 0:c0], ws[:, :, 0:c0]))
            else:
                kb.op("dve", [ws], [wb], lambda e, ws=ws, wb=wb, c0=c0: e.tensor_copy(
                    wb[:, :, 0:c0], ws[:, :, 0:c0]))
            return wb

        evi = [0]

        def evac(ps, out_ap, out_trks, src_ap, extra_reads=()):
            evi[0] += 1
            if evi[0] % 2:
                kb.op("act", [ps] + list(extra_reads), out_trks, lambda e: e.copy(out_ap, src_ap))
            else:
                kb.op("dve", [ps] + list(extra_reads), out_trks, lambda e: e.tensor_copy(out_ap, src_ap))

        ostage = Ring([kb.sb("ost%d" % i, [128, T], BF16) for i in range(2)])

        for p in range(4):
            wb = load_w([w_in[:, p * 512:(p + 1) * 512]])
            for cc in range(4):
                ch = p * 4 + cc
                og = ostage.next()
                for n in range(NB):
                    ps = psr.next()
                    mm_group(kb, ps, ps[:, :],
                             [wb[:, kc, cc * 128:(cc + 1) * 128] for kc in range(NKC)],
                             [hT[:, kc, 128 + 512 * n:128 + 512 * (n + 1)] for kc in range(NKC)],
                             [wb] + hT_blk(n))
                    evac(ps, og[:, 512 * n:512 * (n + 1)], [og], ps[:, :])
                dst = qT if ch < 8 else kT
                r0 = (ch % 8) * 128
                kb.dma("pool", dst[r0:r0 + 128, :], og[:, :], [og], [OUT])

        vstage = Ring([kb.sb("vst%d" % i, [128, 512], BF16) for i in range(3)])
        for p in range(2):
            wb = load_w([w_in[:, 2048 + p * 512:2048 + (p + 1) * 512]])
            for i in range(NT):
                ps = psr.next()
                mm_group(kb, ps, ps[:, :],
                         [hT[:, kc, 128 + 128 * i:128 + 128 * (i + 1)] for kc in range(NKC)],
                         [wb[:, kc, :] for kc in range(NKC)],
                         [wb, hT.trk(1 + i)])
                vs = vstage.next()
                evac(ps, vs[:, :], [vs], ps[:, :])
                kb.dma("pool", v[i * 128:(i + 1) * 128, p * 512:(p + 1) * 512], vs[:, :], [vs], [OUT])

        NH = 2 if T >= 1024 else 1
        TH2 = T // NH
        NB2 = TH2 // 512
        gT = kb.sb("gT", [128, NKC, TH2], BF16)
        uT = kb.sb("uT", [128, TH2 + 2], F32)
        cbT = kb.sb("cbT", [128, TH2], F32)
        yacc = kb.sb("yacc", [128, TH2], F32)
        tmpr = Ring([kb.sb("tmp%d" % i, [128, 512], F32) for i in range(2)])
        for hf in range(NH):
            t0 = hf * TH2
            for j in range(8):
                wb = load_w([w_in[:, 3072 + 128 * j:3072 + 128 * (j + 1)],
                             w_in[:, 4096 + 128 * j:4096 + 128 * (j + 1)],
                             w_in[:, 5120 + 128 * j:5120 + 128 * (j + 1)]])
                ps = psr.next()
                hc = 128 + t0
                htr = [hT.trk(0)] if hf == 0 else [hT.trk((hc - 2) // 128)]

                def halo_mm(e, ps=ps, wb=wb, hc=hc):
                    ins = None
                    for g in range(2):
                        for kc in range(NKC):
                            ins = e.matmul(ps[:, 2 * g:2 * g + 2], wb[:, kc, 128 * (g + 1):128 * (g + 2)],
                                           hT[:, kc, hc - 2:hc], start=(kc == 0), stop=(kc == NKC - 1))
                    return ins
                kb.op("pe", [wb] + htr, [ps], halo_mm)
                tm = tmpr.next()
                kb.op("act", [ps], [tm], lambda e, ps=ps, tm=tm: e.copy(tm[:, 0:2], ps[:, 0:2]))
                kb.op("dve", [ps, tm], [tm], lambda e, ps=ps, tm=tm: e.tensor_tensor(
                    tm[:, 2:4], tm[:, 0:2], ps[:, 2:4], ALU.mult))
                if hf == 0:
                    kb.op("dve", [tm, hm], [uT], lambda e, tm=tm: e.tensor_scalar(
                        uT[:, 0:2], tm[:, 2:4], hm[:, 0:1], None, ALU.mult))
                else:
                    kb.op("dve", [tm], [uT], lambda e, tm=tm: e.tensor_copy(uT[:, 0:2], tm[:, 2:4]))
                for n2 in range(NB2):
                    n = hf * NB2 + n2
                    rhs = [hT[:, kc, 128 + 512 * n:128 + 512 * (n + 1)] for kc in range(NKC)]
                    pcb, pcc, pcx = psr.next(), psr.next(), psr.next()
                    for g, ps in enumerate((pcb, pcc, pcx)):
                        mm_group(kb, ps, ps[:, :], [wb[:, kc, 128 * g:128 * (g + 1)] for kc in range(NKC)],
                                 rhs, [wb] + hT_blk(n))
                    tm = tmpr.next()
                    kb.op("act", [pcc], [tm], lambda e, pcc=pcc, tm=tm: e.copy(tm[:, :], pcc[:, :]))
                    kb.op("dve", [tm, pcx], [uT], lambda e, tm=tm, pcx=pcx, n2=n2: e.tensor_tensor(
                        uT[:, 2 + 512 * n2:2 + 512 * (n2 + 1)], tm[:, :], pcx[:, :], ALU.mult))
                    kb.op("act", [pcb], [cbT], lambda e, pcb=pcb, n2=n2: e.copy(
                        cbT[:, 512 * n2:512 * (n2 + 1)], pcb[:, :]))
                kb.op("dve", [uT, cw], [yacc], lambda e, j=j: e.tensor_scalar(
                    yacc[:, :], uT[:, 2:TH2 + 2], cw[:, 16 + j:17 + j], None, ALU.mult))
                kb.op("dve", [uT, cw, yacc], [yacc], lambda e, j=j: e.scalar_tensor_tensor(
                    yacc[:, :], uT[:, 1:TH2 + 1], cw[:, 8 + j:9 + j], yacc[:, :], ALU.mult, ALU.add))
                kb.op("dve", [uT, cw, yacc], [yacc], lambda e, j=j: e.scalar_tensor_tensor(
                    yacc[:, :], uT[:, 0:TH2], cw[:, j:j + 1], yacc[:, :], ALU.mult, ALU.add))
                kb.op("dve", [yacc, cbT], [gT.trk(j)], lambda e, j=j: e.tensor_tensor(
                    gT[:, j, :], yacc[:, :], cbT[:, :], ALU.mult))

            gT_all = [gT.trk(j) for j in range(8)]
            for j in range(8):
                wb = load_w([w_in[:, 7168 + 128 * j:7168 + 128 * (j + 1)], w_cp[:, 128 * j:128 * (j + 1)]])
                og = ostage.next()
                for n2 in range(NB2):
                    n = hf * NB2 + n2
                    pyc, pgc = psr.next(), psr.next()
                    mm_group(kb, pyc, pyc[:, :], [wb[:, kc, 128:256] for kc in range(NKC)],
                             [gT[:, kc, 512 * n2:512 * (n2 + 1)] for kc in range(NKC)], [wb] + gT_all)
                    mm_group(kb, pgc, pgc[:, :], [wb[:, kc, 0:128] for kc in range(NKC)],
                             [hT[:, kc, 128 + 512 * n:128 + 512 * (n + 1)] for kc in range(NKC)],
                             [wb] + hT_blk(n))
                    tm = tmpr.next()
                    kb.op("act", [pgc, gb], [tm], lambda e, pgc=pgc, tm=tm, j=j: e.activation(
                        tm[:, :], pgc[:, :], AF.Sigmoid, bias=gb[:, 8 + j:9 + j]))
                    kb.op("dve", [tm, pyc], [og], lambda e, tm=tm, pyc=pyc, og=og, n2=n2: e.tensor_tensor(
                        og[:, 512 * n2:512 * (n2 + 1)], tm[:, :], pyc[:, :], ALU.mult))
                kb.dma("pool", YT[128 * j:128 * (j + 1), t0:t0 + TH2], og[:, 0:TH2], [og], [OUT])

        for p in range(2):
            wb = load_w([w_in[:, 6144 + p * 512:6144 + (p + 1) * 512]])
            for cc in range(4):
                j = p * 4 + cc
                og = ostage.next()
                for n in range(NB):
                    ps = psr.next()
                    mm_group(kb, ps, ps[:, :],
                             [wb[:, kc, cc * 128:(cc + 1) * 128] for kc in range(NKC)],
                             [hT[:, kc, 128 + 512 * n:128 + 512 * (n + 1)] for kc in range(NKC)],
                             [wb] + hT_blk(n))
                    kb.op("act", [ps, gb], [og], lambda e, ps=ps, og=og, n=n, j=j: e.activation(
                        og[:, 512 * n:512 * (n + 1)], ps[:, :], AF.Sigmoid, bias=gb[:, j:j + 1]))
                kb.dma("pool", SaT[128 * j:128 * (j + 1), :], og[:, :], [og], [OUT])
        kb.finish()
    return nc


FL = 1151
FP = FL + 1


def rel_bucket_np(rel):
    n = np.maximum(rel, 0)
    nf = np.maximum(n, 1).astype(np.float32)
    large = 16 + (np.log(nf / np.float32(16)) / np.float32(math.log(128 / 16)) * np.float32(16)).astype(np.int32)
    large = np.minimum(large, 31)
    return np.where(n < 16, n, large)


def bias_onehot():
    rel = np.arange(FL) - 511
    b = rel_bucket_np(rel)
    oh = np.zeros((33, FL), np.float32)
    for i in range(FL):
        if rel[i] < 0:
            oh[32, i] = 1.0
        else:
            oh[b[i], i] = 1.0
    return oh


def build_ATT(S):
    nc = bass.Bass("TRN2", target_bir_lowering=False)
    NQB = S // 512
    NKCH = S // 128
    with ExitStack() as st:
        kb = KB(nc, st)
        dt = nc.dram_tensor
        qT = dt("qT", [256, S], BF16, kind="ExternalInput").ap()
        kT = dt("kT", [256, S], BF16, kind="ExternalInput").ap()
        v = dt("v", [S, 256], BF16, kind="ExternalInput").ap()
        oh = dt("oh", [33, FL], F32, kind="ExternalInput").ap()
        tab = dt("tab", [33, 2], F32, kind="ExternalInput").ap()
        lamv = dt("lamv", [128, 256], F32, kind="ExternalInput").ap()
        subg = dt("subg", [128, 128], F32, kind="ExternalInput").ap()
        lamc_d = dt("lamc", [128, 2], F32, kind="ExternalInput").ap()
        ident = dt("ident", [128, 128], F32, kind="ExternalInput").ap()
        oT = dt("oT", [256, S], BF16, kind="ExternalOutput").ap()
        fscr = [nc.dram_tensor("fscr%d" % h, [128 * FP], F32) for h in range(2)]
        OUT = kb.out_trk

        identf = load_const(kb, "identf", ident, [128, 128])
        identb = kb.sb("identb", [128, 128], BF16)
        kb.op("dve", [identf], [identb], lambda e: e.tensor_copy(identb[:, :], identf[:, :]))
        ohs = load_const(kb, "ohs", oh, [33, FL])
        tabs = load_const(kb, "tabs", tab, [33, 2])
        lv = load_const(kb, "lv", lamv, [128, 256])
        g_rep = load_const(kb, "g_rep", subg, [128, 128])
        lamc = load_const(kb, "lamc_s", lamc_d, [128, 2])
        kb.op("dve", [g_rep, lamc], [g_rep], lambda e: e.tensor_scalar(
            g_rep[:, :], g_rep[:, :], lamc[:, 1:2], None, ALU.mult))
        epst = kb.sb("epst", [128, 1], F32)
        kb.op("dve", [], [epst], lambda e: e.memset(epst[:, :], LN_EPS))

        lt = kb.sb("lt", [128, 128], F32)
        ls = kb.sb("ls", [128, 2], F32)
        nlam = kb.sb("nlam", [128, 1], F32)
        kb.op("dve", [lv], [lt], lambda e: e.tensor_tensor(
            lt[:, 0:64], lv[:, 0:64], lv[:, 64:128], ALU.mult))
        kb.op("dve", [lv, lt], [lt], lambda e: e.tensor_tensor(
            lt[:, 64:128], lv[:, 128:192], lv[:, 192:256], ALU.mult))
        kb.op("dve", [lt], [ls], lambda e: e.reduce_sum(
            ls[:, 0:2], lt[:, :].rearrange("p (a b) -> p a b", a=2), axis=AX.X))
        kb.op("act", [ls], [ls], lambda e: e.activation(ls[:, :], ls[:, :], AF.Exp))
        kb.op("dve", [ls], [nlam], lambda e: e.tensor_tensor(
            nlam[:, :], ls[:, 1:2], ls[:, 0:1], ALU.subtract))
        kb.op("dve", [nlam, lamc], [nlam], lambda e: e.tensor_scalar(
            nlam[:, :], nlam[:, :], lamc[:, 0:1], None, ALU.add))

        s12 = Ring([kb.ps("s12_%d" % i, [128, 1024], F32) for i in range(2)])
        oacc = kb.ps("oacc", [128, 3, 512], F32)
        pT = kb.ps("pT", [128, 4, 128], BF16)

        def acc_ap(h2, qc):
            g = h2 * 4 + qc
            return oacc[:, g // 3, (g % 3) * 129:(g % 3) * 129 + 129]

        kTs = kb.sb("kTs", [128, S], BF16)
        vaug = kb.sb("vaug", [128, NKCH, 129], BF16)
        tabrep = kb.sb("tabrep", [33, 128], F32)
        frep = kb.sb("frep", [128, FL], F32)
        c31 = kb.sb("c31", [128, 1], F32)
        biasT = [kb.sb("biasT%d" % d, [128, 512], F32) for d in range(5)]
        qr = Ring([kb.sb("qb%d" % i, [128, 512], BF16) for i in range(3)])
        pr = Ring([kb.sb("pb%d" % i, [128, 1024], BF16) for i in range(3)])
        tmpr = Ring([kb.sb("tm%d" % i, [128, 1024], F32) for i in range(2)])
        osb = Ring([kb.sb("osb%d" % i, [128, 8, 132], F32) for i in range(2)])
        ostage = Ring([kb.sb("ostg%d" % i, [128, 512], BF16) for i in range(2)])
        ofr = Ring([kb.sb("of%d" % i, [128, 4, 128], F32) for i in range(3)])
        onr = Ring([kb.sb("on%d" % i, [128, 4, 128], BF16) for i in range(2)])
        smr = Ring([kb.sb("sm%d" % i, [128, 16], F32) for i in range(3)])
        junk = kb.sb("junk", [128, 128], F32)

        for hh in range(2):
            kb.dma("sp", kTs[:, :], kT[hh * 128:(hh + 1) * 128, :], [], [kTs])
            kb.dma("sp", vaug[:, :, 0:128],
                   v[:, hh * 128:(hh + 1) * 128].rearrange("(c p) e -> p c e", p=128), [], [vaug])
            kb.op("pool", [], [vaug], lambda e: e.memset(vaug[:, :, 128:129], 1.0))
            kb.op("dve", [tabs], [tabrep], lambda e, hh=hh: e.tensor_copy(
                tabrep[:, :], tabs[:, hh:hh + 1].to_broadcast([33, 128])))
            ps = s12.next()
            ps2 = s12.next()

            def fmm(e, ps=ps, ps2=ps2):
                e.matmul(ps[:, 0:512], tabrep[:, :], ohs[:, 0:512], start=True, stop=True)
                e.matmul(ps[:, 512:1024], tabrep[:, :], ohs[:, 512:1024], start=True, stop=True)
                return e.matmul(ps2[:, 0:FL - 1024], tabrep[:, :], ohs[:, 1024:FL], start=True, stop=True)
            kb.op("pe", [tabrep, ohs], [ps, ps2], fmm)
            kb.op("dve", [ps], [frep], lambda e, ps=ps: e.tensor_copy(frep[:, 0:1024], ps[:, :]))
            kb.op("dve", [ps2, frep], [frep], lambda e, ps2=ps2: e.tensor_copy(
                frep[:, 1024:FL], ps2[:, 0:FL - 1024]))
            kb.op("dve", [frep], [c31], lambda e: e.tensor_copy(c31[:, :], frep[:, FL - 1:FL]))
            ftrk = Trk()
            kb.dma("sp", bass.AP(fscr[hh], 0, [[FP, 128], [1, FL]]), frep[:, :], [frep], [ftrk])
            for d in range(5):
                delta = -128 + 128 * d
                kb.dma("sp", biasT[d][:, :], bass.AP(fscr[hh], 511 - delta, [[FL, 128], [1, 512]]),
                       [ftrk], [biasT[d]])

            steps = [(I, j) for I in range(NQB) for j in range(4 * I + 4)]
            qblk = {}
            deferred = []

            def get_q(I):
                if I not in qblk:
                    qb = qr.next()
                    kb.dma("sp", qb[:, :], qT[hh * 128:(hh + 1) * 128, 512 * I:512 * (I + 1)], [], [qb])
                    qblk[I] = qb
                return qblk[I]

            def qk(I, j):
                qb = get_q(I)
                ps = s12.next()

                def fn(e, ps=ps, qb=qb, j=j):
                    e.matmul(ps[:, 0:512], kTs[0:64, 128 * j:128 * (j + 1)], qb[0:64, :],
                             start=True, stop=True)
                    return e.matmul(ps[:, 512:1024], kTs[64:128, 128 * j:128 * (j + 1)], qb[64:128, :],
                                    start=True, stop=True)
                kb.op("pe", [kTs, qb], [ps], fn)
                return ps

            pend = [qk(*steps[k]) for k in range(min(2, len(steps)))]
            for si, (I, j) in enumerate(steps):
                cur = pend.pop(0)
                pb = pr.next()
                if j <= 4 * I - 2:
                    kb.op("act", [cur, c31], [pb], lambda e, cur=cur, pb=pb: e.activation(
                        pb[:, :], cur[:, :], AF.Exp, bias=c31[:, 0:1], scale=0.125))
                else:
                    d = j - (4 * I - 1)
                    bt = biasT[d]
                    tm = tmpr.next()
                    for h2 in range(2):
                        kb.op("dve", [cur, bt], [tm], lambda e, cur=cur, tm=tm, bt=bt, h2=h2: e.scalar_tensor_tensor(
                            tm[:, 512 * h2:512 * (h2 + 1)], cur[:, 512 * h2:512 * (h2 + 1)], 0.125, bt[:, :],
                            ALU.mult, ALU.add))
                    kb.op("act", [tm], [pb], lambda e, tm=tm, pb=pb: e.activation(
                        pb[:, :], tm[:, :], AF.Exp))
                last = (j == 4 * I + 3)
                if si + 2 < len(steps):
                    pend.append(qk(*steps[si + 2]))

                def av(e, pb=pb, j=j, last=last):
                    ins = None
                    for h2 in range(2):
                        for qc in range(4):
                            g = h2 * 4 + qc
                            ins = e.matmul(acc_ap(h2, qc), pb[:, 512 * h2 + 128 * qc:512 * h2 + 128 * (qc + 1)],
                                           vaug[:, j, :], start=(j == 0 and g % 3 == 0), stop=last,
                                           skip_group_check=True)
                    return ins
                kb.op("pe", [pb, vaug], [oacc], av)
                if last:
                    ob = osb.next()
                    for bnk in range(3):
                        ng = 3 if bnk < 2 else 2
                        kb.op("dve", [oacc], [ob], lambda e, ob=ob, bnk=bnk, ng=ng: e.tensor_copy(
                            ob[:, 3 * bnk:3 * bnk + ng, 0:129],
                            oacc[:, bnk, 0:ng * 129].rearrange("p (g c) -> p g c", g=ng)))
                    rl = smr.next()
                    of = ofr.next()
                    kb.op("dve", [ob], [rl], lambda e, ob=ob, rl=rl: e.reciprocal(
                        rl[:, 0:8].unsqueeze(2), ob[:, 0:8, 128:129]))
                    kb.op("dve", [rl, nlam], [rl], lambda e, rl=rl: e.tensor_scalar(
                        rl[:, 4:8], rl[:, 4:8], nlam[:, 0:1], None, ALU.mult))
                    kb.op("dve", [ob, rl], [of], lambda e, ob=ob, rl=rl, of=of: e.tensor_tensor(
                        of[:, :, :], ob[:, 0:4, 0:128], rl[:, 0:4].unsqueeze(2).broadcast_to([128, 4, 128]),
                        ALU.mult))
                    kb.op("dve", [ob, rl], [ob], lambda e, ob=ob, rl=rl: e.tensor_tensor(
                        ob[:, 4:8, 0:128], ob[:, 4:8, 0:128],
                        rl[:, 4:8].unsqueeze(2).broadcast_to([128, 4, 128]), ALU.mult))
                    kb.op("dve", [ob, of], [of], lambda e, ob=ob, of=of: e.tensor_tensor(
                        of[:, :, :], of[:, :, :], ob[:, 4:8, 0:128], ALU.add))
                    kb.op("dve", [of], [ob], lambda e, ob=ob, of=of: e.tensor_tensor(
                        ob[:, 0:4, 0:128], of[:, :, :], of[:, :, :], ALU.mult))
                    kb.op("dve", [ob], [rl], lambda e, ob=ob, rl=rl: e.reduce_sum(
                        rl[:, 8:12], ob[:, 0:4, 0:128], axis=AX.X))

                    def e2(rl=rl):
                        kb.op("act", [rl, epst], [rl], lambda e: e.activation(
                            rl[:, 12:16], rl[:, 8:12], AF.Ln, bias=epst[:, 0:1], scale=1.0 / 128))
                        kb.op("act", [rl], [rl], lambda e: e.activation(
                            rl[:, 8:12], rl[:, 12:16], AF.Exp, scale=-0.5))

                    def e3(rl=rl, of=of, I=I):
                        on = onr.next()
                        og = ostage.next()
                        kb.op("dve", [of, rl], [of], lambda e: e.tensor_tensor(
                            of[:, :, :], of[:, :, :], rl[:, 8:12].unsqueeze(2).broadcast_to([128, 4, 128]),
                            ALU.mult))
                        kb.op("dve", [of, g_rep], [on], lambda e: e.tensor_tensor(
                            on[:, :, :], of[:, :, :], g_rep[:, :].unsqueeze(1).broadcast_to([128, 4, 128]),
                            ALU.mult))

                        def trq(e):
                            ins = None
                            for qc in range(4):
                                ins = e.transpose(pT[:, qc, :], on[:, qc, :], identb[:, :])
                            return ins
                        kb.op("pe", [on, identb], [pT], trq)
                        kb.op("dve", [pT], [og], lambda e: e.tensor_copy(
                            og[:, :].rearrange("p (a b) -> p a b", a=4), pT[:, :, :]))
                        kb.dma("sp", oT[hh * 128:(hh + 1) * 128, 512 * I:512 * (I + 1)], og[:, :], [og], [OUT])
                    deferred.append((si + 3, e2))
                    deferred.append((si + 6, e3))
                while deferred and deferred[0][0] <= si:
                    deferred.pop(0)[1]()
            while deferred:
                deferred.pop(0)[1]()
        kb.finish()
    return nc


def load_w_resident(kb, name, w_ap, ncols, wst):
    wb = kb.sb(name, [128, NKC, ncols], BF16)
    for p in range(0, ncols, 512):
        nco = min(512, ncols - p)
        ws = wst.next()
        kb.dma("sp", ws[:, :, 0:nco], w_ap[:, p:p + nco].rearrange("(kc p) n -> p kc n", p=128), [], [ws])
        kb.op("pool", [ws], [wb], lambda e, ws=ws, p=p, nco=nco: e.tensor_copy(
            wb[:, :, p:p + nco], ws[:, :, 0:nco]))
    return wb


def build_C1(T):
    nc = bass.Bass("TRN2", target_bir_lowering=False)
    NB = T // 512
    with ExitStack() as st:
        kb = KB(nc, st)
        dt = nc.dram_tensor
        oT = dt("oT", [D, T], BF16, kind="ExternalInput").ap()
        SaT = dt("SaT", [D, T], BF16, kind="ExternalInput").ap()
        YT = dt("YT", [D, T], BF16, kind="ExternalInput").ap()
        h = dt("h", [T, D], F32, kind="ExternalInput").ap()
        w_ap = dt("w_ap", [D, D], F32, kind="ExternalInput").ap()
        w_out = dt("w_out", [D, D], F32, kind="ExternalInput").ap()
        lng = dt("lng", [128, D], F32, kind="ExternalInput").ap()
        lnb = dt("lnb", [128, D], F32, kind="ExternalInput").ap()
        h1 = dt("h1", [T, D], F32, kind="ExternalOutput").ap()
        OUT = kb.out_trk
        g_rep = load_const(kb, "g_rep", lng, [128, D])
        b_rep = load_const(kb, "b_rep", lnb, [128, D])
        wst = Ring([kb.sb("wst%d" % i, [128, NKC, 512], F32) for i in range(2)])
        wap_b = load_w_resident(kb, "wap_b", w_ap, D, wst)
        wout_b = load_w_resident(kb, "wout_b", w_out, D, wst)
        psr = PsumRing(kb, 6)
        obr = Ring([kb.sb("ob%d" % i, [128, NKC, 512], BF16) for i in range(2)])
        sar = Ring([kb.sb("sa%d" % i, [128, NKC, 512], BF16) for i in range(2)])
        yr = Ring([kb.sb("yb%d" % i, [128, NKC, 512], BF16) for i in range(2)])
        mbr = Ring([kb.sb("mb%d" % i, [128, NKC, 512], BF16) for i in range(2)])
        tmr = Ring([kb.sb("tm%d" % i, [128, 512], F32) for i in range(3)])
        xr = Ring([kb.sb("x%d" % i, [128, D], F32) for i in range(3)])
        st6 = kb.sb("st6", [128, 12], F32)
        mv = kb.sb("mv", [128, 2], F32)
        rstd = kb.sb("rstd", [128, 1], F32)
        for n in range(NB):
            ob, sa, yb, mb = obr.next(), sar.next(), yr.next(), mbr.next()
            sl = slice(512 * n, 512 * (n + 1))
            kb.dma("sp", ob[:, :, :], oT[:, sl].rearrange("(c p) t -> p c t", p=128), [], [ob])
            kb.dma("sp", sa[:, :, :], SaT[:, sl].rearrange("(c p) t -> p c t", p=128), [], [sa])
            kb.dma("sp", yb[:, :, :], YT[:, sl].rearrange("(c p) t -> p c t", p=128), [], [yb])
            for j in range(8):
                ps = psr.next()
                mm_group(kb, ps, ps[:, :], [wap_b[:, kc, 128 * j:128 * (j + 1)] for kc in range(NKC)],
                         [ob[:, kc, :] for kc in range(NKC)], [wap_b, ob])
                tm = tmr.next()
                kb.op("dve", [ps, sa], [tm], lambda e, ps=ps, sa=sa, tm=tm, j=j: e.tensor_tensor(
                    tm[:, :], ps[:, :], sa[:, j, :], ALU.mult))
                kb.op("pool", [tm, yb], [mb], lambda e, tm=tm, yb=yb, mb=mb, j=j: e.tensor_tensor(
                    mb[:, j, :], tm[:, :], yb[:, j, :], ALU.add))
            for a in range(4):
                i = 4 * n + a
                x = xr.next()
                kb.dma("sp", x[:, :], h[i * 128:(i + 1) * 128, :], [], [x])
                for half in range(2):
                    ps = psr.next()
                    mm_group(kb, ps, ps[:, :], [mb[:, kc, 128 * a:128 * (a + 1)] for kc in range(NKC)],
                             [wout_b[:, kc, 512 * half:512 * (half + 1)] for kc in range(NKC)], [wout_b, mb])
                    kb.op("dve", [ps, x], [x], lambda e, ps=ps, x=x, half=half: e.scalar_tensor_tensor(
                        x[:, 512 * half:512 * (half + 1)], x[:, 512 * half:512 * (half + 1)], DN_ALPHA,
                        ps[:, :], ALU.mult, ALU.add))
                layer_norm_tile(kb, x, x, g_rep, b_rep, st6, mv, rstd)
                kb.dma("act", h1[i * 128:(i + 1) * 128, :], x[:, :], [x], [OUT])
        kb.finish()
    return nc


def build_C2(T):
    nc = bass.Bass("TRN2", target_bir_lowering=False)
    NT = T // 128
    NBLK = (2 * T + 32 * (MOE_BS - 1) + MOE_BS - 1) // MOE_BS
    NSUB = MOE_BS // 128
    with ExitStack() as st:
        kb = KB(nc, st)
        dt = nc.dram_tensor
        h1 = dt("h1", [T, D], F32, kind="ExternalInput").ap()
        w_r = dt("w_r", [D, 36], F32, kind="ExternalInput").ap()
        b_r = dt("b_r", [128, 36], F32, kind="ExternalInput").ap()
        w1 = dt("w1", [32, D, 512], F32, kind="ExternalInput").ap()
        w3 = dt("w3", [32, D, 512], F32, kind="ExternalInput").ap()
        w2 = dt("w2", [32, 512, D], F32, kind="ExternalInput").ap()
        lng = dt("lng", [128, D], F32, kind="ExternalInput").ap()
        lnb = dt("lnb", [128, D], F32, kind="ExternalInput").ap()
        ident = dt("ident", [128, 128], F32, kind="ExternalInput").ap()
        utri = dt("utri", [128, 128], F32, kind="ExternalInput").ap()
        blk128 = dt("blk128", [128, NBLK], F32, kind="ExternalInput").ap()
        pidx_d = dt("pidx", [128, 1], F32, kind="ExternalInput").ap()
        h2 = dt("h2", [T, D], F32, kind="ExternalOutput").ap()
        xbuf = nc.dram_tensor("xbuf", [NBLK * MOE_BS, D], BF16).ap()
        ybuf = nc.dram_tensor("ybuf", [NBLK * MOE_BS, D], F32).ap()
        OUT = kb.out_trk

        g_rep = load_const(kb, "g_rep", lng, [128, D])
        b_rep = load_const(kb, "b_rep", lnb, [128, D])
        identf = load_const(kb, "identf", ident, [128, 128])
        identb = kb.sb("identb", [128, 128], BF16)
        kb.op("dve", [identf], [identb], lambda e: e.tensor_copy(identb[:, :], identf[:, :]))
        utf = load_const(kb, "utf", utri, [128, 128])
        utb = kb.sb("utb", [128, 128], BF16)
        kb.op("dve", [utf], [utb], lambda e: e.tensor_copy(utb[:, :], utf[:, :]))
        onesb = kb.sb("onesb", [128, 128], BF16)
        kb.op("dve", [], [onesb], lambda e: e.memset(onesb[:, :], 1.0))
        b128 = load_const(kb, "b128", blk128, [128, NBLK])
        brs = load_const(kb, "brs", b_r, [128, 36])
        wrf = kb.sb("wrf", [128, NKC, 36], F32)
        kb.dma("sp", wrf[:, :, :], w_r.rearrange("(kc p) n -> p kc n", p=128), [], [wrf])
        wrb = kb.sb("wrb", [128, NKC, 36], BF16)
        kb.op("dve", [wrf], [wrb], lambda e: e.tensor_copy(wrb[:, :, :], wrf[:, :, :]))

        psr = PsumRing(kb, 6)
        pT = kb.ps("pT", [128, NKC, 128], BF16)
        pT2 = kb.ps("pT2", [128, 4, 128], BF16)
        xr = Ring([kb.sb("x%d" % i, [128, D], F32) for i in range(3)])
        hbr = Ring([kb.sb("hb%d" % i, [128, D], BF16) for i in range(2)])
        hTr = Ring([kb.sb("hTt%d" % i, [128, NKC, 128], BF16) for i in range(2)])
        st6 = kb.sb("st6", [128, 12], F32)
        mv = kb.sb("mv", [128, 2], F32)
        rstd = kb.sb("rstd", [128, 1], F32)

        M1 = kb.sb("M1", [128, NT, 32], F32)
        M2 = kb.sb("M2", [128, NT, 32], F32)
        Mb = kb.sb("Mb", [128, NT, 32], BF16)
        rank = kb.sb("rank", [128, NT, 32], F32)
        gates = kb.sb("gates", [128, NT, 2], F32)
        destf = kb.sb("destf", [128, NT * 2], F32)
        desti = kb.sb("desti", [128, NT * 2], I32)
        lgr = Ring([kb.sb("lg%d" % i, [128, 36], F32) for i in range(2)])
        smr = Ring([kb.sb("smr%d" % i, [128, 48], F32) for i in range(2)])

        for i in range(NT):
            x = xr.next()
            hb = hbr.next()
            hTt = hTr.next()
            kb.dma("sp", x[:, :], h1[i * 128:(i + 1) * 128, :], [], [x])
            kb.op("act", [x], [hb], lambda e, x=x, hb=hb: e.copy(hb[:, :], x[:, :]))
            transpose_to(kb, hb, identb, pT, hTt[:, :, :], [hTt], NKC, eng="act")
            ps = psr.next()
            mm_group(kb, ps, ps[:, 0:36], [hTt[:, kc, :] for kc in range(NKC)],
                     [wrb[:, kc, :] for kc in range(NKC)], [hTt, wrb])
            lg = lgr.next()
            sm = smr.next()
            m1 = M1.trk(i)
            m2 = M2.trk(i)
            kb.op("dve", [ps, brs], [lg], lambda e, ps=ps, lg=lg: e.tensor_tensor(
                lg[:, :], ps[:, 0:36], brs[:, :], ALU.add))
            kb.op("dve", [lg], [sm], lambda e, lg=lg, sm=sm: e.reduce_max(sm[:, 0:1], lg[:, 0:4], axis=AX.X))
            kb.op("dve", [sm], [sm], lambda e, sm=sm: e.tensor_scalar(
                sm[:, 1:2], sm[:, 0:1], -1.0, None, ALU.mult))
            kb.op("act", [lg, sm], [sm], lambda e, lg=lg, sm=sm: e.activation(
                sm[:, 44:48], lg[:, 0:4], AF.Exp, bias=sm[:, 1:2], accum_out=sm[:, 2:3]))
            kb.op("dve", [sm], [sm], lambda e, sm=sm: e.reciprocal(sm[:, 3:4], sm[:, 2:3]))
            kb.op("dve", [lg, sm], [sm], lambda e, lg=lg, sm=sm: e.tensor_scalar(
                sm[:, 4:8], lg[:, 0:4], sm[:, 0:1], None, ALU.is_equal))
            kb.op("dve", [lg, sm], [sm], lambda e, lg=lg, sm=sm: e.tensor_scalar(
                sm[:, 8:16], lg[:, 4:12], sm[:, 4:5], None, ALU.mult))
            for g in range(1, 4):
                kb.op("dve", [lg, sm], [sm], lambda e, lg=lg, sm=sm, g=g: e.scalar_tensor_tensor(
                    sm[:, 8:16], lg[:, 4 + 8 * g:12 + 8 * g], sm[:, 4 + g:5 + g], sm[:, 8:16], ALU.mult, ALU.add))
            kb.op("dve", [sm], [sm], lambda e, sm=sm: e.max(sm[:, 16:24], sm[:, 8:16]))
            kb.op("dve", [sm], [sm], lambda e, sm=sm: e.tensor_scalar(
                sm[:, 24:32], sm[:, 8:16], sm[:, 16:17], None, ALU.is_equal))
            kb.op("dve", [sm], [sm], lambda e, sm=sm: e.tensor_scalar(
                sm[:, 32:40], sm[:, 8:16], sm[:, 17:18], None, ALU.is_equal))
            kb.op("dve", [sm], [sm], lambda e, sm=sm: e.tensor_tensor(
                sm[:, 40:41], sm[:, 17:18], sm[:, 16:17], ALU.subtract))
            kb.op("act", [sm], [sm], lambda e, sm=sm: e.activation(sm[:, 40:41], sm[:, 40:41], AF.Exp))
            kb.op("dve", [sm], [sm], lambda e, sm=sm: e.tensor_scalar(
                sm[:, 41:42], sm[:, 40:41], 1.0, None, ALU.add))
            kb.op("dve", [sm], [sm], lambda e, sm=sm: e.reciprocal(sm[:, 41:42], sm[:, 41:42]))
            kb.op("dve", [sm], [gates.trk(i)], lambda e, sm=sm, i=i: e.tensor_tensor(
                gates[:, i, 0:1], sm[:, 3:4], sm[:, 41:42], ALU.mult))
            kb.op("dve", [sm, gates.trk(i)], [gates.trk(i)], lambda e, sm=sm, i=i: e.tensor_tensor(
                gates[:, i, 1:2], gates[:, i, 0:1], sm[:, 40:41], ALU.mult))
            for g in range(4):
                kb.op("dve", [sm], [m1], lambda e, sm=sm, i=i, g=g: e.tensor_scalar(
                    M1[:, i, 8 * g:8 * g + 8], sm[:, 24:32], sm[:, 4 + g:5 + g], None, ALU.mult))
                kb.op("dve", [sm], [m2], lambda e, sm=sm, i=i, g=g: e.tensor_scalar(
                    M2[:, i, 8 * g:8 * g + 8], sm[:, 32:40], sm[:, 4 + g:5 + g], None, ALU.mult))
            kb.op("dve", [m1, m2], [Mb.trk(i)], lambda e, i=i: e.tensor_tensor(
                Mb[:, i, :], M1[:, i, :], M2[:, i, :], ALU.add))

        maccr = Ring([kb.sb("macc%d" % i, [128, 32], BF16) for i in range(2)])
        macc = maccr.next()
        kb.op("dve", [], [macc], lambda e, macc=macc: e.memset(macc[:, :], 0.0))
        for i in range(NT):
            ps = psr.next()

            def rk(e, ps=ps, i=i, macc=macc):
                e.matmul(ps[:, 0:32], utb[:, :], Mb[:, i, :], start=True, stop=False)
                return e.matmul(ps[:, 0:32], onesb[:, :], macc[:, :], start=False, stop=True)
            kb.op("pe", [utb, onesb, Mb.trk(i), macc], [ps], rk)
            kb.op("act", [ps], [rank.trk(i)], lambda e, ps=ps, i=i: e.copy(rank[:, i, :], ps[:, 0:32]))
            nm = maccr.next()
            kb.op("dve", [macc, Mb.trk(i)], [nm], lambda e, macc=macc, nm=nm, i=i: e.tensor_tensor(
                nm[:, :], macc[:, :], Mb[:, i, :], ALU.add))
            macc = nm
        ps = psr.next()
        kb.op("pe", [onesb, macc], [ps], lambda e, ps=ps, macc=macc: e.matmul(
            ps[:, 0:32], onesb[:, :], macc[:, :], start=True, stop=True))
        sc = kb.sb("sc", [128, 6, 32], F32)
        SC = [sc]
        kb.op("dve", [ps], SC, lambda e, ps=ps: e.tensor_copy(sc[:, 0, :], ps[:, 0:32]))
        sci = kb.sb("sci", [128, 2, 32], I32)
        kb.op("dve", SC, SC, lambda e: e.tensor_scalar(sc[:, 1, :], sc[:, 0, :], float(MOE_BS - 1), None, ALU.add))
        kb.op("dve", SC, [sci], lambda e: e.tensor_copy(sci[:, 0, :], sc[:, 1, :]))
        kb.op("dve", [sci], [sci], lambda e: e.tensor_scalar(
            sci[:, 1, :], sci[:, 0, :], MOE_SH, MOE_SH, ALU.arith_shift_right, ALU.logical_shift_left))
        kb.op("dve", [sci], SC, lambda e: e.tensor_copy(sc[:, 2, :], sci[:, 1, :]))
        kb.op("dve", SC, SC, lambda e: e.tensor_copy(sc[:, 3, :], sc[:, 2, :]))
        a, b = 3, 4
        for sft in (1, 2, 4, 8, 16):
            kb.op("dve", SC, SC, lambda e, a=a, b=b: e.tensor_copy(sc[:, b, :], sc[:, a, :]))
            kb.op("dve", SC, SC, lambda e, a=a, b=b, sft=sft: e.tensor_tensor(
                sc[:, b, sft:32], sc[:, a, sft:32], sc[:, a, 0:32 - sft], ALU.add))
            a, b = b, a
        pe_idx = a
        kb.op("dve", SC, SC, lambda e: e.tensor_tensor(sc[:, 5, :], sc[:, pe_idx, :], sc[:, 2, :], ALU.subtract))
        tmpd = Ring([kb.sb("tmpd%d" % i, [128, 32], F32) for i in range(2)])
        junk32 = kb.sb("junk32", [128, 2, 32], F32)
        for i in range(NT):
            td = tmpd.next()
            kb.op("dve", SC + [rank.trk(i)], [td], lambda e, td=td, i=i: e.tensor_tensor(
                td[:, :], rank[:, i, :], sc[:, 5, :], ALU.add))
            for jj, (MM, mt) in enumerate(((M1, M1.trk(i)), (M2, M2.trk(i)))):
                td2 = tmpd.next() if False else None
                kb.op("dve", [td, mt], [destf], lambda e, td=td, MM=MM, i=i, jj=jj: e.tensor_tensor(
                    junk32[:, jj, :], td[:, :], MM[:, i, :], ALU.mult))
                kb.op("dve", [destf], [destf], lambda e, i=i, jj=jj: e.reduce_sum(
                    destf[:, 2 * i + jj:2 * i + jj + 1], junk32[:, jj, :], axis=AX.X))
        kb.op("dve", [destf], [desti], lambda e: e.tensor_copy(desti[:, :], destf[:, :]))
        blkf = kb.sb("blkf", [128, NBLK], F32)
        kb.op("dve", [], [blkf], lambda e: e.memset(blkf[:, :], 0.0))
        for ex in range(32):
            kb.op("dve", SC + [b128, blkf], [blkf], lambda e, ex=ex: e.scalar_tensor_tensor(
                blkf[:, :], b128[:, :], sc[:, pe_idx, ex:ex + 1], blkf[:, :], ALU.is_ge, ALU.add))
        kb.op("dve", [blkf], [blkf], lambda e: e.tensor_scalar(blkf[:, :], blkf[:, :], 31.0, None, ALU.min))

        pidx = load_const(kb, "pidx_s", pidx_d, [128, 1])
        widx = kb.sb("widx", [128, NBLK], I32)
        kb.op("dve", [blkf, pidx], [blkf], lambda e: e.tensor_scalar(
            blkf[:, :], blkf[:, :], 128.0, pidx[:, 0:1], ALU.mult, ALU.add))
        kb.op("dve", [blkf], [widx], lambda e: e.tensor_copy(widx[:, :], blkf[:, :]))
        w1rows = w1.rearrange("e (p kk) n -> (e p) (kk n)", kk=8)
        w3rows = w3.rearrange("e (p kk) n -> (e p) (kk n)", kk=8)
        w2rows = w2.rearrange("e (p kk) n -> (e p) (kk n)", kk=4)

        zt = kb.sb("zt", [128, D], BF16)
        kb.op("pool", [], [zt], lambda e: e.memset(zt[:, :], 0.0))
        xz = Trk()
        for bq in range(NBLK * NSUB):
            kb.dma("sp", xbuf[bq * 128:(bq + 1) * 128, :], zt[:, :], [zt], [xz])
        xs = Trk()
        for i in range(NT):
            x = xr.next()
            hb = hbr.next()
            kb.dma("sp", x[:, :], h1[i * 128:(i + 1) * 128, :], [], [x])
            kb.op("act", [x], [hb], lambda e, x=x, hb=hb: e.copy(hb[:, :], x[:, :]))
            for jj in range(2):
                kb.dma_fn("pool", lambda e, hb=hb, i=i, jj=jj: e.indirect_dma_start(
                    out=xbuf, out_offset=bass.IndirectOffsetOnAxis(ap=desti[:, 2 * i + jj:2 * i + jj + 1], axis=0),
                    in_=hb[:, :], in_offset=None), [hb, desti, xz], [xs])

        w1s = Ring([kb.sb("w1s%d" % i, [128, NKC, 512], F32) for i in range(2)])
        w3s = Ring([kb.sb("w3s%d" % i, [128, NKC, 512], F32) for i in range(1)])
        w2s = Ring([kb.sb("w2s%d" % i, [128, 4, D], F32) for i in range(1)])
        w1b = Ring([kb.sb("w1b%d" % i, [128, NKC, 512], BF16) for i in range(2)])
        w3b = Ring([kb.sb("w3b%d" % i, [128, NKC, 512], BF16) for i in range(2)])
        w2b = Ring([kb.sb("w2b%d" % i, [128, 4, D], BF16) for i in range(2)])
        xbr = Ring([kb.sb("xb%d" % i, [128, D], BF16) for i in range(2)])
        xTr = Ring([kb.sb("xT%d" % i, [128, NKC, 128], BF16) for i in range(2)])
        sar = Ring([kb.sb("sA%d" % i, [128, 512], F32) for i in range(2)])
        gr = Ring([kb.sb("g%d" % i, [128, 512], BF16) for i in range(3)])
        gTr = Ring([kb.sb("gT%d" % i, [128, 4, 128], BF16) for i in range(2)])
        yr = Ring([kb.sb("y%d" % i, [128, D], F32) for i in range(2)])
        ys = Trk()
        cur_w = {}

        def stage1(sidx):
            bq, sub = divmod(sidx, NSUB)
            if sub == 0:
                ws1, ws3, ws2 = w1s.next(), w3s.next(), w2s.next()
                for wsx, wrows in ((ws1, w1rows), (ws3, w3rows), (ws2, w2rows)):
                    kb.dma_fn("pool", lambda e, wsx=wsx, wrows=wrows, bq=bq: e.indirect_dma_start(
                        out=wsx[:, :, :].rearrange("p a b -> p (a b)"), out_offset=None, in_=wrows,
                        in_offset=bass.IndirectOffsetOnAxis(ap=widx[:, bq:bq + 1], axis=0)), [widx], [wsx])
                wb1, wb3, wb2 = w1b.next(), w3b.next(), w2b.next()
                kb.op("dve", [ws1], [wb1], lambda e: e.tensor_copy(wb1[:, :, :], ws1[:, :, :]))
                kb.op("dve", [ws3], [wb3], lambda e: e.tensor_copy(wb3[:, :, :], ws3[:, :, :]))
                kb.op("act", [ws2], [wb2], lambda e: e.copy(wb2[:, :, :], ws2[:, :, :]))
                cur_w[bq] = (wb1, wb3, wb2)
            wb1, wb3, wb2 = cur_w[bq]
            r0 = sidx * 128
            xb = xbr.next()
            kb.dma("sp", xb[:, :], xbuf[r0:r0 + 128, :], [xs], [xb])
            xT = xTr.next()
            transpose_to(kb, xb, identb, pT, xT[:, :, :], [xT], NKC, eng="dve", strided=True)
            pa, pb = psr.next(), psr.next()
            mm_group(kb, pa, pa[:, :], [xT[:, kc, :] for kc in range(NKC)],
                     [wb1[:, kc, :] for kc in range(NKC)], [xT, wb1])
            mm_group(kb, pb, pb[:, :], [xT[:, kc, :] for kc in range(NKC)],
                     [wb3[:, kc, :] for kc in range(NKC)], [xT, wb3])
            sA = sar.next()
            gg = gr.next()
            kb.op("act", [pa], [sA], lambda e: e.activation(sA[:, :], pa[:, :], AF.Silu))
            kb.op("dve", [sA, pb], [gg], lambda e: e.tensor_tensor(gg[:, :], sA[:, :], pb[:, :], ALU.mult))
            return (r0, gg, wb2)

        def stage2(ctx):
            r0, gg, wb2 = ctx
            gT = gTr.next()

            def trg(e):
                ins = None
                for c in range(4):
                    ins = e.transpose(pT2[:, c, :], gg[:, slice(c, None, 4)], identb[:, :])
                return ins
            kb.op("pe", [gg, identb], [pT2], trg)
            kb.op("dve", [pT2], [gT], lambda e: e.tensor_copy(gT[:, :, :], pT2[:, :, :]))
            y = yr.next()
            for half in range(2):
                pc = psr.next()
                mm_group(kb, pc, pc[:, :], [gT[:, kc, :] for kc in range(4)],
                         [wb2[:, kc, 512 * half:512 * (half + 1)] for kc in range(4)], [gT, wb2])
                if half == 0:
                    kb.op("act", [pc], [y], lambda e, pc=pc: e.copy(y[:, 0:512], pc[:, :]))
                else:
                    kb.op("dve", [pc], [y], lambda e, pc=pc: e.tensor_copy(y[:, 512:1024], pc[:, :]))
            kb.dma("act", ybuf[r0:r0 + 128, :], y[:, :], [y], [ys])

        NS_ALL = NBLK * NSUB
        ctx = stage1(0)
        for sidx in range(NS_ALL):
            nctx = stage1(sidx + 1) if sidx + 1 < NS_ALL else None
            stage2(ctx)
            ctx = nctx

        y1r = yr
        y2r = Ring([kb.sb("y2_%d" % i, [128, D], F32) for i in range(2)])
        for i in range(NT):
            x = xr.next()
            y1, y2 = y1r.next(), y2r.next()
            kb.dma("sp", x[:, :], h1[i * 128:(i + 1) * 128, :], [], [x])
            for jj, yy in enumerate((y1, y2)):
                kb.dma_fn("pool", lambda e, yy=yy, i=i, jj=jj: e.indirect_dma_start(
                    out=yy[:, :], out_offset=None, in_=ybuf,
                    in_offset=bass.IndirectOffsetOnAxis(ap=desti[:, 2 * i + jj:2 * i + jj + 1], axis=0)),
                    [desti, ys], [yy])
            gt = gates.trk(i)
            kb.op("dve", [y1, gt], [y1], lambda e, y1=y1, i=i: e.tensor_scalar(
                y1[:, :], y1[:, :], gates[:, i, 0:1], None, ALU.mult))
            kb.op("dve", [y1, y2, gt], [y1], lambda e, y1=y1, y2=y2, i=i: e.scalar_tensor_tensor(
                y1[:, :], y2[:, :], gates[:, i, 1:2], y1[:, :], ALU.mult, ALU.add))
            kb.op("dve", [x, y1], [x], lambda e, x=x, y1=y1: e.scalar_tensor_tensor(
                x[:, :], x[:, :], DN_ALPHA, y1[:, :], ALU.mult, ALU.add))
            layer_norm_tile(kb, x, x, g_rep, b_rep, st6, mv, rstd)
            kb.dma("act", h2[i * 128:(i + 1) * 128, :], x[:, :], [x], [OUT])
        kb.finish()
    return nc


def build_LN0(T):
    nc = bass.Bass("TRN2", target_bir_lowering=False)
    with ExitStack() as st:
        kb = KB(nc, st)
        dt = nc.dram_tensor
        x_d = dt("x", [T, D], F32, kind="ExternalInput").ap()
        lng = dt("lng", [128, D], F32, kind="ExternalInput").ap()
        lnb = dt("lnb", [128, D], F32, kind="ExternalInput").ap()
        h0 = dt("h0", [T, D], F32, kind="ExternalOutput").ap()
        g_rep = load_const(kb, "g_rep", lng, [128, D])
        b_rep = load_const(kb, "b_rep", lnb, [128, D])
        xr = Ring([kb.sb("x%d" % i, [128, D], F32) for i in range(4)])
        st6 = kb.sb("st6", [128, 12], F32)
        mv = kb.sb("mv", [128, 2], F32)
        rstd = kb.sb("rstd", [128, 1], F32)
        for i in range(T // 128):
            x = xr.next()
            kb.dma("sp", x[:, :], x_d[i * 128:(i + 1) * 128, :], [], [x])
            layer_norm_tile(kb, x, x, g_rep, b_rep, st6, mv, rstd)
            kb.dma("act", h0[i * 128:(i + 1) * 128, :], x[:, :], [x], [kb.out_trk])
        kb.finish()
    return nc


_PROGS = {}


def _prog(name, fn, *args):
    key = (name,) + args
    if key not in _PROGS:
        _PROGS[key] = fn(*args)
    return _PROGS[key]


def _lay(a):
    J = a.shape[0]
    return np.ascontiguousarray(a.reshape(J, 8, 128).transpose(2, 0, 1).reshape(128, J * 8))


def _rep(a):
    return np.ascontiguousarray(np.broadcast_to(np.asarray(a, np.float32).reshape(1, -1), (128, a.size)))


def kernel(x, ln0_g, ln0_b, rel_table, w_in, gate_b, lam_vecs, subln_g, w_attn_proj, conv_w,
           w_conv_proj, w_out, ln1_g, ln1_b, w_rg, b_rg, w_re, b_re, w1, w3, w2, ln2_g, ln2_b):
    f32 = np.float32
    x = np.asarray(x, f32)
    B, S, _ = x.shape
    NC = 8
    T = B * S // NC
    CPS = S // T
    cores = list(range(NC))
    depth = w_in.shape[0]
    ident = np.eye(128, dtype=f32)
    xt = x.reshape(B * S, D)

    def run(nc, maps):
        return run_bass_kernel_spmd(nc, maps, core_ids=cores).results

    res = run(_prog("LN0", build_LN0, T),
              [{"x": xt[c * T:(c + 1) * T], "lng": _rep(ln0_g), "lnb": _rep(ln0_b)} for c in cores])
    h = [r["h0"] for r in res]

    oh = bias_onehot()
    NBLK = (2 * T + 32 * (MOE_BS - 1) + MOE_BS - 1) // MOE_BS
    utri = np.triu(np.ones((128, 128), f32), 1)
    blk128 = np.ascontiguousarray(np.broadcast_to(np.arange(NBLK, dtype=f32) * MOE_BS, (128, NBLK)))
    pidx = np.arange(128, dtype=f32).reshape(128, 1)

    for l in range(depth):
        lam_init = 0.8 - 0.6 * math.exp(-0.3 * l)
        maps = []
        for c in cores:
            first = (c % CPS == 0)
            halo = np.zeros((128, D), f32) if first else h[c - 1][T - 128:]
            maps.append({"hin": np.concatenate([halo, h[c]], 0),
                         "halo_mask": np.full((128, 1), 0.0 if first else 1.0, f32),
                         "w_in": np.asarray(w_in[l], f32), "gate_b": _lay(np.asarray(gate_b[l], f32)),
                         "conv_w": _lay(np.asarray(conv_w[l], f32)),
                         "w_cp": np.asarray(w_conv_proj[l], f32), "ident": ident})
        ra = run(_prog("A", build_A, T, False), maps)
        maps = []
        for c in cores:
            b, hp = c // CPS, c % CPS
            src = [ra[b * CPS + t] for t in range(CPS)]
            rows = slice(hp * 256, (hp + 1) * 256)
            tab = np.concatenate([np.asarray(rel_table, f32)[:, 2 * hp:2 * hp + 2],
                                  np.full((1, 2), -30000.0, f32)], 0)
            maps.append({"qT": np.concatenate([r["qT"][rows] for r in src], 1),
                         "kT": np.concatenate([r["kT"][rows] for r in src], 1),
                         "v": np.concatenate([r["v"][:, rows] for r in src], 0),
                         "oh": oh, "tab": tab,
                         "lamv": _rep(np.asarray(lam_vecs[l], f32).reshape(-1)),
                         "subg": _rep(subln_g[l]),
                         "lamc": np.ascontiguousarray(np.broadcast_to(
                             np.array([[-lam_init, 1.0 - lam_init]], f32), (128, 2))),
                         "ident": ident})
        rt = run(_prog("ATT", build_ATT, S), maps)
        maps = []
        for c in cores:
            b, t = c // CPS, c % CPS
            oT = np.concatenate([rt[b * CPS + hp]["oT"][:, t * T:(t + 1) * T] for hp in range(CPS)], 0)
            maps.append({"oT": oT, "SaT": ra[c]["SaT"], "YT": ra[c]["YT"], "h": h[c],
                         "w_ap": np.asarray(w_attn_proj[l], f32), "w_out": np.asarray(w_out[l], f32),
                         "lng": _rep(ln1_g[l]), "lnb": _rep(ln1_b[l])})
        r1 = run(_prog("C1", build_C1, T), maps)
        w_r = np.concatenate([np.asarray(w_rg[l], f32), np.asarray(w_re[l], f32)], 1)
        b_r = _rep(np.concatenate([np.asarray(b_rg[l], f32), np.asarray(b_re[l], f32)]))
        w1l, w3l, w2l = np.asarray(w1[l], f32), np.asarray(w3[l], f32), np.asarray(w2[l], f32)
        maps = [{"h1": r1[c]["h1"], "w_r": w_r, "b_r": b_r, "w1": w1l, "w3": w3l, "w2": w2l,
                 "lng": _rep(ln2_g[l]), "lnb": _rep(ln2_b[l]), "ident": ident, "utri": utri,
                 "blk128": blk128, "pidx": pidx} for c in cores]
        r2 = run(_prog("C2", build_C2, T), maps)
        h = [r["h2"] for r in r2]
    return np.concatenate(h, 0).reshape(B, S, D).astype(f32)
```

```python
import math
from contextlib import ExitStack
import numpy as np
import concourse.bass as bass
import concourse.mybir as mybir
from concourse.bass_utils import run_bass_kernel_spmd

F32 = mybir.dt.float32
BF16 = mybir.dt.bfloat16
I32 = mybir.dt.int32
AF = mybir.ActivationFunctionType
ALU = mybir.AluOpType
AX = mybir.AxisListType


class Trk:
    __slots__ = ("w", "r")

    def __init__(self):
        self.w = None
        self.r = {}


class Buf(Trk):
    __slots__ = ("t", "subs")

    def __init__(self, t):
        Trk.__init__(self)
        self.t = t
        self.subs = {}

    def __getitem__(self, idx):
        return self.t[idx]

    def trk(self, key):
        s = self.subs.get(key)
        if s is None:
            s = self.subs[key] = Trk()
        return s


class KB:
    NDMA = 32

    def __init__(self, nc, st):
        self.nc = nc
        self.st = st
        self.eng = {"pe": nc.tensor, "act": nc.scalar, "dve": nc.vector,
                    "pool": nc.gpsimd, "sp": nc.sync}
        self.sem = {}
        self.cnt = {}
        self.known = {k: {} for k in self.eng}
        for k in ("pe", "act", "dve", "pool"):
            self.sem[k] = st.enter_context(nc.semaphore("s_" + k))
            self.cnt[k] = 0
        self.dsem = []
        for i in range(self.NDMA):
            key = "d%d" % i
            self.sem[key] = st.enter_context(nc.semaphore("s_" + key))
            self.cnt[key] = 0
            self.dsem.append(key)
        self.dnext = 0
        self._clear_sems()
        self.out_trk = Trk()
        self.prog = {k: [] for k in self.eng}
        self.nbuf = 0

    def _clear_sems(self):
        for h in self.sem.values():
            self.nc.gpsimd.sem_clear(h)
        self.nc.all_engine_barrier()

    def sb(self, name, shape, dtype):
        t = self.st.enter_context(self.nc.sbuf_tensor(name, list(shape), dtype))
        return Buf(t)

    def ps(self, name, shape, dtype):
        t = self.st.enter_context(self.nc.psum_tensor(name, list(shape), dtype))
        return Buf(t)

    def _wait(self, ename, deps):
        kn = self.known[ename]
        best = {}
        for d in deps:
            if d is None:
                continue
            k, v = d
            if best.get(k, 0) < v:
                best[k] = v
        waits = []
        for k, v in best.items():
            if k == "pe" and ename == "pe":
                continue
            if kn.get(k, 0) >= v:
                continue
            waits.append((k, v))
            kn[k] = v
        return waits

    def _deps(self, reads, writes):
        deps = []
        for b in reads:
            deps.append(b.w)
        for b in writes:
            deps.append(b.w)
            for k, v in b.r.items():
                deps.append((k, v))
        return deps

    def _commit(self, tok, reads, writes):
        k, v = tok
        for b in reads:
            if b.r.get(k, 0) < v:
                b.r[k] = v
        for b in writes:
            b.w = tok
            b.r = {}

    def op(self, ename, reads, writes, fn):
        waits = self._wait(ename, self._deps(reads, writes))
        self.cnt[ename] += 1
        self.prog[ename].append((waits, fn, ename, 1))
        tok = (ename, self.cnt[ename])
        self._commit(tok, reads, writes)
        return tok

    def dma_fn(self, qname, fn, reads, writes):
        key = self.dsem[self.dnext]
        self.dnext = (self.dnext + 1) % self.NDMA
        deps = self._deps(reads, writes)
        if self.cnt[key] > 0:
            deps.append((key, self.cnt[key]))
        waits = self._wait(qname, deps)
        self.cnt[key] += 16
        self.prog[qname].append((waits, fn, key, 16))
        tok = (key, self.cnt[key])
        self._commit(tok, reads, writes)
        return tok

    def dma(self, qname, out, in_, reads, writes, **kw):
        return self.dma_fn(qname, lambda e: e.dma_start(out=out, in_=in_, **kw), reads, writes)

    def finish(self):
        deps = [self.out_trk.w]
        for key in self.dsem:
            if self.cnt[key] > 0:
                deps.append((key, self.cnt[key]))
        for k in ("pe", "act", "dve", "pool"):
            if self.cnt[k] > 0:
                deps.append((k, self.cnt[k]))
        final_waits = self._wait("sp", deps)
        sem = self.sem
        prog = self.prog

        def emit(e, items, tail=()):
            for waits, fn, ik, iv in items:
                for k, v in waits:
                    e.wait_ge(sem[k], v)
                fn(e).then_inc(sem[ik], iv)
            for k, v in tail:
                e.wait_ge(sem[k], v)

        with self.nc.Block() as block:
            @block.tensor
            def _(e):
                emit(e, prog["pe"])

            @block.scalar
            def _(e):
                emit(e, prog["act"])

            @block.vector
            def _(e):
                emit(e, prog["dve"])

            @block.gpsimd
            def _(e):
                emit(e, prog["pool"])

            @block.sync
            def _(e):
                emit(e, prog["sp"], final_waits)

        self._clear_sems()


D = 1024
NKC = 8
LN_EPS = 1e-5
DN_ALPHA = 4 ** 0.25
MOE_BS = 256
MOE_SH = 8


class PsumRing:
    def __init__(self, kb, n, name="ps"):
        self.bufs = [kb.ps("%s%d" % (name, i), [128, 512], F32) for i in range(n)]
        self.i = 0

    def next(self):
        b = self.bufs[self.i]
        self.i = (self.i + 1) % len(self.bufs)
        return b


class Ring:
    def __init__(self, bufs):
        self.bufs = bufs
        self.i = 0

    def next(self):
        b = self.bufs[self.i]
        self.i = (self.i + 1) % len(self.bufs)
        return b


def mm_group(kb, ps, out_ap, lhs, rhs, reads):
    n = len(lhs)

    def fn(e):
        ins = None
        for k in range(n):
            ins = e.matmul(out_ap, lhs[k], rhs[k], start=(k == 0), stop=(k == n - 1))
        return ins
    return kb.op("pe", reads, [ps], fn)


def layer_norm_tile(kb, x, xo, g_rep, b_rep, st6, mv, rstd):
    def stats(e):
        e.bn_stats(st6[:, 0:6], x[:, 0:512])
        return e.bn_stats(st6[:, 6:12], x[:, 512:1024])
    kb.op("dve", [x], [st6], stats)
    kb.op("dve", [st6], [mv], lambda e: e.bn_aggr(mv[:, :], st6[:, :]))
    kb.op("dve", [mv], [rstd], lambda e: e.tensor_scalar(
        rstd[:, :], mv[:, 1:2], LN_EPS, None, ALU.add))
    kb.op("act", [rstd], [rstd], lambda e: e.activation(rstd[:, :], rstd[:, :], AF.Sqrt))
    kb.op("dve", [rstd], [rstd], lambda e: e.reciprocal(rstd[:, :], rstd[:, :]))
    kb.op("dve", [x, mv, rstd], [xo], lambda e: e.tensor_scalar(
        xo[:, :], x[:, :], mv[:, 0:1], rstd[:, 0:1], ALU.subtract, ALU.mult))
    kb.op("dve", [xo, g_rep], [xo], lambda e: e.tensor_tensor(
        xo[:, :], xo[:, :], g_rep[:, :], ALU.mult))
    kb.op("dve", [xo, b_rep], [xo], lambda e: e.tensor_tensor(
        xo[:, :], xo[:, :], b_rep[:, :], ALU.add))


def load_const(kb, name, dram_ap, shape, dtype=F32, q="sp"):
    b = kb.sb(name, shape, dtype)
    kb.dma(q, b[tuple(slice(None) for _ in shape)], dram_ap, [], [b])
    return b


def transpose_to(kb, src, identb, pT, dst_ap, dst_trks, nch, eng="act", strided=False):
    def tr(e):
        ins = None
        for c in range(nch):
            sl = slice(c, None, nch) if strided else slice(c * 128, (c + 1) * 128)
            ins = e.transpose(pT[:, c, :], src[:, sl], identb[:, :])
        return ins
    kb.op("pe", [src, identb], [pT], tr)
    if eng == "act":
        kb.op("act", [pT], dst_trks, lambda e: e.copy(dst_ap, pT[:, 0:nch, :]))
    else:
        kb.op("dve", [pT], dst_trks, lambda e: e.tensor_copy(dst_ap, pT[:, 0:nch, :]))


def build_A(T, layer0):
    nc = bass.Bass("TRN2", target_bir_lowering=False)
    TH = T + 128
    NB = T // 512
    NT = T // 128
    with ExitStack() as st:
        kb = KB(nc, st)
        dt = nc.dram_tensor
        hin = dt("hin", [TH, D], F32, kind="ExternalInput").ap()
        halo_mask = dt("halo_mask", [128, 1], F32, kind="ExternalInput").ap()
        w_in = dt("w_in", [D, 8192], F32, kind="ExternalInput").ap()
        gate_b = dt("gate_b", [128, 16], F32, kind="ExternalInput").ap()
        conv_w = dt("conv_w", [128, 24], F32, kind="ExternalInput").ap()
        w_cp = dt("w_cp", [D, D], F32, kind="ExternalInput").ap()
        ident = dt("ident", [128, 128], F32, kind="ExternalInput").ap()
        if layer0:
            lng = dt("lng", [128, D], F32, kind="ExternalInput").ap()
            lnb = dt("lnb", [128, D], F32, kind="ExternalInput").ap()
            h0 = dt("h0", [T, D], F32, kind="ExternalOutput").ap()
        qT = dt("qT", [D, T], BF16, kind="ExternalOutput").ap()
        kT = dt("kT", [D, T], BF16, kind="ExternalOutput").ap()
        v = dt("v", [T, D], BF16, kind="ExternalOutput").ap()
        SaT = dt("SaT", [D, T], BF16, kind="ExternalOutput").ap()
        YT = dt("YT", [D, T], BF16, kind="ExternalOutput").ap()
        OUT = kb.out_trk

        identf = load_const(kb, "identf", ident, [128, 128])
        identb = kb.sb("identb", [128, 128], BF16)
        kb.op("dve", [identf], [identb], lambda e: e.tensor_copy(identb[:, :], identf[:, :]))
        gb = load_const(kb, "gb", gate_b, [128, 16])
        cw = load_const(kb, "cw", conv_w, [128, 24])
        hm = load_const(kb, "hm", halo_mask, [128, 1])
        if layer0:
            g_rep = load_const(kb, "g_rep", lng, [128, D])
            b_rep = load_const(kb, "b_rep", lnb, [128, D])

        hT = kb.sb("hT", [128, NKC, TH], BF16)
        pT = kb.ps("pT", [128, NKC, 128], BF16)
        psr = PsumRing(kb, 6)

        xr = Ring([kb.sb("x%d" % i, [128, D], F32) for i in range(3)])
        hbr = Ring([kb.sb("hb%d" % i, [128, D], BF16) for i in range(2)])
        st6 = kb.sb("st6", [128, 12], F32)
        mv = kb.sb("mv", [128, 2], F32)
        rstd = kb.sb("rstd", [128, 1], F32)
        for i in range(TH // 128):
            x = xr.next()
            hb = hbr.next()
            kb.dma("sp", x[:, :], hin[i * 128:(i + 1) * 128, :], [], [x])
            if layer0:
                layer_norm_tile(kb, x, x, g_rep, b_rep, st6, mv, rstd)
                if i >= 1:
                    kb.dma("pool", h0[(i - 1) * 128:i * 128, :], x[:, :], [x], [OUT])
            kb.op("act", [x], [hb], lambda e, x=x, hb=hb: e.copy(hb[:, :], x[:, :]))
            transpose_to(kb, hb, identb, pT, hT[:, :, i * 128:(i + 1) * 128], [hT.trk(i)], NKC,
                         eng="dve" if i % 2 else "act")

        def hT_blk(n):
            return [hT.trk(1 + 4 * n + a) for a in range(4)]

        wst = Ring([kb.sb("wst%d" % i, [128, NKC, 512], F32) for i in range(1)])
        wbf = Ring([kb.sb("wbf%d" % i, [128, NKC, 512], BF16) for i in range(2)])

        wci = [0]

        def load_w(srcs):
            ws = wst.next()
            wb = wbf.next()
            c0 = 0
            for ap in srcs:
                nco = ap.shape[1]
                kb.dma("sp", ws[:, :, c0:c0 + nco], ap.rearrange("(kc p) n -> p kc n", p=128),
                       [], [ws])
                c0 += nco
            wci[0] += 1
            if wci[0] % 2:
                kb.op("act", [ws], [wb], lambda e, ws=ws, wb=wb, c0=c0: e.copy(
                    wb[:, :, 0:c0], ws[:, :, 0:c0]))
            else:
                kb.op("dve", [ws], [wb], lambda e, ws=ws, wb=wb, c0=c0: e.tensor_copy(
                    wb[:, :, 0:c0], ws[:, :, 0:c0]))
            return wb

        evi = [0]

        def evac(ps, out_ap, out_trks, src_ap, extra_reads=()):
            evi[0] += 1
            if evi[0] % 2:
                kb.op("act", [ps] + list(extra_reads), out_trks, lambda e: e.copy(out_ap, src_ap))
            else:
                kb.op("dve", [ps] + list(extra_reads), out_trks, lambda e: e.tensor_copy(out_ap, src_ap))

        ostage = Ring([kb.sb("ost%d" % i, [128, T], BF16) for i in range(2)])

        for p in range(4):
            wb = load_w([w_in[:, p * 512:(p + 1) * 512]])
            for cc in range(4):
                ch = p * 4 + cc
                og = ostage.next()
                for n in range(NB):
                    ps = psr.next()
                    mm_group(kb, ps, ps[:, :],
                             [wb[:, kc, cc * 128:(cc + 1) * 128] for kc in range(NKC)],
                             [hT[:, kc, 128 + 512 * n:128 + 512 * (n + 1)] for kc in range(NKC)],
                             [wb] + hT_blk(n))
                    evac(ps, og[:, 512 * n:512 * (n + 1)], [og], ps[:, :])
                dst = qT if ch < 8 else kT
                r0 = (ch % 8) * 128
                kb.dma("pool", dst[r0:r0 + 128, :], og[:, :], [og], [OUT])

        vstage = Ring([kb.sb("vst%d" % i, [128, 512], BF16) for i in range(3)])
        for p in range(2):
            wb = load_w([w_in[:, 2048 + p * 512:2048 + (p + 1) * 512]])
            for i in range(NT):
                ps = psr.next()
                mm_group(kb, ps, ps[:, :],
                         [hT[:, kc, 128 + 128 * i:128 + 128 * (i + 1)] for kc in range(NKC)],
                         [wb[:, kc, :] for kc in range(NKC)],
                         [wb, hT.trk(1 + i)])
                vs = vstage.next()
                evac(ps, vs[:, :], [vs], ps[:, :])
                kb.dma("pool", v[i * 128:(i + 1) * 128, p * 512:(p + 1) * 512], vs[:, :], [vs], [OUT])

        NH = 2 if T >= 1024 else 1
        TH2 = T // NH
        NB2 = TH2 // 512
        gT = kb.sb("gT", [128, NKC, TH2], BF16)
        uT = kb.sb("uT", [128, TH2 + 2], F32)
        cbT = kb.sb("cbT", [128, TH2], F32)
        yacc = kb.sb("yacc", [128, TH2], F32)
        tmpr = Ring([kb.sb("tmp%d" % i, [128, 512], F32) for i in range(2)])
        for hf in range(NH):
            t0 = hf * TH2
            for j in range(8):
                wb = load_w([w_in[:, 3072 + 128 * j:3072 + 128 * (j + 1)],
                             w_in[:, 4096 + 128 * j:4096 + 128 * (j + 1)],
                             w_in[:, 5120 + 128 * j:5120 + 128 * (j + 1)]])
                ps = psr.next()
                hc = 128 + t0
                htr = [hT.trk(0)] if hf == 0 else [hT.trk((hc - 2) // 128)]

                def halo_mm(e, ps=ps, wb=wb, hc=hc):
                    ins = None
                    for g in range(2):
                        for kc in range(NKC):
                            ins = e.matmul(ps[:, 2 * g:2 * g + 2], wb[:, kc, 128 * (g + 1):128 * (g + 2)],
                                           hT[:, kc, hc - 2:hc], start=(kc == 0), stop=(kc == NKC - 1))
                    return ins
                kb.op("pe", [wb] + htr, [ps], halo_mm)
                tm = tmpr.next()
                kb.op("act", [ps], [tm], lambda e, ps=ps, tm=tm: e.copy(tm[:, 0:2], ps[:, 0:2]))
                kb.op("dve", [ps, tm], [tm], lambda e, ps=ps, tm=tm: e.tensor_tensor(
                    tm[:, 2:4], tm[:, 0:2], ps[:, 2:4], ALU.mult))
                if hf == 0:
                    kb.op("dve", [tm, hm], [uT], lambda e, tm=tm: e.tensor_scalar(
                        uT[:, 0:2], tm[:, 2:4], hm[:, 0:1], None, ALU.mult))
                else:
                    kb.op("dve", [tm], [uT], lambda e, tm=tm: e.tensor_copy(uT[:, 0:2], tm[:, 2:4]))
                for n2 in range(NB2):
                    n = hf * NB2 + n2
                    rhs = [hT[:, kc, 128 + 512 * n:128 + 512 * (n + 1)] for kc in range(NKC)]
                    pcb, pcc, pcx = psr.next(), psr.next(), psr.next()
                    for g, ps in enumerate((pcb, pcc, pcx)):
                        mm_group(kb, ps, ps[:, :], [wb[:, kc, 128 * g:128 * (g + 1)] for kc in range(NKC)],
                                 rhs, [wb] + hT_blk(n))
                    tm = tmpr.next()
                    kb.op("act", [pcc], [tm], lambda e, pcc=pcc, tm=tm: e.copy(tm[:, :], pcc[:, :]))
                    kb.op("dve", [tm, pcx], [uT], lambda e, tm=tm, pcx=pcx, n2=n2: e.tensor_tensor(
                        uT[:, 2 + 512 * n2:2 + 512 * (n2 + 1)], tm[:, :], pcx[:, :], ALU.mult))
                    kb.op("act", [pcb], [cbT], lambda e, pcb=pcb, n2=n2: e.copy(
                        cbT[:, 512 * n2:512 * (n2 + 1)], pcb[:, :]))
                kb.op("dve", [uT, cw], [yacc], lambda e, j=j: e.tensor_scalar(
                    yacc[:, :], uT[:, 2:TH2 + 2], cw[:, 16 + j:17 + j], None, ALU.mult))
                kb.op("dve", [uT, cw, yacc], [yacc], lambda e, j=j: e.scalar_tensor_tensor(
                    yacc[:, :], uT[:, 1:TH2 + 1], cw[:, 8 + j:9 + j], yacc[:, :], ALU.mult, ALU.add))
                kb.op("dve", [uT, cw, yacc], [yacc], lambda e, j=j: e.scalar_tensor_tensor(
                    yacc[:, :], uT[:, 0:TH2], cw[:, j:j + 1], yacc[:, :], ALU.mult, ALU.add))
                kb.op("dve", [yacc, cbT], [gT.trk(j)], lambda e, j=j: e.tensor_tensor(
                    gT[:, j, :], yacc[:, :], cbT[:, :], ALU.mult))

            gT_all = [gT.trk(j) for j in range(8)]
            for j in range(8):
                wb = load_w([w_in[:, 7168 + 128 * j:7168 + 128 * (j + 1)], w_cp[:, 128 * j:128 * (j + 1)]])
                og = ostage.next()
                for n2 in range(NB2):
                    n = hf * NB2 + n2
                    pyc, pgc = psr.next(), psr.next()
                    mm_group(kb, pyc, pyc[:, :], [wb[:, kc, 128:256] for kc in range(NKC)],
                             [gT[:, kc, 512 * n2:512 * (n2 + 1)] for kc in range(NKC)], [wb] + gT_all)
                    mm_group(kb, pgc, pgc[:, :], [wb[:, kc, 0:128] for kc in range(NKC)],
                             [hT[:, kc, 128 + 512 * n:128 + 512 * (n + 1)] for kc in range(NKC)],
                             [wb] + hT_blk(n))
                    tm = tmpr.next()
                    kb.op("act", [pgc, gb], [tm], lambda e, pgc=pgc, tm=tm, j=j: e.activation(
                        tm[:, :], pgc[:, :], AF.Sigmoid, bias=gb[:, 8 + j:9 + j]))
                    kb.op("dve", [tm, pyc], [og], lambda e, tm=tm, pyc=pyc, og=og, n2=n2: e.tensor_tensor(
                        og[:, 512 * n2:512 * (n2 + 1)], tm[:, :], pyc[:, :], ALU.mult))
                kb.dma("pool", YT[128 * j:128 * (j + 1), t0:t0 + TH2], og[:, 0:TH2], [og], [OUT])

        for p in range(2):
            wb = load_w([w_in[:, 6144 + p * 512:6144 + (p + 1) * 512]])
            for cc in range(4):
                j = p * 4 + cc
                og = ostage.next()
                for n in range(NB):
                    ps = psr.next()
                    mm_group(kb, ps, ps[:, :],
                             [wb[:, kc, cc * 128:(cc + 1) * 128] for kc in range(NKC)],
                             [hT[:, kc, 128 + 512 * n:128 + 512 * (n + 1)] for kc in range(NKC)],
                             [wb] + hT_blk(n))
                    kb.op("act", [ps, gb], [og], lambda e, ps=ps, og=og, n=n, j=j: e.activation(
                        og[:, 512 * n:512 * (n + 1)], ps[:, :], AF.Sigmoid, bias=gb[:, j:j + 1]))
                kb.dma("pool", SaT[128 * j:128 * (j + 1), :], og[:, :], [og], [OUT])
        kb.finish()
    return nc


FL = 1151
FP = FL + 1


def rel_bucket_np(rel):
    n = np.maximum(rel, 0)
    nf = np.maximum(n, 1).astype(np.float32)
    large = 16 + (np.log(nf / np.float32(16)) / np.float32(math.log(128 / 16)) * np.float32(16)).astype(np.int32)
    large = np.minimum(large, 31)
    return np.where(n < 16, n, large)


def bias_onehot():
    rel = np.arange(FL) - 511
    b = rel_bucket_np(rel)
    oh = np.zeros((33, FL), np.float32)
    for i in range(FL):
        if rel[i] < 0:
            oh[32, i] = 1.0
        else:
            oh[b[i], i] = 1.0
    return oh


def build_ATT(S):
    nc = bass.Bass("TRN2", target_bir_lowering=False)
    NQB = S // 512
    NKCH = S // 128
    with ExitStack() as st:
        kb = KB(nc, st)
        dt = nc.dram_tensor
        qT = dt("qT", [256, S], BF16, kind="ExternalInput").ap()
        kT = dt("kT", [256, S], BF16, kind="ExternalInput").ap()
        v = dt("v", [S, 256], BF16, kind="ExternalInput").ap()
        oh = dt("oh", [33, FL], F32, kind="ExternalInput").ap()
        tab = dt("tab", [33, 2], F32, kind="ExternalInput").ap()
        lamv = dt("lamv", [128, 256], F32, kind="ExternalInput").ap()
        subg = dt("subg", [128, 128], F32, kind="ExternalInput").ap()
        lamc_d = dt("lamc", [128, 2], F32, kind="ExternalInput").ap()
        ident = dt("ident", [128, 128], F32, kind="ExternalInput").ap()
        oT = dt("oT", [256, S], BF16, kind="ExternalOutput").ap()
        fscr = [nc.dram_tensor("fscr%d" % h, [128 * FP], F32) for h in range(2)]
        OUT = kb.out_trk

        identf = load_const(kb, "identf", ident, [128, 128])
        identb = kb.sb("identb", [128, 128], BF16)
        kb.op("dve", [identf], [identb], lambda e: e.tensor_copy(identb[:, :], identf[:, :]))
        ohs = load_const(kb, "ohs", oh, [33, FL])
        tabs = load_const(kb, "tabs", tab, [33, 2])
        lv = load_const(kb, "lv", lamv, [128, 256])
        g_rep = load_const(kb, "g_rep", subg, [128, 128])
        lamc = load_const(kb, "lamc_s", lamc_d, [128, 2])
        kb.op("dve", [g_rep, lamc], [g_rep], lambda e: e.tensor_scalar(
            g_rep[:, :], g_rep[:, :], lamc[:, 1:2], None, ALU.mult))
        epst = kb.sb("epst", [128, 1], F32)
        kb.op("dve", [], [epst], lambda e: e.memset(epst[:, :], LN_EPS))

        lt = kb.sb("lt", [128, 128], F32)
        ls = kb.sb("ls", [128, 2], F32)
        nlam = kb.sb("nlam", [128, 1], F32)
        kb.op("dve", [lv], [lt], lambda e: e.tensor_tensor(
            lt[:, 0:64], lv[:, 0:64], lv[:, 64:128], ALU.mult))
        kb.op("dve", [lv, lt], [lt], lambda e: e.tensor_tensor(
            lt[:, 64:128], lv[:, 128:192], lv[:, 192:256], ALU.mult))
        kb.op("dve", [lt], [ls], lambda e: e.reduce_sum(
            ls[:, 0:2], lt[:, :].rearrange("p (a b) -> p a b", a=2), axis=AX.X))
        kb.op("act", [ls], [ls], lambda e: e.activation(ls[:, :], ls[:, :], AF.Exp))
        kb.op("dve", [ls], [nlam], lambda e: e.tensor_tensor(
            nlam[:, :], ls[:, 1:2], ls[:, 0:1], ALU.subtract))
        kb.op("dve", [nlam, lamc], [nlam], lambda e: e.tensor_scalar(
            nlam[:, :], nlam[:, :], lamc[:, 0:1], None, ALU.add))

        s12 = Ring([kb.ps("s12_%d" % i, [128, 1024], F32) for i in range(2)])
        oacc = kb.ps("oacc", [128, 3, 512], F32)
        pT = kb.ps("pT", [128, 4, 128], BF16)

        def acc_ap(h2, qc):
            g = h2 * 4 + qc
            return oacc[:, g // 3, (g % 3) * 129:(g % 3) * 129 + 129]

        kTs = kb.sb("kTs", [128, S], BF16)
        vaug = kb.sb("vaug", [128, NKCH, 129], BF16)
        tabrep = kb.sb("tabrep", [33, 128], F32)
        frep = kb.sb("frep", [128, FL], F32)
        c31 = kb.sb("c31", [128, 1], F32)
        biasT = [kb.sb("biasT%d" % d, [128, 512], F32) for d in range(5)]
        qr = Ring([kb.sb("qb%d" % i, [128, 512], BF16) for i in range(3)])
        pr = Ring([kb.sb("pb%d" % i, [128, 1024], BF16) for i in range(3)])
        tmpr = Ring([kb.sb("tm%d" % i, [128, 1024], F32) for i in range(2)])
        osb = Ring([kb.sb("osb%d" % i, [128, 8, 132], F32) for i in range(2)])
        ostage = Ring([kb.sb("ostg%d" % i, [128, 512], BF16) for i in range(2)])
        ofr = Ring([kb.sb("of%d" % i, [128, 4, 128], F32) for i in range(3)])
        onr = Ring([kb.sb("on%d" % i, [128, 4, 128], BF16) for i in range(2)])
        smr = Ring([kb.sb("sm%d" % i, [128, 16], F32) for i in range(3)])
        junk = kb.sb("junk", [128, 128], F32)

        for hh in range(2):
            kb.dma("sp", kTs[:, :], kT[hh * 128:(hh + 1) * 128, :], [], [kTs])
            kb.dma("sp", vaug[:, :, 0:128],
                   v[:, hh * 128:(hh + 1) * 128].rearrange("(c p) e -> p c e", p=128), [], [vaug])
            kb.op("pool", [], [vaug], lambda e: e.memset(vaug[:, :, 128:129], 1.0))
            kb.op("dve", [tabs], [tabrep], lambda e, hh=hh: e.tensor_copy(
                tabrep[:, :], tabs[:, hh:hh + 1].to_broadcast([33, 128])))
            ps = s12.next()
            ps2 = s12.next()

            def fmm(e, ps=ps, ps2=ps2):
                e.matmul(ps[:, 0:512], tabrep[:, :], ohs[:, 0:512], start=True, stop=True)
                e.matmul(ps[:, 512:1024], tabrep[:, :], ohs[:, 512:1024], start=True, stop=True)
                return e.matmul(ps2[:, 0:FL - 1024], tabrep[:, :], ohs[:, 1024:FL], start=True, stop=True)
            kb.op("pe", [tabrep, ohs], [ps, ps2], fmm)
            kb.op("dve", [ps], [frep], lambda e, ps=ps: e.tensor_copy(frep[:, 0:1024], ps[:, :]))
            kb.op("dve", [ps2, frep], [frep], lambda e, ps2=ps2: e.tensor_copy(
                frep[:, 1024:FL], ps2[:, 0:FL - 1024]))
            kb.op("dve", [frep], [c31], lambda e: e.tensor_copy(c31[:, :], frep[:, FL - 1:FL]))
            ftrk = Trk()
            kb.dma("sp", bass.AP(fscr[hh], 0, [[FP, 128], [1, FL]]), frep[:, :], [frep], [ftrk])
            for d in range(5):
                delta = -128 + 128 * d
                kb.dma("sp", biasT[d][:, :], bass.AP(fscr[hh], 511 - delta, [[FL, 128], [1, 512]]),
                       [ftrk], [biasT[d]])

            steps = [(I, j) for I in range(NQB) for j in range(4 * I + 4)]
            qblk = {}
            deferred = []

            def get_q(I):
                if I not in qblk:
                    qb = qr.next()
                    kb.dma("sp", qb[:, :], qT[hh * 128:(hh + 1) * 128, 512 * I:512 * (I + 1)], [], [qb])
                    qblk[I] = qb
                return qblk[I]

            def qk(I, j):
                qb = get_q(I)
                ps = s12.next()

                def fn(e, ps=ps, qb=qb, j=j):
                    e.matmul(ps[:, 0:512], kTs[0:64, 128 * j:128 * (j + 1)], qb[0:64, :],
                             start=True, stop=True)
                    return e.matmul(ps[:, 512:1024], kTs[64:128, 128 * j:128 * (j + 1)], qb[64:128, :],
                                    start=True, stop=True)
                kb.op("pe", [kTs, qb], [ps], fn)
                return ps

            pend = [qk(*steps[k]) for k in range(min(2, len(steps)))]
            for si, (I, j) in enumerate(steps):
                cur = pend.pop(0)
                pb = pr.next()
                if j <= 4 * I - 2:
                    kb.op("act", [cur, c31], [pb], lambda e, cur=cur, pb=pb: e.activation(
                        pb[:, :], cur[:, :], AF.Exp, bias=c31[:, 0:1], scale=0.125))
                else:
                    d = j - (4 * I - 1)
                    bt = biasT[d]
                    tm = tmpr.next()
                    for h2 in range(2):
                        kb.op("dve", [cur, bt], [tm], lambda e, cur=cur, tm=tm, bt=bt, h2=h2: e.scalar_tensor_tensor(
                            tm[:, 512 * h2:512 * (h2 + 1)], cur[:, 512 * h2:512 * (h2 + 1)], 0.125, bt[:, :],
                            ALU.mult, ALU.add))
                    kb.op("act", [tm], [pb], lambda e, tm=tm, pb=pb: e.activation(
                        pb[:, :], tm[:, :], AF.Exp))
                last = (j == 4 * I + 3)
                if si + 2 < len(steps):
                    pend.append(qk(*steps[si + 2]))

                def av(e, pb=pb, j=j, last=last):
                    ins = None
                    for h2 in range(2):
                        for qc in range(4):
                            g = h2 * 4 + qc
                            ins = e.matmul(acc_ap(h2, qc), pb[:, 512 * h2 + 128 * qc:512 * h2 + 128 * (qc + 1)],
                                           vaug[:, j, :], start=(j == 0 and g % 3 == 0), stop=last,
                                           skip_group_check=True)
                    return ins
                kb.op("pe", [pb, vaug], [oacc], av)
                if last:
                    ob = osb.next()
                    for bnk in range(3):
                        ng = 3 if bnk < 2 else 2
                        kb.op("dve", [oacc], [ob], lambda e, ob=ob, bnk=bnk, ng=ng: e.tensor_copy(
                            ob[:, 3 * bnk:3 * bnk + ng, 0:129],
                            oacc[:, bnk, 0:ng * 129].rearrange("p (g c) -> p g c", g=ng)))
                    rl = smr.next()
                    of = ofr.next()
                    kb.op("dve", [ob], [rl], lambda e, ob=ob, rl=rl: e.reciprocal(
                        rl[:, 0:8].unsqueeze(2), ob[:, 0:8, 128:129]))
                    kb.op("dve", [rl, nlam], [rl], lambda e, rl=rl: e.tensor_scalar(
                        rl[:, 4:8], rl[:, 4:8], nlam[:, 0:1], None, ALU.mult))
                    kb.op("dve", [ob, rl], [of], lambda e, ob=ob, rl=rl, of=of: e.tensor_tensor(
                        of[:, :, :], ob[:, 0:4, 0:128], rl[:, 0:4].unsqueeze(2).broadcast_to([128, 4, 128]),
                        ALU.mult))
                    kb.op("dve", [ob, rl], [ob], lambda e, ob=ob, rl=rl: e.tensor_tensor(
                        ob[:, 4:8, 0:128], ob[:, 4:8, 0:128],
                        rl[:, 4:8].unsqueeze(2).broadcast_to([128, 4, 128]), ALU.mult))
                    kb.op("dve", [ob, of], [of], lambda e, ob=ob, of=of: e.tensor_tensor(
                        of[:, :, :], of[:, :, :], ob[:, 4:8, 0:128], ALU.add))
                    kb.op("dve", [of], [ob], lambda e, ob=ob, of=of: e.tensor_tensor(
                        ob[:, 0:4, 0:128], of[:, :, :], of[:, :, :], ALU.mult))
                    kb.op("dve", [ob], [rl], lambda e, ob=ob, rl=rl: e.reduce_sum(
                        rl[:, 8:12], ob[:, 0:4, 0:128], axis=AX.X))

                    def e2(rl=rl):
                        kb.op("act", [rl, epst], [rl], lambda e: e.activation(
                            rl[:, 12:16], rl[:, 8:12], AF.Ln, bias=epst[:, 0:1], scale=1.0 / 128))
                        kb.op("act", [rl], [rl], lambda e: e.activation(
                            rl[:, 8:12], rl[:, 12:16], AF.Exp, scale=-0.5))

                    def e3(rl=rl, of=of, I=I):
                        on = onr.next()
                        og = ostage.next()
                        kb.op("dve", [of, rl], [of], lambda e: e.tensor_tensor(
                            of[:, :, :], of[:, :, :], rl[:, 8:12].unsqueeze(2).broadcast_to([128, 4, 128]),
                            ALU.mult))
                        kb.op("dve", [of, g_rep], [on], lambda e: e.tensor_tensor(
                            on[:, :, :], of[:, :, :], g_rep[:, :].unsqueeze(1).broadcast_to([128, 4, 128]),
                            ALU.mult))

                        def trq(e):
                            ins = None
                            for qc in range(4):
                                ins = e.transpose(pT[:, qc, :], on[:, qc, :], identb[:, :])
                            return ins
                        kb.op("pe", [on, identb], [pT], trq)
                        kb.op("dve", [pT], [og], lambda e: e.tensor_copy(
                            og[:, :].rearrange("p (a b) -> p a b", a=4), pT[:, :, :]))
                        kb.dma("sp", oT[hh * 128:(hh + 1) * 128, 512 * I:512 * (I + 1)], og[:, :], [og], [OUT])
                    deferred.append((si + 3, e2))
                    deferred.append((si + 6, e3))
                while deferred and deferred[0][0] <= si:
                    deferred.pop(0)[1]()
            while deferred:
                deferred.pop(0)[1]()
        kb.finish()
    return nc


def load_w_resident(kb, name, w_ap, ncols, wst):
    wb = kb.sb(name, [128, NKC, ncols], BF16)
    for p in range(0, ncols, 512):
        nco = min(512, ncols - p)
        ws = wst.next()
        kb.dma("sp", ws[:, :, 0:nco], w_ap[:, p:p + nco].rearrange("(kc p) n -> p kc n", p=128), [], [ws])
        kb.op("pool", [ws], [wb], lambda e, ws=ws, p=p, nco=nco: e.tensor_copy(
            wb[:, :, p:p + nco], ws[:, :, 0:nco]))
    return wb


def build_C1(T):
    nc = bass.Bass("TRN2", target_bir_lowering=False)
    NB = T // 512
    with ExitStack() as st:
        kb = KB(nc, st)
        dt = nc.dram_tensor
        oT = dt("oT", [D, T], BF16, kind="ExternalInput").ap()
        SaT = dt("SaT", [D, T], BF16, kind="ExternalInput").ap()
        YT = dt("YT", [D, T], BF16, kind="ExternalInput").ap()
        h = dt("h", [T, D], F32, kind="ExternalInput").ap()
        w_ap = dt("w_ap", [D, D], F32, kind="ExternalInput").ap()
        w_out = dt("w_out", [D, D], F32, kind="ExternalInput").ap()
        lng = dt("lng", [128, D], F32, kind="ExternalInput").ap()
        lnb = dt("lnb", [128, D], F32, kind="ExternalInput").ap()
        h1 = dt("h1", [T, D], F32, kind="ExternalOutput").ap()
        OUT = kb.out_trk
        g_rep = load_const(kb, "g_rep", lng, [128, D])
        b_rep = load_const(kb, "b_rep", lnb, [128, D])
        wst = Ring([kb.sb("wst%d" % i, [128, NKC, 512], F32) for i in range(2)])
        wap_b = load_w_resident(kb, "wap_b", w_ap, D, wst)
        wout_b = load_w_resident(kb, "wout_b", w_out, D, wst)
        psr = PsumRing(kb, 6)
        obr = Ring([kb.sb("ob%d" % i, [128, NKC, 512], BF16) for i in range(2)])
        sar = Ring([kb.sb("sa%d" % i, [128, NKC, 512], BF16) for i in range(2)])
        yr = Ring([kb.sb("yb%d" % i, [128, NKC, 512], BF16) for i in range(2)])
        mbr = Ring([kb.sb("mb%d" % i, [128, NKC, 512], BF16) for i in range(2)])
        tmr = Ring([kb.sb("tm%d" % i, [128, 512], F32) for i in range(3)])
        xr = Ring([kb.sb("x%d" % i, [128, D], F32) for i in range(3)])
        st6 = kb.sb("st6", [128, 12], F32)
        mv = kb.sb("mv", [128, 2], F32)
        rstd = kb.sb("rstd", [128, 1], F32)
        for n in range(NB):
            ob, sa, yb, mb = obr.next(), sar.next(), yr.next(), mbr.next()
            sl = slice(512 * n, 512 * (n + 1))
            kb.dma("sp", ob[:, :, :], oT[:, sl].rearrange("(c p) t -> p c t", p=128), [], [ob])
            kb.dma("sp", sa[:, :, :], SaT[:, sl].rearrange("(c p) t -> p c t", p=128), [], [sa])
            kb.dma("sp", yb[:, :, :], YT[:, sl].rearrange("(c p) t -> p c t", p=128), [], [yb])
            for j in range(8):
                ps = psr.next()
                mm_group(kb, ps, ps[:, :], [wap_b[:, kc, 128 * j:128 * (j + 1)] for kc in range(NKC)],
                         [ob[:, kc, :] for kc in range(NKC)], [wap_b, ob])
                tm = tmr.next()
                kb.op("dve", [ps, sa], [tm], lambda e, ps=ps, sa=sa, tm=tm, j=j: e.tensor_tensor(
                    tm[:, :], ps[:, :], sa[:, j, :], ALU.mult))
                kb.op("pool", [tm, yb], [mb], lambda e, tm=tm, yb=yb, mb=mb, j=j: e.tensor_tensor(
                    mb[:, j, :], tm[:, :], yb[:, j, :], ALU.add))
            for a in range(4):
                i = 4 * n + a
                x = xr.next()
                kb.dma("sp", x[:, :], h[i * 128:(i + 1) * 128, :], [], [x])
                for half in range(2):
                    ps = psr.next()
                    mm_group(kb, ps, ps[:, :], [mb[:, kc, 128 * a:128 * (a + 1)] for kc in range(NKC)],
                             [wout_b[:, kc, 512 * half:512 * (half + 1)] for kc in range(NKC)], [wout_b, mb])
                    kb.op("dve", [ps, x], [x], lambda e, ps=ps, x=x, half=half: e.scalar_tensor_tensor(
                        x[:, 512 * half:512 * (half + 1)], x[:, 512 * half:512 * (half + 1)], DN_ALPHA,
                        ps[:, :], ALU.mult, ALU.add))
                layer_norm_tile(kb, x, x, g_rep, b_rep, st6, mv, rstd)
                kb.dma("act", h1[i * 128:(i + 1) * 128, :], x[:, :], [x], [OUT])
        kb.finish()
    return nc


def build_C2(T):
    nc = bass.Bass("TRN2", target_bir_lowering=False)
    NT = T // 128
    NBLK = (2 * T + 32 * (MOE_BS - 1) + MOE_BS - 1) // MOE_BS
    NSUB = MOE_BS // 128
    with ExitStack() as st:
        kb = KB(nc, st)
        dt = nc.dram_tensor
        h1 = dt("h1", [T, D], F32, kind="ExternalInput").ap()
        w_r = dt("w_r", [D, 36], F32, kind="ExternalInput").ap()
        b_r = dt("b_r", [128, 36], F32, kind="ExternalInput").ap()
        w1 = dt("w1", [32, D, 512], F32, kind="ExternalInput").ap()
        w3 = dt("w3", [32, D, 512], F32, kind="ExternalInput").ap()
        w2 = dt("w2", [32, 512, D], F32, kind="ExternalInput").ap()
        lng = dt("lng", [128, D], F32, kind="ExternalInput").ap()
        lnb = dt("lnb", [128, D], F32, kind="ExternalInput").ap()
        ident = dt("ident", [128, 128], F32, kind="ExternalInput").ap()
        utri = dt("utri", [128, 128], F32, kind="ExternalInput").ap()
        blk128 = dt("blk128", [128, NBLK], F32, kind="ExternalInput").ap()
        pidx_d = dt("pidx", [128, 1], F32, kind="ExternalInput").ap()
        h2 = dt("h2", [T, D], F32, kind="ExternalOutput").ap()
        xbuf = nc.dram_tensor("xbuf", [NBLK * MOE_BS, D], BF16).ap()
        ybuf = nc.dram_tensor("ybuf", [NBLK * MOE_BS, D], F32).ap()
        OUT = kb.out_trk

        g_rep = load_const(kb, "g_rep", lng, [128, D])
        b_rep = load_const(kb, "b_rep", lnb, [128, D])
        identf = load_const(kb, "identf", ident, [128, 128])
        identb = kb.sb("identb", [128, 128], BF16)
        kb.op("dve", [identf], [identb], lambda e: e.tensor_copy(identb[:, :], identf[:, :]))
        utf = load_const(kb, "utf", utri, [128, 128])
        utb = kb.sb("utb", [128, 128], BF16)
        kb.op("dve", [utf], [utb], lambda e: e.tensor_copy(utb[:, :], utf[:, :]))
        onesb = kb.sb("onesb", [128, 128], BF16)
        kb.op("dve", [], [onesb], lambda e: e.memset(onesb[:, :], 1.0))
        b128 = load_const(kb, "b128", blk128, [128, NBLK])
        brs = load_const(kb, "brs", b_r, [128, 36])
        wrf = kb.sb("wrf", [128, NKC, 36], F32)
        kb.dma("sp", wrf[:, :, :], w_r.rearrange("(kc p) n -> p kc n", p=128), [], [wrf])
        wrb = kb.sb("wrb", [128, NKC, 36], BF16)
        kb.op("dve", [wrf], [wrb], lambda e: e.tensor_copy(wrb[:, :, :], wrf[:, :, :]))

        psr = PsumRing(kb, 6)
        pT = kb.ps("pT", [128, NKC, 128], BF16)
        pT2 = kb.ps("pT2", [128, 4, 128], BF16)
        xr = Ring([kb.sb("x%d" % i, [128, D], F32) for i in range(3)])
        hbr = Ring([kb.sb("hb%d" % i, [128, D], BF16) for i in range(2)])
        hTr = Ring([kb.sb("hTt%d" % i, [128, NKC, 128], BF16) for i in range(2)])
        st6 = kb.sb("st6", [128, 12], F32)
        mv = kb.sb("mv", [128, 2], F32)
        rstd = kb.sb("rstd", [128, 1], F32)

        M1 = kb.sb("M1", [128, NT, 32], F32)
        M2 = kb.sb("M2", [128, NT, 32], F32)
        Mb = kb.sb("Mb", [128, NT, 32], BF16)
        rank = kb.sb("rank", [128, NT, 32], F32)
        gates = kb.sb("gates", [128, NT, 2], F32)
        destf = kb.sb("destf", [128, NT * 2], F32)
        desti = kb.sb("desti", [128, NT * 2], I32)
        lgr = Ring([kb.sb("lg%d" % i, [128, 36], F32) for i in range(2)])
        smr = Ring([kb.sb("smr%d" % i, [128, 48], F32) for i in range(2)])

        for i in range(NT):
            x = xr.next()
            hb = hbr.next()
            hTt = hTr.next()
            kb.dma("sp", x[:, :], h1[i * 128:(i + 1) * 128, :], [], [x])
            kb.op("act", [x], [hb], lambda e, x=x, hb=hb: e.copy(hb[:, :], x[:, :]))
            transpose_to(kb, hb, identb, pT, hTt[:, :, :], [hTt], NKC, eng="act")
            ps = psr.next()
            mm_group(kb, ps, ps[:, 0:36], [hTt[:, kc, :] for kc in range(NKC)],
                     [wrb[:, kc, :] for kc in range(NKC)], [hTt, wrb])
            lg = lgr.next()
            sm = smr.next()
            m1 = M1.trk(i)
            m2 = M2.trk(i)
            kb.op("dve", [ps, brs], [lg], lambda e, ps=ps, lg=lg: e.tensor_tensor(
                lg[:, :], ps[:, 0:36], brs[:, :], ALU.add))
            kb.op("dve", [lg], [sm], lambda e, lg=lg, sm=sm: e.reduce_max(sm[:, 0:1], lg[:, 0:4], axis=AX.X))
            kb.op("dve", [sm], [sm], lambda e, sm=sm: e.tensor_scalar(
                sm[:, 1:2], sm[:, 0:1], -1.0, None, ALU.mult))
            kb.op("act", [lg, sm], [sm], lambda e, lg=lg, sm=sm: e.activation(
                sm[:, 44:48], lg[:, 0:4], AF.Exp, bias=sm[:, 1:2], accum_out=sm[:, 2:3]))
            kb.op("dve", [sm], [sm], lambda e, sm=sm: e.reciprocal(sm[:, 3:4], sm[:, 2:3]))
            kb.op("dve", [lg, sm], [sm], lambda e, lg=lg, sm=sm: e.tensor_scalar(
                sm[:, 4:8], lg[:, 0:4], sm[:, 0:1], None, ALU.is_equal))
            kb.op("dve", [lg, sm], [sm], lambda e, lg=lg, sm=sm: e.tensor_scalar(
                sm[:, 8:16], lg[:, 4:12], sm[:, 4:5], None, ALU.mult))
            for g in range(1, 4):
                kb.op("dve", [lg, sm], [sm], lambda e, lg=lg, sm=sm, g=g: e.scalar_tensor_tensor(
                    sm[:, 8:16], lg[:, 4 + 8 * g:12 + 8 * g], sm[:, 4 + g:5 + g], sm[:, 8:16], ALU.mult, ALU.add))
            kb.op("dve", [sm], [sm], lambda e, sm=sm: e.max(sm[:, 16:24], sm[:, 8:16]))
            kb.op("dve", [sm], [sm], lambda e, sm=sm: e.tensor_scalar(
                sm[:, 24:32], sm[:, 8:16], sm[:, 16:17], None, ALU.is_equal))
            kb.op("dve", [sm], [sm], lambda e, sm=sm: e.tensor_scalar(
                sm[:, 32:40], sm[:, 8:16], sm[:, 17:18], None, ALU.is_equal))
            kb.op("dve", [sm], [sm], lambda e, sm=sm: e.tensor_tensor(
                sm[:, 40:41], sm[:, 17:18], sm[:, 16:17], ALU.subtract))
            kb.op("act", [sm], [sm], lambda e, sm=sm: e.activation(sm[:, 40:41], sm[:, 40:41], AF.Exp))
            kb.op("dve", [sm], [sm], lambda e, sm=sm: e.tensor_scalar(
                sm[:, 41:42], sm[:, 40:41], 1.0, None, ALU.add))
            kb.op("dve", [sm], [sm], lambda e, sm=sm: e.reciprocal(sm[:, 41:42], sm[:, 41:42]))
            kb.op("dve", [sm], [gates.trk(i)], lambda e, sm=sm, i=i: e.tensor_tensor(
                gates[:, i, 0:1], sm[:, 3:4], sm[:, 41:42], ALU.mult))
            kb.op("dve", [sm, gates.trk(i)], [gates.trk(i)], lambda e, sm=sm, i=i: e.tensor_tensor(
                gates[:, i, 1:2], gates[:, i, 0:1], sm[:, 40:41], ALU.mult))
            for g in range(4):
                kb.op("dve", [sm], [m1], lambda e, sm=sm, i=i, g=g: e.tensor_scalar(
                    M1[:, i, 8 * g:8 * g + 8], sm[:, 24:32], sm[:, 4 + g:5 + g], None, ALU.mult))
                kb.op("dve", [sm], [m2], lambda e, sm=sm, i=i, g=g: e.tensor_scalar(
                    M2[:, i, 8 * g:8 * g + 8], sm[:, 32:40], sm[:, 4 + g:5 + g], None, ALU.mult))
            kb.op("dve", [m1, m2], [Mb.trk(i)], lambda e, i=i: e.tensor_tensor(
                Mb[:, i, :], M1[:, i, :], M2[:, i, :], ALU.add))

        maccr = Ring([kb.sb("macc%d" % i, [128, 32], BF16) for i in range(2)])
        macc = maccr.next()
        kb.op("dve", [], [macc], lambda e, macc=macc: e.memset(macc[:, :], 0.0))
        for i in range(NT):
            ps = psr.next()

            def rk(e, ps=ps, i=i, macc=macc):
                e.matmul(ps[:, 0:32], utb[:, :], Mb[:, i, :], start=True, stop=False)
                return e.matmul(ps[:, 0:32], onesb[:, :], macc[:, :], start=False, stop=True)
            kb.op("pe", [utb, onesb, Mb.trk(i), macc], [ps], rk)
            kb.op("act", [ps], [rank.trk(i)], lambda e, ps=ps, i=i: e.copy(rank[:, i, :], ps[:, 0:32]))
            nm = maccr.next()
            kb.op("dve", [macc, Mb.trk(i)], [nm], lambda e, macc=macc, nm=nm, i=i: e.tensor_tensor(
                nm[:, :], macc[:, :], Mb[:, i, :], ALU.add))
            macc = nm
        ps = psr.next()
        kb.op("pe", [onesb, macc], [ps], lambda e, ps=ps, macc=macc: e.matmul(
            ps[:, 0:32], onesb[:, :], macc[:, :], start=True, stop=True))
        sc = kb.sb("sc", [128, 6, 32], F32)
        SC = [sc]
        kb.op("dve", [ps], SC, lambda e, ps=ps: e.tensor_copy(sc[:, 0, :], ps[:, 0:32]))
        sci = kb.sb("sci", [128, 2, 32], I32)
        kb.op("dve", SC, SC, lambda e: e.tensor_scalar(sc[:, 1, :], sc[:, 0, :], float(MOE_BS - 1), None, ALU.add))
        kb.op("dve", SC, [sci], lambda e: e.tensor_copy(sci[:, 0, :], sc[:, 1, :]))
        kb.op("dve", [sci], [sci], lambda e: e.tensor_scalar(
            sci[:, 1, :], sci[:, 0, :], MOE_SH, MOE_SH, ALU.arith_shift_right, ALU.logical_shift_left))
        kb.op("dve", [sci], SC, lambda e: e.tensor_copy(sc[:, 2, :], sci[:, 1, :]))
        kb.op("dve", SC, SC, lambda e: e.tensor_copy(sc[:, 3, :], sc[:, 2, :]))
        a, b = 3, 4
        for sft in (1, 2, 4, 8, 16):
            kb.op("dve", SC, SC, lambda e, a=a, b=b: e.tensor_copy(sc[:, b, :], sc[:, a, :]))
            kb.op("dve", SC, SC, lambda e, a=a, b=b, sft=sft: e.tensor_tensor(
                sc[:, b, sft:32], sc[:, a, sft:32], sc[:, a, 0:32 - sft], ALU.add))
            a, b = b, a
        pe_idx = a
        kb.op("dve", SC, SC, lambda e: e.tensor_tensor(sc[:, 5, :], sc[:, pe_idx, :], sc[:, 2, :], ALU.subtract))
        tmpd = Ring([kb.sb("tmpd%d" % i, [128, 32], F32) for i in range(2)])
        junk32 = kb.sb("junk32", [128, 2, 32], F32)
        for i in range(NT):
            td = tmpd.next()
            kb.op("dve", SC + [rank.trk(i)], [td], lambda e, td=td, i=i: e.tensor_tensor(
                td[:, :], rank[:, i, :], sc[:, 5, :], ALU.add))
            for jj, (MM, mt) in enumerate(((M1, M1.trk(i)), (M2, M2.trk(i)))):
                td2 = tmpd.next() if False else None
                kb.op("dve", [td, mt], [destf], lambda e, td=td, MM=MM, i=i, jj=jj: e.tensor_tensor(
                    junk32[:, jj, :], td[:, :], MM[:, i, :], ALU.mult))
                kb.op("dve", [destf], [destf], lambda e, i=i, jj=jj: e.reduce_sum(
                    destf[:, 2 * i + jj:2 * i + jj + 1], junk32[:, jj, :], axis=AX.X))
        kb.op("dve", [destf], [desti], lambda e: e.tensor_copy(desti[:, :], destf[:, :]))
        blkf = kb.sb("blkf", [128, NBLK], F32)
        kb.op("dve", [], [blkf], lambda e: e.memset(blkf[:, :], 0.0))
        for ex in range(32):
            kb.op("dve", SC + [b128, blkf], [blkf], lambda e, ex=ex: e.scalar_tensor_tensor(
                blkf[:, :], b128[:, :], sc[:, pe_idx, ex:ex + 1], blkf[:, :], ALU.is_ge, ALU.add))
        kb.op("dve", [blkf], [blkf], lambda e: e.tensor_scalar(blkf[:, :], blkf[:, :], 31.0, None, ALU.min))

        pidx = load_const(kb, "pidx_s", pidx_d, [128, 1])
        widx = kb.sb("widx", [128, NBLK], I32)
        kb.op("dve", [blkf, pidx], [blkf], lambda e: e.tensor_scalar(
            blkf[:, :], blkf[:, :], 128.0, pidx[:, 0:1], ALU.mult, ALU.add))
        kb.op("dve", [blkf], [widx], lambda e: e.tensor_copy(widx[:, :], blkf[:, :]))
        w1rows = w1.rearrange("e (p kk) n -> (e p) (kk n)", kk=8)
        w3rows = w3.rearrange("e (p kk) n -> (e p) (kk n)", kk=8)
        w2rows = w2.rearrange("e (p kk) n -> (e p) (kk n)", kk=4)

        zt = kb.sb("zt", [128, D], BF16)
        kb.op("pool", [], [zt], lambda e: e.memset(zt[:, :], 0.0))
        xz = Trk()
        for bq in range(NBLK * NSUB):
            kb.dma("sp", xbuf[bq * 128:(bq + 1) * 128, :], zt[:, :], [zt], [xz])
        xs = Trk()
        for i in range(NT):
            x = xr.next()
            hb = hbr.next()
            kb.dma("sp", x[:, :], h1[i * 128:(i + 1) * 128, :], [], [x])
            kb.op("act", [x], [hb], lambda e, x=x, hb=hb: e.copy(hb[:, :], x[:, :]))
            for jj in range(2):
                kb.dma_fn("pool", lambda e, hb=hb, i=i, jj=jj: e.indirect_dma_start(
                    out=xbuf, out_offset=bass.IndirectOffsetOnAxis(ap=desti[:, 2 * i + jj:2 * i + jj + 1], axis=0),
                    in_=hb[:, :], in_offset=None), [hb, desti, xz], [xs])

        w1s = Ring([kb.sb("w1s%d" % i, [128, NKC, 512], F32) for i in range(2)])
        w3s = Ring([kb.sb("w3s%d" % i, [128, NKC, 512], F32) for i in range(1)])
        w2s = Ring([kb.sb("w2s%d" % i, [128, 4, D], F32) for i in range(1)])
        w1b = Ring([kb.sb("w1b%d" % i, [128, NKC, 512], BF16) for i in range(2)])
        w3b = Ring([kb.sb("w3b%d" % i, [128, NKC, 512], BF16) for i in range(2)])
        w2b = Ring([kb.sb("w2b%d" % i, [128, 4, D], BF16) for i in range(2)])
        xbr = Ring([kb.sb("xb%d" % i, [128, D], BF16) for i in range(2)])
        xTr = Ring([kb.sb("xT%d" % i, [128, NKC, 128], BF16) for i in range(2)])
        sar = Ring([kb.sb("sA%d" % i, [128, 512], F32) for i in range(2)])
        gr = Ring([kb.sb("g%d" % i, [128, 512], BF16) for i in range(3)])
        gTr = Ring([kb.sb("gT%d" % i, [128, 4, 128], BF16) for i in range(2)])
        yr = Ring([kb.sb("y%d" % i, [128, D], F32) for i in range(2)])
        ys = Trk()
        cur_w = {}

        def stage1(sidx):
            bq, sub = divmod(sidx, NSUB)
            if sub == 0:
                ws1, ws3, ws2 = w1s.next(), w3s.next(), w2s.next()
                for wsx, wrows in ((ws1, w1rows), (ws3, w3rows), (ws2, w2rows)):
                    kb.dma_fn("pool", lambda e, wsx=wsx, wrows=wrows, bq=bq: e.indirect_dma_start(
                        out=wsx[:, :, :].rearrange("p a b -> p (a b)"), out_offset=None, in_=wrows,
                        in_offset=bass.IndirectOffsetOnAxis(ap=widx[:, bq:bq + 1], axis=0)), [widx], [wsx])
                wb1, wb3, wb2 = w1b.next(), w3b.next(), w2b.next()
                kb.op("dve", [ws1], [wb1], lambda e: e.tensor_copy(wb1[:, :, :], ws1[:, :, :]))
                kb.op("dve", [ws3], [wb3], lambda e: e.tensor_copy(wb3[:, :, :], ws3[:, :, :]))
                kb.op("act", [ws2], [wb2], lambda e: e.copy(wb2[:, :, :], ws2[:, :, :]))
                cur_w[bq] = (wb1, wb3, wb2)
            wb1, wb3, wb2 = cur_w[bq]
            r0 = sidx * 128
            xb = xbr.next()
            kb.dma("sp", xb[:, :], xbuf[r0:r0 + 128, :], [xs], [xb])
            xT = xTr.next()
            transpose_to(kb, xb, identb, pT, xT[:, :, :], [xT], NKC, eng="dve", strided=True)
            pa, pb = psr.next(), psr.next()
            mm_group(kb, pa, pa[:, :], [xT[:, kc, :] for kc in range(NKC)],
                     [wb1[:, kc, :] for kc in range(NKC)], [xT, wb1])
            mm_group(kb, pb, pb[:, :], [xT[:, kc, :] for kc in range(NKC)],
                     [wb3[:, kc, :] for kc in range(NKC)], [xT, wb3])
            sA = sar.next()
            gg = gr.next()
            kb.op("act", [pa], [sA], lambda e: e.activation(sA[:, :], pa[:, :], AF.Silu))
            kb.op("dve", [sA, pb], [gg], lambda e: e.tensor_tensor(gg[:, :], sA[:, :], pb[:, :], ALU.mult))
            return (r0, gg, wb2)

        def stage2(ctx):
            r0, gg, wb2 = ctx
            gT = gTr.next()

            def trg(e):
                ins = None
                for c in range(4):
                    ins = e.transpose(pT2[:, c, :], gg[:, slice(c, None, 4)], identb[:, :])
                return ins
            kb.op("pe", [gg, identb], [pT2], trg)
            kb.op("dve", [pT2], [gT], lambda e: e.tensor_copy(gT[:, :, :], pT2[:, :, :]))
            y = yr.next()
            for half in range(2):
                pc = psr.next()
                mm_group(kb, pc, pc[:, :], [gT[:, kc, :] for kc in range(4)],
                         [wb2[:, kc, 512 * half:512 * (half + 1)] for kc in range(4)], [gT, wb2])
                if half == 0:
                    kb.op("act", [pc], [y], lambda e, pc=pc: e.copy(y[:, 0:512], pc[:, :]))
                else:
                    kb.op("dve", [pc], [y], lambda e, pc=pc: e.tensor_copy(y[:, 512:1024], pc[:, :]))
            kb.dma("act", ybuf[r0:r0 + 128, :], y[:, :], [y], [ys])

        NS_ALL = NBLK * NSUB
        ctx = stage1(0)
        for sidx in range(NS_ALL):
            nctx = stage1(sidx + 1) if sidx + 1 < NS_ALL else None
            stage2(ctx)
            ctx = nctx

        y1r = yr
        y2r = Ring([kb.sb("y2_%d" % i, [128, D], F32) for i in range(2)])
        for i in range(NT):
            x = xr.next()
            y1, y2 = y1r.next(), y2r.next()
            kb.dma("sp", x[:, :], h1[i * 128:(i + 1) * 128, :], [], [x])
            for jj, yy in enumerate((y1, y2)):
                kb.dma_fn("pool", lambda e, yy=yy, i=i, jj=jj: e.indirect_dma_start(
                    out=yy[:, :], out_offset=None, in_=ybuf,
                    in_offset=bass.IndirectOffsetOnAxis(ap=desti[:, 2 * i + jj:2 * i + jj + 1], axis=0)),
                    [desti, ys], [yy])
            gt = gates.trk(i)
            kb.op("dve", [y1, gt], [y1], lambda e, y1=y1, i=i: e.tensor_scalar(
                y1[:, :], y1[:, :], gates[:, i, 0:1], None, ALU.mult))
            kb.op("dve", [y1, y2, gt], [y1], lambda e, y1=y1, y2=y2, i=i: e.scalar_tensor_tensor(
                y1[:, :], y2[:, :], gates[:, i, 1:2], y1[:, :], ALU.mult, ALU.add))
            kb.op("dve", [x, y1], [x], lambda e, x=x, y1=y1: e.scalar_tensor_tensor(
                x[:, :], x[:, :], DN_ALPHA, y1[:, :], ALU.mult, ALU.add))
            layer_norm_tile(kb, x, x, g_rep, b_rep, st6, mv, rstd)
            kb.dma("act", h2[i * 128:(i + 1) * 128, :], x[:, :], [x], [OUT])
        kb.finish()
    return nc


def build_LN0(T):
    nc = bass.Bass("TRN2", target_bir_lowering=False)
    with ExitStack() as st:
        kb = KB(nc, st)
        dt = nc.dram_tensor
        x_d = dt("x", [T, D], F32, kind="ExternalInput").ap()
        lng = dt("lng", [128, D], F32, kind="ExternalInput").ap()
        lnb = dt("lnb", [128, D], F32, kind="ExternalInput").ap()
        h0 = dt("h0", [T, D], F32, kind="ExternalOutput").ap()
        g_rep = load_const(kb, "g_rep", lng, [128, D])
        b_rep = load_const(kb, "b_rep", lnb, [128, D])
        xr = Ring([kb.sb("x%d" % i, [128, D], F32) for i in range(4)])
        st6 = kb.sb("st6", [128, 12], F32)
        mv = kb.sb("mv", [128, 2], F32)
        rstd = kb.sb("rstd", [128, 1], F32)
        for i in range(T // 128):
            x = xr.next()
            kb.dma("sp", x[:, :], x_d[i * 128:(i + 1) * 128, :], [], [x])
            layer_norm_tile(kb, x, x, g_rep, b_rep, st6, mv, rstd)
            kb.dma("act", h0[i * 128:(i + 1) * 128, :], x[:, :], [x], [kb.out_trk])
        kb.finish()
    return nc


_PROGS = {}


def _prog(name, fn, *args):
    key = (name,) + args
    if key not in _PROGS:
        _PROGS[key] = fn(*args)
    return _PROGS[key]


def _lay(a):
    J = a.shape[0]
    return np.ascontiguousarray(a.reshape(J, 8, 128).transpose(2, 0, 1).reshape(128, J * 8))


def _rep(a):
    return np.ascontiguousarray(np.broadcast_to(np.asarray(a, np.float32).reshape(1, -1), (128, a.size)))


def kernel(x, ln0_g, ln0_b, rel_table, w_in, gate_b, lam_vecs, subln_g, w_attn_proj, conv_w,
           w_conv_proj, w_out, ln1_g, ln1_b, w_rg, b_rg, w_re, b_re, w1, w3, w2, ln2_g, ln2_b):
    f32 = np.float32
    x = np.asarray(x, f32)
    B, S, _ = x.shape
    NC = 8
    T = B * S // NC
    CPS = S // T
    cores = list(range(NC))
    depth = w_in.shape[0]
    ident = np.eye(128, dtype=f32)
    xt = x.reshape(B * S, D)

    def run(nc, maps):
        return run_bass_kernel_spmd(nc, maps, core_ids=cores).results

    h = [None] * NC

    oh = bias_onehot()
    NBLK = (2 * T + 32 * (MOE_BS - 1) + MOE_BS - 1) // MOE_BS
    utri = np.triu(np.ones((128, 128), f32), 1)
    blk128 = np.ascontiguousarray(np.broadcast_to(np.arange(NBLK, dtype=f32) * MOE_BS, (128, NBLK)))
    pidx = np.arange(128, dtype=f32).reshape(128, 1)

    for l in range(depth):
        lam_init = 0.8 - 0.6 * math.exp(-0.3 * l)
        maps = []
        for c in cores:
            first = (c % CPS == 0)
            if l == 0:
                halo = np.zeros((128, D), f32) if first else xt[c * T - 128:c * T]
                own = xt[c * T:(c + 1) * T]
            else:
                halo = np.zeros((128, D), f32) if first else h[c - 1][T - 128:]
                own = h[c]
            m = {"hin": np.concatenate([halo, own], 0),
                 "halo_mask": np.full((128, 1), 0.0 if first else 1.0, f32),
                 "w_in": np.asarray(w_in[l], f32), "gate_b": _lay(np.asarray(gate_b[l], f32)),
                 "conv_w": _lay(np.asarray(conv_w[l], f32)),
                 "w_cp": np.asarray(w_conv_proj[l], f32), "ident": ident}
            if l == 0:
                m["lng"] = _rep(ln0_g)
                m["lnb"] = _rep(ln0_b)
            maps.append(m)
        ra = run(_prog("A", build_A, T, l == 0), maps)
        if l == 0:
            h = [r["h0"] for r in ra]
        maps = []
        for c in cores:
            b, hp = c // CPS, c % CPS
            src = [ra[b * CPS + t] for t in range(CPS)]
            rows = slice(hp * 256, (hp + 1) * 256)
            tab = np.concatenate([np.asarray(rel_table, f32)[:, 2 * hp:2 * hp + 2],
                                  np.full((1, 2), -30000.0, f32)], 0)
            maps.append({"qT": np.concatenate([r["qT"][rows] for r in src], 1),
                         "kT": np.concatenate([r["kT"][rows] for r in src], 1),
                         "v": np.concatenate([r["v"][:, rows] for r in src], 0),
                         "oh": oh, "tab": tab,
                         "lamv": _rep(np.asarray(lam_vecs[l], f32).reshape(-1)),
                         "subg": _rep(subln_g[l]),
                         "lamc": np.ascontiguousarray(np.broadcast_to(
                             np.array([[-lam_init, 1.0 - lam_init]], f32), (128, 2))),
                         "ident": ident})
        rt = run(_prog("ATT", build_ATT, S), maps)
        maps = []
        for c in cores:
            b, t = c // CPS, c % CPS
            oT = np.concatenate([rt[b * CPS + hp]["oT"][:, t * T:(t + 1) * T] for hp in range(CPS)], 0)
            maps.append({"oT": oT, "SaT": ra[c]["SaT"], "YT": ra[c]["YT"], "h": h[c],
                         "w_ap": np.asarray(w_attn_proj[l], f32), "w_out": np.asarray(w_out[l], f32),
                         "lng": _rep(ln1_g[l]), "lnb": _rep(ln1_b[l])})
        r1 = run(_prog("C1", build_C1, T), maps)
        w_r = np.concatenate([np.asarray(w_rg[l], f32), np.asarray(w_re[l], f32)], 1)
        b_r = _rep(np.concatenate([np.asarray(b_rg[l], f32), np.asarray(b_re[l], f32)]))
        w1l, w3l, w2l = np.asarray(w1[l], f32), np.asarray(w3[l], f32), np.asarray(w2[l], f32)
        maps = [{"h1": r1[c]["h1"], "w_r": w_r, "b_r": b_r, "w1": w1l, "w3": w3l, "w2": w2l,
                 "lng": _rep(ln2_g[l]), "lnb": _rep(ln2_b[l]), "ident": ident, "utri": utri,
                 "blk128": blk128, "pidx": pidx} for c in cores]
        r2 = run(_prog("C2", build_C2, T), maps)
        h = [r["h2"] for r in r2]
    return np.concatenate(h, 0).reshape(B, S, D).astype(f32)
```
